# Optimizing a Trainium2 kernel written in Bass

```python
import math
import jax, jax.numpy as jnp
from jax import lax
import numpy as np

D_MODEL = 2048
BATCH = 1
SEQ = 8192
DEPTH = 2

HEAD_DIM = 128
N_GROUPS = 4
GROUP_WIDTH = D_MODEL // N_GROUPS
GROUP_HEADS = GROUP_WIDTH // HEAD_DIM
MIX_WIDTH = N_GROUPS * GROUP_WIDTH
ROPE_THETA = 500000.0
ROT_FRAC = 4
NORM_EPS = 1e-6
CONV_WIDTH = 31
SGU_CHUNK = 128
NSA_CMP_LEN = 32
NSA_CMP_STRIDE = 16
NSA_SEL_LEN = 64
NSA_SEL_TOP = 16
NSA_WINDOW = 512
NSA_N_BRANCH = 3
DSA_IDX_HEADS = 8
DSA_IDX_DIM = 64
DSA_TOPK_MAX = 256
PEER_HEADS = 8
PEER_NKEYS = 128
PEER_DKEY = 128
PEER_TOPK = 16
PEER_EXPERTS = PEER_NKEYS * PEER_NKEYS
Q_BLOCK = 128
NEG = -1e30

CONV_COLS = 2 * GROUP_WIDTH
SGU_COLS = 2 * GROUP_WIDTH
NSA_COLS = GROUP_WIDTH + 2 * NSA_N_BRANCH * HEAD_DIM + NSA_N_BRANCH * GROUP_HEADS
DSA_COLS = GROUP_WIDTH + 2 * HEAD_DIM + DSA_IDX_HEADS * DSA_IDX_DIM + DSA_IDX_DIM + DSA_IDX_HEADS
IN_COLS = CONV_COLS + SGU_COLS + NSA_COLS + DSA_COLS
IN_SPLITS = [CONV_COLS, CONV_COLS + SGU_COLS, CONV_COLS + SGU_COLS + NSA_COLS]
NSA_SPLITS = [GROUP_WIDTH, GROUP_WIDTH + 2 * NSA_N_BRANCH * HEAD_DIM]
DSA_SPLITS = [GROUP_WIDTH, GROUP_WIDTH + HEAD_DIM, GROUP_WIDTH + 2 * HEAD_DIM,
              GROUP_WIDTH + 2 * HEAD_DIM + DSA_IDX_HEADS * DSA_IDX_DIM,
              GROUP_WIDTH + 2 * HEAD_DIM + DSA_IDX_HEADS * DSA_IDX_DIM + DSA_IDX_DIM]

kernel_name = "hybrid_conv_sgu_nsa_dsa_peer"


def rms_norm(x, g):
    x32 = x.astype(jnp.float32)
    y = x32 * lax.rsqrt(jnp.mean(x32 * x32, axis=-1, keepdims=True) + NORM_EPS)
    return y.astype(x.dtype) * g


def layer_norm(x, g, b):
    x32 = x.astype(jnp.float32)
    mu = jnp.mean(x32, axis=-1, keepdims=True)
    var = jnp.mean(jnp.square(x32 - mu), axis=-1, keepdims=True)
    return ((x32 - mu) * lax.rsqrt(var + NORM_EPS)).astype(x.dtype) * g + b


def rope_tables(positions, rot_dim):
    half = rot_dim // 2
    inv = ROPE_THETA ** (-jnp.arange(half, dtype=jnp.float32) / half)
    ang = positions.astype(jnp.float32)[..., None] * inv
    return jnp.cos(ang), jnp.sin(ang)


def apply_rope(x, cos, sin):
    half = cos.shape[-1]
    if x.ndim == 4:
        cos, sin = cos[:, :, None], sin[:, :, None]
    c = cos.astype(x.dtype)
    s = sin.astype(x.dtype)
    x1 = x[..., :half]
    x2 = x[..., half:2 * half]
    return jnp.concatenate([x1 * c - x2 * s, x2 * c + x1 * s, x[..., 2 * half:]], axis=-1)


def masked_softmax(s, mask):
    p = jax.nn.softmax(jnp.where(mask, s.astype(jnp.float32), NEG), axis=-1)
    return p * mask


def to_blocks(a):
    b, t = a.shape[:2]
    return jnp.moveaxis(a.reshape((b, t // Q_BLOCK, Q_BLOCK) + a.shape[2:]), 1, 0)


def from_blocks(a):
    a = jnp.moveaxis(a, 0, 1)
    return a.reshape((a.shape[0], a.shape[1] * a.shape[2]) + a.shape[3:])


def conv_module(h, conv_w, conv_b, ln_g, ln_b):
    a, g = jnp.split(h, 2, axis=-1)
    z = a * jax.nn.sigmoid(g)
    z = lax.conv_general_dilated(z, conv_w[:, None, :], window_strides=(1,),
                                 padding=[(CONV_WIDTH - 1, 0)],
                                 dimension_numbers=('NWC', 'WIO', 'NWC'),
                                 feature_group_count=GROUP_WIDTH) + conv_b
    return jax.nn.silu(layer_norm(z, ln_g, ln_b))


def sgu_module(h, ln_g, ln_b, w_s, b_s):
    u, v = jnp.split(h, 2, axis=-1)
    v = layer_norm(v, ln_g, ln_b)
    b_, t = v.shape[:2]
    v = v.reshape(b_, t // SGU_CHUNK, SGU_CHUNK, GROUP_HEADS, HEAD_DIM)
    causal = jnp.tril(jnp.ones((SGU_CHUNK, SGU_CHUNK), dtype=bool))
    w = jnp.where(causal, w_s, 0.0)
    s = jnp.einsum('hij,bnjhd->bnihd', w, v) + b_s.T[None, None, :, :, None]
    return u * s.reshape(b_, t, GROUP_WIDTH)


def nsa_module(h, cos, sin, q_g, k_g, cmp_pos, cmp_w1, cmp_w2):
    b_, t = h.shape[:2]
    scale = HEAD_DIM ** -0.5
    q, kv, gates = jnp.split(h, NSA_SPLITS, axis=-1)
    q = rms_norm(q.reshape(b_, t, GROUP_HEADS, HEAD_DIM), q_g)
    k_cmp, v_cmp, k_slc, v_slc, k_win, v_win = jnp.split(kv, 2 * NSA_N_BRANCH, axis=-1)
    gates = jax.nn.sigmoid(gates.astype(jnp.float32)).astype(h.dtype)
    gates = gates.reshape(b_, t, GROUP_HEADS, NSA_N_BRANCH)
    t_pos = jnp.arange(t)

    n_cmp = (t - NSA_CMP_LEN) // NSA_CMP_STRIDE + 1
    cmp_start = jnp.arange(n_cmp) * NSA_CMP_STRIDE
    tok = cmp_start[:, None] + jnp.arange(NSA_CMP_LEN)[None, :]

    def compress(z, pos, w1, w2):
        blk = (z[:, tok] + pos).reshape(b_, n_cmp, NSA_CMP_LEN * HEAD_DIM)
        return jax.nn.gelu(blk @ w1) @ w2

    kc = rms_norm(compress(k_cmp, cmp_pos[0], cmp_w1[0], cmp_w2[0]), k_g[0])
    vc = compress(v_cmp, cmp_pos[1], cmp_w1[1], cmp_w2[1])
    cmp_mask = (cmp_start + NSA_CMP_LEN - 1)[None, :] <= t_pos[:, None]
    p_c = masked_softmax(jnp.einsum('bthd,bnd->bhtn', q, kc) * scale, cmp_mask)
    o_cmp = jnp.einsum('bhtn,bnd->bthd', p_c.astype(vc.dtype), vc)

    n_sel = t // NSA_SEL_LEN
    sel_start = jnp.arange(n_sel) * NSA_SEL_LEN
    overlap = ((cmp_start[:, None] < sel_start[None, :] + NSA_SEL_LEN)
               & (cmp_start[:, None] + NSA_CMP_LEN > sel_start[None, :])).astype(jnp.float32)
    imp = jnp.einsum('bhtn,nj->btj', p_c, overlap)
    cur = t_pos // NSA_SEL_LEN
    j = jnp.arange(n_sel)
    forced = (j[None] == 0) | (j[None] == cur[:, None]) | (j[None] == cur[:, None] - 1)
    future = j[None] > cur[:, None]
    imp = jnp.where(forced, jnp.inf, jnp.where(future, -jnp.inf, imp))
    n_top = min(NSA_SEL_TOP, n_sel)
    _, sel_idx = lax.top_k(imp, n_top)

    q_r = apply_rope(q, cos, sin)
    ks = apply_rope(rms_norm(k_slc, k_g[1]), cos, sin)
    kw = apply_rope(rms_norm(k_win, k_g[2]), cos, sin)
    ks_blk = ks.reshape(b_, n_sel, NSA_SEL_LEN, HEAD_DIM)
    vs_blk = v_slc.reshape(b_, n_sel, NSA_SEL_LEN, HEAD_DIM)
    kw_pad = jnp.pad(kw, ((0, 0), (NSA_WINDOW, 0), (0, 0)))
    vw_pad = jnp.pad(v_win, ((0, 0), (NSA_WINDOW, 0), (0, 0)))
    win_len = NSA_WINDOW + Q_BLOCK
    n_keys_sel = n_top * NSA_SEL_LEN

    def block_fn(args):
        qb, idx, bi = args
        tq = bi * Q_BLOCK + jnp.arange(Q_BLOCK)
        kg = jax.vmap(lambda kb, ib: kb[ib])(ks_blk, idx)
        vg = jax.vmap(lambda vb, ib: vb[ib])(vs_blk, idx)
        key_pos = idx[..., None] * NSA_SEL_LEN + jnp.arange(NSA_SEL_LEN)
        m = (key_pos <= tq[None, :, None, None]).reshape(b_, Q_BLOCK, 1, n_keys_sel)
        s = jnp.einsum('bqhd,bqnsd->bqhns', qb, kg).reshape(b_, Q_BLOCK, GROUP_HEADS, n_keys_sel) * scale
        p = masked_softmax(s, m)
        o_s = jnp.einsum('bqhk,bqkd->bqhd', p.astype(vg.dtype),
                         vg.reshape(b_, Q_BLOCK, n_keys_sel, HEAD_DIM))
        start = bi * Q_BLOCK
        kwb = lax.dynamic_slice_in_dim(kw_pad, start, win_len, axis=1)
        vwb = lax.dynamic_slice_in_dim(vw_pad, start, win_len, axis=1)
        kpos = start - NSA_WINDOW + jnp.arange(win_len)
        mw = ((kpos[None, :] <= tq[:, None]) & (kpos[None, :] > tq[:, None] - NSA_WINDOW)
              & (kpos[None, :] >= 0))
        pw = masked_softmax(jnp.einsum('bqhd,bkd->bqhk', qb, kwb) * scale, mw[None, :, None, :])
        o_w = jnp.einsum('bqhk,bkd->bqhd', pw.astype(vwb.dtype), vwb)
        return o_s, o_w

    o_s, o_w = lax.map(block_fn, (to_blocks(q_r), to_blocks(sel_idx), jnp.arange(t // Q_BLOCK)))
    o = (gates[..., 0:1] * o_cmp + gates[..., 1:2] * from_blocks(o_s)
         + gates[..., 2:3] * from_blocks(o_w))
    return o.reshape(b_, t, GROUP_WIDTH)


def dsa_module(h, cos, sin, cos_i, sin_i, q_g, k_g):
    b_, t = h.shape[:2]
    scale = HEAD_DIM ** -0.5
    q, k, v, iq, ik, iw = jnp.split(h, DSA_SPLITS, axis=-1)
    q = apply_rope(rms_norm(q.reshape(b_, t, GROUP_HEADS, HEAD_DIM), q_g), cos, sin)
    k = apply_rope(rms_norm(k, k_g), cos, sin)
    iq = apply_rope(iq.reshape(b_, t, DSA_IDX_HEADS, DSA_IDX_DIM), cos_i, sin_i)
    ik = apply_rope(ik, cos_i, sin_i)
    iw = iw * (DSA_IDX_HEADS ** -0.5)
    top = min(DSA_TOPK_MAX, t // 4)
    key_pos = jnp.arange(t)

    def block_fn(args):
        qb, iqb, iwb, bi = args
        tq = bi * Q_BLOCK + jnp.arange(Q_BLOCK)
        logits = jnp.einsum('bqhd,bsd->bqhs', iqb, ik).astype(jnp.float32) * (DSA_IDX_DIM ** -0.5)
        score = jnp.einsum('bqh,bqhs->bqs', iwb.astype(jnp.float32), jax.nn.relu(logits))
        score = jnp.where(key_pos[None, :] <= tq[:, None], score, -jnp.inf)
        _, idx = lax.top_k(score, top)
        kg = jax.vmap(lambda kk, ii: kk[ii])(k, idx)
        vg = jax.vmap(lambda vv, ii: vv[ii])(v, idx)
        m = (idx <= tq[None, :, None])[:, :, None, :]
        p = masked_softmax(jnp.einsum('bqhd,bqkd->bqhk', qb, kg) * scale, m)
        return jnp.einsum('bqhk,bqkd->bqhd', p.astype(vg.dtype), vg)

    o = lax.map(block_fn, (to_blocks(q), to_blocks(iq), to_blocks(iw), jnp.arange(t // Q_BLOCK)))
    return from_blocks(o).reshape(b_, t, GROUP_WIDTH)


def peer_ffn(x, wq, subkeys, u_tab, v_tab):
    b_, t, d = x.shape
    xb = x.reshape(b_ * t // Q_BLOCK, Q_BLOCK, d)

    def block_fn(xt):
        q = (xt @ wq).reshape(Q_BLOCK, PEER_HEADS, 2, PEER_DKEY // 2)
        s = jnp.einsum('thpd,hpnd->thpn', q, subkeys).astype(jnp.float32)
        sv, si = lax.top_k(s, PEER_TOPK)
        cand = (sv[:, :, 0, :, None] + sv[:, :, 1, None, :]).reshape(Q_BLOCK, PEER_HEADS, PEER_TOPK * PEER_TOPK)
        cv, ci = lax.top_k(cand, PEER_TOPK)
        i1 = jnp.take_along_axis(si[:, :, 0], ci // PEER_TOPK, axis=-1)
        i2 = jnp.take_along_axis(si[:, :, 1], ci % PEER_TOPK, axis=-1)
        e = i1 * PEER_NKEYS + i2
        g = jax.nn.softmax(cv, axis=-1).astype(xt.dtype)
        ug = u_tab[e]
        vg = v_tab[e]
        a = jax.nn.gelu(jnp.einsum('thkd,td->thk', ug, xt)) * g
        return jnp.einsum('thk,thkd->td', a, vg)

    return lax.map(block_fn, xb).reshape(b_, t, d)


def setup_inputs(seed: int = 0) -> dict:
    key = jax.random.key(seed)
    ks = jax.random.split(key, 32)
    f32 = jnp.float32
    L = DEPTH

    def nrm(k, shape, scale):
        return jax.random.normal(k, shape, f32) * scale

    def gain(k, shape):
        return 1.0 + 0.05 * jax.random.normal(k, shape, f32)

    return {
        "x": nrm(ks[0], (BATCH, SEQ, D_MODEL), 1.0),
        "c": nrm(ks[1], (BATCH, D_MODEL), 1.0),
        "positions": jnp.broadcast_to(jnp.arange(SEQ, dtype=jnp.int32), (BATCH, SEQ)),
        "ada_w": nrm(ks[2], (L, D_MODEL, 6 * D_MODEL), 0.5 * D_MODEL ** -0.5),
        "ada_b": nrm(ks[3], (L, 6 * D_MODEL), 0.02),
        "norm1_g": gain(ks[4], (L, D_MODEL)),
        "norm2_g": gain(ks[5], (L, D_MODEL)),
        "w_in": nrm(ks[6], (L, D_MODEL, IN_COLS), D_MODEL ** -0.5),
        "w_out": nrm(ks[7], (L, MIX_WIDTH, D_MODEL), MIX_WIDTH ** -0.5),
        "conv_w": nrm(ks[8], (L, CONV_WIDTH, GROUP_WIDTH), CONV_WIDTH ** -0.5),
        "conv_b": nrm(ks[9], (L, GROUP_WIDTH), 0.02),
        "conv_ln_g": gain(ks[10], (L, GROUP_WIDTH)),
        "conv_ln_b": nrm(ks[11], (L, GROUP_WIDTH), 0.02),
        "sgu_ln_g": gain(ks[12], (L, GROUP_WIDTH)),
        "sgu_ln_b": nrm(ks[13], (L, GROUP_WIDTH), 0.02),
        "sgu_w": nrm(ks[14], (L, GROUP_HEADS, SGU_CHUNK, SGU_CHUNK), SGU_CHUNK ** -0.5),
        "sgu_b": gain(ks[15], (L, GROUP_HEADS, SGU_CHUNK)),
        "nsa_q_g": gain(ks[16], (L, HEAD_DIM)),
        "nsa_k_g": gain(ks[17], (L, NSA_N_BRANCH, HEAD_DIM)),
        "nsa_cmp_pos": nrm(ks[18], (L, 2, NSA_CMP_LEN, HEAD_DIM), 0.1),
        "nsa_cmp_w1": nrm(ks[19], (L, 2, NSA_CMP_LEN * HEAD_DIM, HEAD_DIM), (NSA_CMP_LEN * HEAD_DIM) ** -0.5),
        "nsa_cmp_w2": nrm(ks[20], (L, 2, HEAD_DIM, HEAD_DIM), HEAD_DIM ** -0.5),
        "dsa_q_g": gain(ks[21], (L, HEAD_DIM)),
        "dsa_k_g": gain(ks[22], (L, HEAD_DIM)),
        "peer_wq": nrm(ks[23], (L, D_MODEL, PEER_HEADS * PEER_DKEY), D_MODEL ** -0.5),
        "peer_subkeys": nrm(ks[24], (L, PEER_HEADS, 2, PEER_NKEYS, PEER_DKEY // 2), (PEER_DKEY // 2) ** -0.5),
        "peer_u": nrm(ks[25], (L, PEER_EXPERTS, D_MODEL), D_MODEL ** -0.5),
        "peer_v": nrm(ks[26], (L, PEER_EXPERTS, D_MODEL), 0.5),
    }


def reference(x, c, positions, ada_w, ada_b, norm1_g, norm2_g, w_in, w_out,
              conv_w, conv_b, conv_ln_g, conv_ln_b, sgu_ln_g, sgu_ln_b, sgu_w, sgu_b,
              nsa_q_g, nsa_k_g, nsa_cmp_pos, nsa_cmp_w1, nsa_cmp_w2, dsa_q_g, dsa_k_g,
              peer_wq, peer_subkeys, peer_u, peer_v):
    cos, sin = rope_tables(positions, HEAD_DIM // ROT_FRAC)
    cos_i, sin_i = rope_tables(positions, DSA_IDX_DIM // ROT_FRAC)
    c_act = jax.nn.silu(c)
    for i in range(DEPTH):
        ada = c_act @ ada_w[i] + ada_b[i]
        sh1, sc1, g1, sh2, sc2, g2 = [a[:, None, :] for a in jnp.split(ada, 6, axis=-1)]
        hn = rms_norm(x, norm1_g[i]) * (1.0 + sc1) + sh1
        proj = hn @ w_in[i]
        pa, pb, pc, pd = jnp.split(proj, IN_SPLITS, axis=-1)
        ya = conv_module(pa, conv_w[i], conv_b[i], conv_ln_g[i], conv_ln_b[i])
        yb = sgu_module(pb, sgu_ln_g[i], sgu_ln_b[i], sgu_w[i], sgu_b[i])
        yc = nsa_module(pc, cos, sin, nsa_q_g[i], nsa_k_g[i], nsa_cmp_pos[i], nsa_cmp_w1[i], nsa_cmp_w2[i])
        yd = dsa_module(pd, cos, sin, cos_i, sin_i, dsa_q_g[i], dsa_k_g[i])
        y = jnp.concatenate([ya, yb, yc, yd], axis=-1) @ w_out[i]
        x = x + g1 * y
        hn = rms_norm(x, norm2_g[i]) * (1.0 + sc2) + sh2
        x = x + g2 * peer_ffn(hn, peer_wq[i], peer_subkeys[i], peer_u[i], peer_v[i])
    return x
```

```python
import contextlib
import os
import numpy as np
import concourse.bass as bass
import concourse.mybir as mybir
from concourse.bass_utils import run_bass_kernel_spmd

F32 = mybir.dt.float32
BF16 = mybir.dt.bfloat16
ALU = mybir.AluOpType
AF = mybir.ActivationFunctionType
AX = mybir.AxisListType
NCORES = 8
EPS = 1e-6


class Buf:
    __slots__ = ("name", "w", "r", "dsem", "dcnt")

    def __init__(self, name):
        self.name = name
        self.w = None
        self.r = []
        self.dsem = None
        self.dcnt = 0


class Sched:
    ENG = ("pe", "act", "dve", "pool", "sp")

    def __init__(self, nc, es):
        self.nc = nc
        self.es = es
        self.sem = {e: es.enter_context(nc.semaphore("sem_" + e)) for e in self.ENG}
        self.cnt = {e: 0 for e in self.ENG}
        self.waited = {e: {} for e in self.ENG}
        self.prog = {e: [] for e in self.ENG}
        self.bufs = []
        self.nsem = 0

    def buf(self, name):
        b = Buf(name)
        self.bufs.append(b)
        return b

    def sb(self, name, shape, dtype):
        t = self.es.enter_context(self.nc.sbuf_tensor("sb_" + name, list(shape), dtype))
        return t, self.buf(name)

    def ps(self, name, shape, dtype=F32):
        t = self.es.enter_context(self.nc.psum_tensor("ps_" + name, list(shape), dtype))
        return t, self.buf(name)

    def _dsem(self, b):
        if b.dsem is None:
            b.dsem = self.es.enter_context(self.nc.semaphore("d_%d_%s" % (self.nsem, b.name)))
            self.nsem += 1
        return b.dsem

    def _waits(self, eng, reads, writes, skip_self_w=False):
        need = {}
        for b in reads:
            if b.w is not None:
                need[b.w[0]] = max(need.get(b.w[0], 0), b.w[1])
        for b in writes:
            if b.w is not None and not (skip_self_w and b.w[0] is self.sem[eng]):
                need[b.w[0]] = max(need.get(b.w[0], 0), b.w[1])
            for tok in b.r:
                need[tok[0]] = max(need.get(tok[0], 0), tok[1])
        out = []
        wd = self.waited[eng]
        for s, v in need.items():
            if wd.get(s, 0) < v:
                wd[s] = v
                out.append((s, v))
        return out

    def op(self, eng, fn, reads=(), writes=(), accumulate=False):
        waits = self._waits(eng, reads, writes, skip_self_w=accumulate)
        self.cnt[eng] += 1
        tok = (self.sem[eng], self.cnt[eng])
        self.prog[eng].append((waits, fn, (self.sem[eng], 1)))
        for b in reads:
            b.r.append(tok)
        for b in writes:
            b.w = tok
            b.r = []
        return tok

    def dma(self, eng, out_ap, in_ap, src, dst, sem_on="dst", **kw):
        reads = [src] if src is not None else []
        writes = [dst] if dst is not None else []
        waits = self._waits(eng, reads, writes)
        sb = dst if sem_on == "dst" else src
        sem = self._dsem(sb)
        sb.dcnt += 16
        tok = (sem, sb.dcnt)

        def fn(e, out_ap=out_ap, in_ap=in_ap, kw=kw):
            return e.dma_start(out=out_ap, in_=in_ap, **kw)

        self.prog[eng].append((waits, fn, (sem, 16)))
        if src is not None:
            src.r.append(tok)
        if dst is not None:
            dst.w = tok
            dst.r = []
        return tok

    def finish(self, final_tokens):
        nc = self.nc
        need = {}
        for s, v in final_tokens:
            if s is None:
                continue
            need[s] = max(need.get(s, 0), v)
        fin = list(need.items())
        with nc.Block() as block:
            def mk(ename):
                def body(e):
                    for waits, fn, inc in self.prog[ename]:
                        for s, v in waits:
                            e.wait_ge(s, v)
                        ins = fn(e)
                        ins.then_inc(inc[0], inc[1])
                    if ename == "sp":
                        for s, v in fin:
                            e.wait_ge(s, v)
                return body
            block.tensor(mk("pe"))
            block.scalar(mk("act"))
            block.vector(mk("dve"))
            block.gpsimd(mk("pool"))
            block.sync(mk("sp"))


def _run(nc, in_maps):
    res = run_bass_kernel_spmd(nc, in_maps, core_ids=list(range(NCORES)))
    return res.results


def rep128(v):
    v = np.asarray(v, dtype=np.float32).reshape(1, -1)
    return np.ascontiguousarray(np.broadcast_to(v, (128, v.shape[1])))


IDENT = np.eye(128, dtype=np.float32)


_GEMM_CACHE = {}


def build_gemm(T, K, N, prologue, epilogue, silu=False):
    nc = bass.Bass("TRN2", target_bir_lowering=False)
    NT = T // 128
    KC = K // 128
    CG = 512
    ncg = (N + CG - 1) // CG
    X = nc.dram_tensor("X", [T, K], F32, kind="ExternalInput").ap()
    W = nc.dram_tensor("W", [K, N], F32, kind="ExternalInput").ap()
    ID = nc.dram_tensor("ident", [128, 128], F32, kind="ExternalInput").ap()
    if prologue:
        G = nc.dram_tensor("pg", [128, K], F32, kind="ExternalInput").ap()
        SC = nc.dram_tensor("psc", [128, K], F32, kind="ExternalInput").ap()
        SH = nc.dram_tensor("psh", [128, K], F32, kind="ExternalInput").ap()
    if epilogue:
        R = nc.dram_tensor("resid", [T, N], F32, kind="ExternalInput").ap()
        GT = nc.dram_tensor("gate", [128, N], F32, kind="ExternalInput").ap()
    Y = nc.dram_tensor("Y", [T, N], F32, kind="ExternalOutput").ap()
    Wv = W.rearrange("(kc p) n -> p kc n", p=128)

    with contextlib.ExitStack() as es:
        S = Sched(nc, es)
        idf, b_idf = S.sb("idf", [128, 128], F32)
        idb, b_idb = S.sb("idb", [128, 128], BF16)
        S.dma("sp", idf[:], ID, None, b_idf)
        S.op("dve", lambda e: e.tensor_copy(out=idb[:], in_=idf[:]), [b_idf], [b_idb])
        if prologue:
            A, b_A = S.sb("A", [128, K], F32)
            B, b_B = S.sb("B", [128, K], F32)
            gt_, b_gt = S.sb("gtmp", [128, K], F32)
            S.dma("sp", A[:], SC, None, b_A)
            S.dma("sp", gt_[:], G, None, b_gt)
            S.dma("sp", B[:], SH, None, b_B)
            S.op("dve", lambda e: e.scalar_tensor_tensor(out=A[:], in0=A[:], scalar=1.0, in1=gt_[:],
                                                         op0=ALU.add, op1=ALU.mult), [b_A, b_gt], [b_A])
        if epilogue:
            gate, b_gate = S.sb("gate", [128, N], F32)
            S.dma("sp", gate[:], GT, None, b_gate)
        hnT, b_hnT = [], []
        for i in range(NT):
            t, b = S.sb("hnT%d" % i, [128, KC, 128], BF16)
            hnT.append(t)
            b_hnT.append(b)
        xs = [S.sb("x%d" % j, [128, K], F32) for j in range(2)]
        hb = [S.sb("hb%d" % j, [128, K], BF16) for j in range(2)]
        tmp, b_tmp = S.sb("tmp", [128, K], F32)
        st = [S.sb("st%d" % j, [128, 4], F32) for j in range(2)]
        pT = [S.ps("pT%d" % j, [128, 512], BF16) for j in range(2)]
        for i in range(NT):
            x, b_x = xs[i % 2]
            h, b_h = hb[i % 2]
            s_, b_s = st[i % 2]
            S.dma("sp", x[:], X[i * 128:(i + 1) * 128, :], None, b_x)
            if prologue:
                S.op("act", lambda e, x=x, s_=s_: e.activation(out=tmp[:], in_=x[:], func=AF.Square,
                                                               accum_out=s_[:, 0:1]), [b_x], [b_tmp, b_s])
                S.op("dve", lambda e, s_=s_: e.tensor_scalar(out=s_[:, 1:2], in0=s_[:, 0:1], scalar1=1.0 / K,
                                                             scalar2=EPS, op0=ALU.mult, op1=ALU.add), [b_s], [b_s])
                S.op("dve", lambda e, s_=s_: e.reciprocal(out=s_[:, 3:4], in_=s_[:, 1:2]), [b_s], [b_s])
                S.op("act", lambda e, s_=s_: e.sqrt(out=s_[:, 2:3], in_=s_[:, 3:4]), [b_s], [b_s])
                S.op("dve", lambda e, x=x, s_=s_: e.scalar_tensor_tensor(out=tmp[:], in0=x[:], scalar=s_[:, 2:3],
                                                                         in1=A[:], op0=ALU.mult, op1=ALU.mult),
                     [b_x, b_s, b_A], [b_tmp])
                S.op("dve", lambda e, h=h: e.tensor_tensor(out=h[:], in0=tmp[:], in1=B[:], op=ALU.add),
                     [b_tmp, b_B], [b_h])
            elif silu:
                S.op("act", lambda e, x=x: e.activation(out=tmp[:], in_=x[:], func=AF.Sigmoid), [b_x], [b_tmp])
                S.op("dve", lambda e, x=x, h=h: e.tensor_tensor(out=h[:], in0=x[:], in1=tmp[:], op=ALU.mult),
                     [b_x, b_tmp], [b_h])
            else:
                S.op("dve", lambda e, x=x, h=h: e.tensor_copy(out=h[:], in_=x[:]), [b_x], [b_h])
            for q in range(KC // 4):
                p, b_p = pT[q % 2]
                for j in range(4):
                    kc = q * 4 + j
                    S.op("pe", lambda e, p=p, h=h, kc=kc, j=j: e.transpose(
                        out=p[:, j * 128:(j + 1) * 128], in_=h[:, kc * 128:(kc + 1) * 128], identity=idb[:]),
                        [b_h, b_idb], [b_p])
                eng = "act" if q % 2 == 0 else "dve"
                if eng == "act":
                    S.op("act", lambda e, p=p, i=i, q=q: e.copy(
                        out=hnT[i][:, q * 4:(q + 1) * 4, :], in_=p[:].rearrange("p (a b) -> p a b", a=4)),
                        [b_p], [b_hnT[i]])
                else:
                    S.op("dve", lambda e, p=p, i=i, q=q: e.tensor_copy(
                        out=hnT[i][:, q * 4:(q + 1) * 4, :], in_=p[:].rearrange("p (a b) -> p a b", a=4)),
                        [b_p], [b_hnT[i]])
        wf = [S.sb("wf%d" % j, [128, KC, CG], F32) for j in range(2)]
        wb = [S.sb("wb%d" % j, [128, KC, CG], BF16) for j in range(2)]
        po = [S.ps("po%d" % j, [128, CG], F32) for j in range(2)]
        ot = [S.sb("ot%d" % j, [128, CG], F32) for j in range(3)]
        if epilogue:
            rt = [S.sb("rt%d" % j, [128, CG], F32) for j in range(2)]
        n_out = 0
        for cg in range(ncg):
            c0 = cg * CG
            cw = min(CG, N - c0)
            w_f, b_wf = wf[cg % 2]
            w_b, b_wb = wb[cg % 2]
            half = KC // 2
            S.dma("sp", w_f[:, 0:half, 0:cw], Wv[:, 0:half, c0:c0 + cw], None, b_wf)
            S.dma("act", w_f[:, half:KC, 0:cw], Wv[:, half:KC, c0:c0 + cw], None, b_wf)
            S.op("pool", lambda e, w_f=w_f, w_b=w_b, cw=cw: e.tensor_copy(out=w_b[:, 0:half, 0:cw],
                                                                         in_=w_f[:, 0:half, 0:cw]),
                 [b_wf], [b_wb])
            S.op("dve", lambda e, w_f=w_f, w_b=w_b, cw=cw: e.tensor_copy(out=w_b[:, half:KC, 0:cw],
                                                                        in_=w_f[:, half:KC, 0:cw]),
                 [b_wf], [b_wb])
            for i in range(NT):
                p, b_p = po[n_out % 2]
                o, b_o = ot[n_out % 3]
                if epilogue:
                    r, b_r = rt[n_out % 2]
                    S.dma("sp", r[:, 0:cw], R[i * 128:(i + 1) * 128, c0:c0 + cw], None, b_r)
                for kc in range(KC):
                    S.op("pe", lambda e, p=p, i=i, kc=kc, w_b=w_b, cw=cw: e.matmul(
                        p[:, 0:cw], hnT[i][:, kc, :], w_b[:, kc, 0:cw], start=(kc == 0), stop=(kc == KC - 1)),
                        [b_hnT[i], b_wb], [b_p], accumulate=(kc > 0))
                if epilogue:
                    S.op("dve", lambda e, o=o, p=p, c0=c0, cw=cw: e.tensor_tensor(
                        out=o[:, 0:cw], in0=p[:, 0:cw], in1=gate[:, c0:c0 + cw], op=ALU.mult),
                        [b_p, b_gate], [b_o])
                    S.op("pool", lambda e, o=o, r=r, cw=cw: e.tensor_tensor(
                        out=o[:, 0:cw], in0=o[:, 0:cw], in1=r[:, 0:cw], op=ALU.add), [b_o, b_r], [b_o])
                else:
                    if n_out % 2 == 0:
                        S.op("act", lambda e, o=o, p=p, cw=cw: e.copy(out=o[:, 0:cw], in_=p[:, 0:cw]),
                             [b_p], [b_o])
                    else:
                        S.op("dve", lambda e, o=o, p=p, cw=cw: e.tensor_copy(out=o[:, 0:cw], in_=p[:, 0:cw]),
                             [b_p], [b_o])
                S.dma("pool", Y[i * 128:(i + 1) * 128, c0:c0 + cw], o[:, 0:cw], b_o, None, sem_on="src")
                n_out += 1
        fin = [(b.dsem, b.dcnt) for _, b in ot]
        S.finish(fin)
    return nc


def gemm(X, W, pro=None, epi=None):
    Ttot, K = X.shape
    N = W.shape[1]
    T = Ttot // NCORES
    key = (T, K, N, pro is not None, epi is not None)
    if key not in _GEMM_CACHE:
        _GEMM_CACHE[key] = build_gemm(T, K, N, pro is not None, epi is not None)
    nc = _GEMM_CACHE[key]
    maps = []
    for c in range(NCORES):
        m = {"X": np.ascontiguousarray(X[c * T:(c + 1) * T]), "W": W, "ident": IDENT}
        if pro is not None:
            m["pg"], m["psc"], m["psh"] = pro
        if epi is not None:
            m["resid"] = np.ascontiguousarray(epi[0][c * T:(c + 1) * T])
            m["gate"] = epi[1]
        maps.append(m)
    res = _run(nc, maps)
    return np.concatenate([r["Y"] for r in res], axis=0)


def TT(S, eng, out, in0, in1, op, R, W):
    return S.op(eng, lambda e: e.tensor_tensor(out=out, in0=in0, in1=in1, op=op), R, W)


def TS(S, eng, out, in0, s1, s2, op0, op1=None, R=(), W=(), accum=None):
    if op1 is None:
        return S.op(eng, lambda e: e.tensor_scalar(out=out, in0=in0, scalar1=s1, scalar2=None, op0=op0), R, W)
    if accum is None:
        return S.op(eng, lambda e: e.tensor_scalar(out=out, in0=in0, scalar1=s1, scalar2=s2, op0=op0, op1=op1), R, W)
    return S.op(eng, lambda e: e.tensor_scalar(out=out, in0=in0, scalar1=s1, scalar2=s2, op0=op0, op1=op1,
                                               accum_out=accum), R, W)


def STT(S, eng, out, in0, scalar, in1, op0, op1, R, W):
    return S.op(eng, lambda e: e.scalar_tensor_tensor(out=out, in0=in0, scalar=scalar, in1=in1, op0=op0, op1=op1),
                R, W)


def ACT(S, out, in_, func, R, W, bias=None, scale=1.0, accum=None):
    kw = {}
    if bias is not None:
        kw["bias"] = bias
    if accum is not None:
        kw["accum_out"] = accum
    return S.op("act", lambda e: e.activation(out=out, in_=in_, func=func, scale=scale, **kw), R, W)


def CP(S, eng, out, in_, R, W):
    if eng == "act":
        return S.op("act", lambda e: e.copy(out=out, in_=in_), R, W)
    return S.op(eng, lambda e: e.tensor_copy(out=out, in_=in_), R, W)


def MM(S, out, lhsT, rhs, start, stop, R, W):
    return S.op("pe", lambda e: e.matmul(out, lhsT, rhs, start=start, stop=stop), R, W, accumulate=not start)


def TR(S, out, in_, ident, R, W, acc=False):
    return S.op("pe", lambda e: e.transpose(out=out, in_=in_, identity=ident), R, W, accumulate=acc)


def RED(S, eng, out, in_, op, R, W):
    return S.op(eng, lambda e: e.tensor_reduce(out=out, in_=in_, axis=AX.X, op=op), R, W)


def RSTD(S, out, tmp, ms, R, W):
    S.op("dve", lambda e: e.reciprocal(out=tmp, in_=ms), R, W)
    S.op("act", lambda e: e.sqrt(out=out, in_=tmp), W, W)


def load_const(S, name, dram_ap, shape, dtype=F32, q="sp", cast=None):
    t, b = S.sb(name, shape, dtype)
    S.dma(q, t[:], dram_ap, None, b)
    if cast is not None:
        t2, b2 = S.sb(name + "_c", shape, cast)
        CP(S, "dve", t2[:], t[:], [b], [b2])
        return t2, b2
    return t, b


def layer_norm_rows(S, pfx, dst, src, src_bufs, dst_bufs, D, g_t, b_t, g_b, b_b, st, b_st, junk, b_junk, tmp, b_tmp):
    ACT(S, tmp, src, AF.Identity, src_bufs + [b_st], [b_tmp, b_st], accum=st[:, 0:1])
    ACT(S, junk, src, AF.Square, src_bufs + [b_st], [b_junk, b_st], accum=st[:, 1:2])
    TS(S, "dve", st[:, 2:3], st[:, 0:1], 1.0 / D, None, ALU.mult, R=[b_st], W=[b_st])
    TT(S, "dve", st[:, 3:4], st[:, 2:3], st[:, 2:3], ALU.mult, [b_st], [b_st])
    STT(S, "dve", st[:, 4:5], st[:, 1:2], 1.0 / D, st[:, 3:4], ALU.mult, ALU.subtract, [b_st], [b_st])
    TS(S, "dve", st[:, 4:5], st[:, 4:5], EPS, None, ALU.add, R=[b_st], W=[b_st])
    RSTD(S, st[:, 5:6], st[:, 6:7], st[:, 4:5], [b_st], [b_st])
    TS(S, "dve", tmp, tmp, st[:, 2:3], st[:, 5:6], ALU.subtract, ALU.mult, R=[b_tmp, b_st], W=[b_tmp])
    TT(S, "dve", tmp, tmp, g_t, ALU.mult, [b_tmp, g_b], [b_tmp])
    TT(S, "dve", dst, tmp, b_t, ALU.add, [b_tmp, b_b], dst_bufs)


def build_conv(T):
    nc = bass.Bass("TRN2", target_bir_lowering=False)
    NT = T // 128
    TH = T + 32
    AT = nc.dram_tensor("aT", [4, 128, TH], F32, kind="ExternalInput").ap()
    GT = nc.dram_tensor("gT", [4, 128, TH], F32, kind="ExternalInput").ap()
    CW = nc.dram_tensor("cw", [4, 128, 32], F32, kind="ExternalInput").ap()
    LG = nc.dram_tensor("lng", [128, 512], F32, kind="ExternalInput").ap()
    LB = nc.dram_tensor("lnb", [128, 512], F32, kind="ExternalInput").ap()
    ID = nc.dram_tensor("ident", [128, 128], F32, kind="ExternalInput").ap()
    Y = nc.dram_tensor("Y", [T, 512], F32, kind="ExternalOutput").ap()
    with contextlib.ExitStack() as es:
        S = Sched(nc, es)
        idf, b_idf = load_const(S, "idf", ID, [128, 128])
        lg, b_lg = load_const(S, "lg", LG, [128, 512])
        lb, b_lb = load_const(S, "lb", LB, [128, 512])
        pz = [S.ps("pz%d" % i, [128, 512], F32) for i in range(NT)]
        for cc in range(4):
            a, b_a = S.sb("a%d" % cc, [128, TH], F32)
            g, b_g = S.sb("g%d" % cc, [128, TH], F32)
            w, b_w = S.sb("w%d" % cc, [128, 32], F32)
            S.dma("sp", a[:], AT[cc], None, b_a)
            S.dma("act", g[:], GT[cc], None, b_g)
            S.dma("sp", w[:], CW[cc], None, b_w)
            ACT(S, g[:], g[:], AF.Sigmoid, [b_g], [b_g])
            TT(S, "dve", a[:], a[:], g[:], ALU.mult, [b_a, b_g], [b_a])
            accA, b_accA = S.sb("accA%d" % cc, [128, T], F32)
            TS(S, "dve", accA[:], a[:, 2:2 + T], w[:, 0:1], w[:, 31:32], ALU.mult, ALU.add, R=[b_a, b_w], W=[b_accA])
            for k in range(1, 31):
                STT(S, "dve", accA[:], a[:, 2 + k:2 + k + T], w[:, k:k + 1], accA[:], ALU.mult, ALU.add,
                    [b_a, b_w, b_accA], [b_accA])
            for i in range(NT):
                TR(S, pz[i][0][:, cc * 128:(cc + 1) * 128], accA[:, i * 128:(i + 1) * 128], idf[:],
                   [b_accA, b_idf], [pz[i][1]], acc=(cc > 0))
        outs = [S.sb("o%d" % j, [128, 512], F32) for j in range(2)]
        tmps = [S.sb("t%d" % j, [128, 512], F32) for j in range(2)]
        junk, b_junk = S.sb("junk", [128, 512], F32)
        sts = [S.sb("st%d" % j, [128, 8], F32) for j in range(2)]
        for i in range(NT):
            o, b_o = outs[i % 2]
            t, b_t = tmps[i % 2]
            st, b_st = sts[i % 2]
            layer_norm_rows(S, "c", t[:], pz[i][0][:], [pz[i][1]], [b_t], 512, lg[:], lb[:], b_lg, b_lb,
                            st, b_st, junk[:], b_junk, t[:], b_t)
            ACT(S, junk[:], t[:], AF.Sigmoid, [b_t], [b_junk])
            TT(S, "dve", o[:], t[:], junk[:], ALU.mult, [b_t, b_junk], [b_o])
            S.dma("pool", Y[i * 128:(i + 1) * 128, :], o[:], b_o, None, sem_on="src")
        S.finish([(b.dsem, b.dcnt) for _, b in outs])
    return nc


def build_sgu(T):
    nc = bass.Bass("TRN2", target_bir_lowering=False)
    NT = T // 128
    U = nc.dram_tensor("u", [T, 512], F32, kind="ExternalInput").ap()
    V = nc.dram_tensor("v", [T, 512], F32, kind="ExternalInput").ap()
    WT = nc.dram_tensor("wT", [128, 4, 128], F32, kind="ExternalInput").ap()
    MK = nc.dram_tensor("mask", [128, 4, 128], F32, kind="ExternalInput").ap()
    BS = nc.dram_tensor("bs", [128, 4], F32, kind="ExternalInput").ap()
    LG = nc.dram_tensor("lng", [128, 512], F32, kind="ExternalInput").ap()
    LB = nc.dram_tensor("lnb", [128, 512], F32, kind="ExternalInput").ap()
    Y = nc.dram_tensor("Y", [T, 512], F32, kind="ExternalOutput").ap()
    with contextlib.ExitStack() as es:
        S = Sched(nc, es)
        lg, b_lg = load_const(S, "lg", LG, [128, 512])
        lb, b_lb = load_const(S, "lb", LB, [128, 512])
        wt, b_wt = load_const(S, "wt", WT, [128, 4, 128])
        mk, b_mk = load_const(S, "mk", MK, [128, 4, 128])
        bs, b_bs = load_const(S, "bs", BS, [128, 4])
        wm, b_wm = S.sb("wm", [128, 4, 128], BF16)
        TT(S, "dve", wm[:], wt[:], mk[:], ALU.mult, [b_wt, b_mk], [b_wm])
        us = [S.sb("u%d" % j, [128, 512], F32) for j in range(2)]
        vs = [S.sb("v%d" % j, [128, 512], F32) for j in range(2)]
        vn = [S.sb("vn%d" % j, [128, 512], BF16) for j in range(2)]
        tmps = [S.sb("t%d" % j, [128, 512], F32) for j in range(2)]
        outs = [S.sb("o%d" % j, [128, 512], F32) for j in range(2)]
        junk, b_junk = S.sb("junk", [128, 512], F32)
        sts = [S.sb("st%d" % j, [128, 8], F32) for j in range(2)]
        pss = [S.ps("ps%d" % j, [128, 512], F32) for j in range(2)]
        for i in range(NT):
            u, b_u = us[i % 2]
            v, b_v = vs[i % 2]
            n, b_n = vn[i % 2]
            t, b_t = tmps[i % 2]
            o, b_o = outs[i % 2]
            st, b_st = sts[i % 2]
            ps, b_ps = pss[i % 2]
            S.dma("sp", u[:], U[i * 128:(i + 1) * 128, :], None, b_u)
            S.dma("act", v[:], V[i * 128:(i + 1) * 128, :], None, b_v)
            layer_norm_rows(S, "s", n[:], v[:], [b_v], [b_n], 512, lg[:], lb[:], b_lg, b_lb,
                            st, b_st, junk[:], b_junk, t[:], b_t)
            for h in range(4):
                MM(S, ps[:, h * 128:(h + 1) * 128], wm[:, h, :], n[:, h * 128:(h + 1) * 128], True, True,
                   [b_wm, b_n], [b_ps])
            for h in range(4):
                STT(S, "dve", o[:, h * 128:(h + 1) * 128], ps[:, h * 128:(h + 1) * 128], bs[:, h:h + 1],
                    u[:, h * 128:(h + 1) * 128], ALU.add, ALU.mult, [b_ps, b_bs, b_u], [b_o])
            S.dma("pool", Y[i * 128:(i + 1) * 128, :], o[:], b_o, None, sem_on="src")
        S.finish([(b.dsem, b.dcnt) for _, b in outs])
    return nc


_CACHE = {}


def _get(key, fn):
    if key not in _CACHE:
        _CACHE[key] = fn()
    return _CACHE[key]


def run_conv(pa, conv_w, conv_b, ln_g, ln_b):
    Ttot = pa.shape[0]
    T = Ttot // NCORES
    nc = _get(("conv", T), lambda: build_conv(T))
    aT = np.zeros((512, Ttot + 32), np.float32)
    gT = np.zeros((512, Ttot + 32), np.float32)
    aT[:, 32:] = pa[:, 0:512].T
    gT[:, 32:] = pa[:, 512:1024].T
    cw = np.zeros((512, 32), np.float32)
    cw[:, 0:31] = conv_w.T
    cw[:, 31] = conv_b
    cw = cw.reshape(4, 128, 32)
    lg, lb = rep128(ln_g), rep128(ln_b)
    maps = []
    for c in range(NCORES):
        maps.append({"aT": np.ascontiguousarray(aT[:, c * T:c * T + T + 32]).reshape(4, 128, T + 32),
                     "gT": np.ascontiguousarray(gT[:, c * T:c * T + T + 32]).reshape(4, 128, T + 32),
                     "cw": cw, "lng": lg, "lnb": lb, "ident": IDENT})
    res = _run(nc, maps)
    return np.concatenate([r["Y"] for r in res], axis=0)


def run_sgu(pb, ln_g, ln_b, sgu_w, sgu_b):
    Ttot = pb.shape[0]
    T = Ttot // NCORES
    nc = _get(("sgu", T), lambda: build_sgu(T))
    wT = np.ascontiguousarray(np.transpose(sgu_w, (2, 0, 1)))
    jj = np.arange(128)
    mask = np.ascontiguousarray(np.broadcast_to((jj[:, None] <= jj[None, :]).astype(np.float32)[:, None, :],
                                                (128, 4, 128)))
    bs = np.ascontiguousarray(sgu_b.T)
    lg, lb = rep128(ln_g), rep128(ln_b)
    maps = []
    for c in range(NCORES):
        maps.append({"u": np.ascontiguousarray(pb[c * T:(c + 1) * T, 0:512]),
                     "v": np.ascontiguousarray(pb[c * T:(c + 1) * T, 512:1024]),
                     "wT": wT, "mask": mask, "bs": bs, "lng": lg, "lnb": lb})
    res = _run(nc, maps)
    return np.concatenate([r["Y"] for r in res], axis=0)


PREP_IN = 2644
PREP_OUT = 2516
I32 = mybir.dt.int32
TWO_PI = float(2.0 * np.pi)


def _rope_inplace(S, Y, b_Y, H, half, c, s, b_sn, tmp, b_tmp):
    Y1 = Y[:, :, 0:half]
    Y2 = Y[:, :, half:2 * half]
    cb = c.unsqueeze(1).to_broadcast([128, H, half])
    sb_ = s.unsqueeze(1).to_broadcast([128, H, half])
    t = [tmp[:, k, 0:H * half].rearrange("p (h d) -> p h d", h=H) for k in range(4)]
    TT(S, "dve", t[0], Y1, cb, ALU.mult, [b_Y, b_sn], [b_tmp])
    TT(S, "dve", t[1], Y2, sb_, ALU.mult, [b_Y, b_sn], [b_tmp])
    TT(S, "dve", t[2], Y2, cb, ALU.mult, [b_Y, b_sn], [b_tmp])
    TT(S, "dve", t[3], Y1, sb_, ALU.mult, [b_Y, b_sn], [b_tmp])
    TT(S, "dve", Y1, t[0], t[1], ALU.subtract, [b_tmp], [b_Y])
    TT(S, "dve", Y2, t[2], t[3], ALU.add, [b_tmp], [b_Y])


def build_prep(T):
    nc = bass.Bass("TRN2", target_bir_lowering=False)
    NT = T // 128
    X = nc.dram_tensor("X", [T, PREP_IN], F32, kind="ExternalInput").ap()
    POS = nc.dram_tensor("pos", [T, 1], I32, kind="ExternalInput").ap()
    GN = nc.dram_tensor("gn", [128, 11, 128], F32, kind="ExternalInput").ap()
    INV = nc.dram_tensor("inv", [128, 48], F32, kind="ExternalInput").ap()
    PH = nc.dram_tensor("ph", [128, 48], F32, kind="ExternalInput").ap()
    Y = nc.dram_tensor("Y", [T, PREP_OUT], F32, kind="ExternalOutput").ap()
    with contextlib.ExitStack() as es:
        S = Sched(nc, es)
        gn, b_gn = load_const(S, "gn", GN, [128, 11, 128])
        inv, b_inv = load_const(S, "inv", INV, [128, 48])
        ph, b_ph = load_const(S, "ph", PH, [128, 48])
        xs = [S.sb("x%d" % j, [128, PREP_IN], F32) for j in range(2)]
        os_ = [S.sb("o%d" % j, [128, PREP_OUT], F32) for j in range(2)]
        pis = [S.sb("pi%d" % j, [128, 1], I32) for j in range(2)]
        sq, b_sq = S.sb("sq", [128, 5, 128], F32)
        st, b_st = S.sb("st", [128, 4, 8], F32)
        ang, b_ang = S.sb("ang", [128, 4, 48], F32)
        ki, b_ki = S.sb("ki", [128, 48], I32)
        sn, b_sn = S.sb("sn", [128, 48], F32)
        rt, b_rt = S.sb("rt", [128, 4, 96], F32)
        for i in range(NT):
            x, b_x = xs[i % 2]
            o, b_o = os_[i % 2]
            pi_, b_pi = pis[i % 2]
            S.dma("sp", x[:], X[i * 128:(i + 1) * 128, :], None, b_x)
            S.dma("act", pi_[:], POS[i * 128:(i + 1) * 128, :], None, b_pi)
            CP(S, "dve", ang[:, 3, 0:1], pi_[:], [b_pi], [b_ang])
            STT(S, "dve", ang[:, 0, :], inv[:], ang[:, 3, 0:1], ph[:], ALU.mult, ALU.add, [b_inv, b_ang, b_ph], [b_ang])
            TS(S, "dve", ki[:], ang[:, 0, :], 1.0 / TWO_PI, None, ALU.mult, R=[b_ang], W=[b_ki])
            CP(S, "dve", ang[:, 1, :], ki[:], [b_ki], [b_ang])
            STT(S, "dve", ang[:, 0, :], ang[:, 1, :], -TWO_PI, ang[:, 0, :], ALU.mult, ALU.add, [b_ang], [b_ang])
            TS(S, "dve", ang[:, 1, :], ang[:, 0, :], float(np.pi), -TWO_PI, ALU.is_gt, ALU.mult, R=[b_ang], W=[b_ang])
            TT(S, "dve", ang[:, 0, :], ang[:, 0, :], ang[:, 1, :], ALU.add, [b_ang], [b_ang])
            TS(S, "dve", ang[:, 1, :], ang[:, 0, :], float(-np.pi), TWO_PI, ALU.is_lt, ALU.mult, R=[b_ang], W=[b_ang])
            TT(S, "dve", ang[:, 0, :], ang[:, 0, :], ang[:, 1, :], ALU.add, [b_ang], [b_ang])
            TS(S, "dve", ang[:, 0, :], ang[:, 0, :], -3.14159, 3.14159, ALU.max, ALU.min, R=[b_ang], W=[b_ang])
            ACT(S, sn[:], ang[:, 0, :], AF.Sin, [b_ang], [b_sn])
            sin16, sin8, cos16, cos8 = sn[:, 0:16], sn[:, 16:24], sn[:, 24:40], sn[:, 40:48]
            groups = [
                (x[:, 0:512].rearrange("p (h d) -> p h d", h=4), 4, gn[:, 0:4, :],
                 o[:, 0:512].rearrange("p (h d) -> p h d", h=4)),
                (x[:, 768:1280].rearrange("p (a b) -> p a b", b=256)[:, :, 0:128], 2, gn[:, 4:6, :],
                 o[:, 1024:1280].rearrange("p (h d) -> p h d", h=2)),
                (x[:, 1292:1932].rearrange("p (h d) -> p h d", h=5), 5, gn[:, 6:11, :],
                 o[:, 1280:1920].rearrange("p (h d) -> p h d", h=5)),
            ]
            for gi, (src, H, gain, dst) in enumerate(groups):
                TT(S, "dve", sq[:, 0:H, :], src, src, ALU.mult, [b_x], [b_sq])
                RED(S, "dve", st[:, 0, 0:H], sq[:, 0:H, :], ALU.add, [b_sq], [b_st])
                TS(S, "dve", st[:, 1, 0:H], st[:, 0, 0:H], 1.0 / 128, EPS, ALU.mult, ALU.add, R=[b_st], W=[b_st])
                RSTD(S, st[:, 2, 0:H], st[:, 3, 0:H], st[:, 1, 0:H], [b_st], [b_st])
                TT(S, "dve", dst, src, st[:, 2, 0:H].unsqueeze(2).to_broadcast([128, H, 128]), ALU.mult,
                   [b_x, b_st], [b_o])
                TT(S, "dve", dst, dst, gain, ALU.mult, [b_o, b_gn], [b_o])
            CP(S, "pool", o[:, 512:1024], o[:, 0:512], [b_o], [b_o])
            _rope_inplace(S, o[:, 512:1024].rearrange("p (h d) -> p h d", h=4), b_o, 4, 16, cos16, sin16, b_sn, rt, b_rt)
            _rope_inplace(S, o[:, 1024:1280].rearrange("p (h d) -> p h d", h=2), b_o, 2, 16, cos16, sin16, b_sn, rt, b_rt)
            _rope_inplace(S, o[:, 1280:1920].rearrange("p (h d) -> p h d", h=5), b_o, 5, 16, cos16, sin16, b_sn, rt, b_rt)
            CP(S, "pool", o[:, 1920:2496], x[:, 2060:2636], [b_x], [b_o])
            _rope_inplace(S, o[:, 1920:2496].rearrange("p (h d) -> p h d", h=9), b_o, 9, 8, cos8, sin8, b_sn, rt, b_rt)
            ACT(S, o[:, 2496:2508], x[:, 1280:1292], AF.Sigmoid, [b_x], [b_o])
            TS(S, "dve", o[:, 2508:2516], x[:, 2636:2644], float(8 ** -0.5), None, ALU.mult, R=[b_x], W=[b_o])
            S.dma("pool", Y[i * 128:(i + 1) * 128, :], o[:], b_o, None, sem_on="src")
        S.finish([(b.dsem, b.dcnt) for _, b in os_])
    return nc


def run_prep(pcd, positions, nsa_q_g, nsa_k_g, dsa_q_g, dsa_k_g):
    Ttot = pcd.shape[0]
    T = Ttot // NCORES
    nc = _get(("prep", T), lambda: build_prep(T))
    gl = [nsa_q_g] * 4 + [nsa_k_g[1], nsa_k_g[2]] + [dsa_q_g] * 4 + [dsa_k_g]
    gn = np.ascontiguousarray(np.broadcast_to(np.stack(gl)[None], (128, 11, 128))).astype(np.float32)
    inv16 = (500000.0 ** (-np.arange(16, dtype=np.float32) / np.float32(16))).astype(np.float32)
    inv8 = (500000.0 ** (-np.arange(8, dtype=np.float32) / np.float32(8))).astype(np.float32)
    inv = rep128(np.concatenate([inv16, inv8, inv16, inv8]))
    ph = rep128(np.concatenate([np.zeros(24, np.float32), np.full(24, np.pi / 2, np.float32)]))
    pos = positions.reshape(-1, 1).astype(np.int32)
    maps = []
    for c in range(NCORES):
        maps.append({"X": np.ascontiguousarray(pcd[c * T:(c + 1) * T]), "pos": np.ascontiguousarray(pos[c * T:(c + 1) * T]),
                     "gn": gn, "inv": inv, "ph": ph})
    res = _run(nc, maps)
    return np.concatenate([r["Y"] for r in res], axis=0)


NIT = 22
NEGBIG = -1.0e30


def _load_cast_cols(S, dst, b_dst, src_ap, P, ncols, stages, piece=2048, tag=""):
    k = 0
    for c0 in range(0, ncols, piece):
        cw = min(piece, ncols - c0)
        st, b_st = stages[k % len(stages)]
        S.dma("sp" if k % 2 == 0 else "act", st[0:P, 0:cw], src_ap[:, c0:c0 + cw], None, b_st)
        CP(S, "pool" if k % 2 == 0 else "dve", dst[0:P, c0:c0 + cw], st[0:P, 0:cw], [b_st], [b_dst])
        k += 1


def build_dsa(NJ):
    nc = bass.Bass("TRN2", target_bir_lowering=False)
    NB = NJ * 8
    TT_ = NB * 128
    IQT = nc.dram_tensor("iqT", [NJ, 64, 8 * 128], F32, kind="ExternalInput").ap()
    IW = nc.dram_tensor("iw", [NJ, 128, 8], F32, kind="ExternalInput").ap()
    QT = nc.dram_tensor("qT", [NJ, 128, 512], F32, kind="ExternalInput").ap()
    IKT = nc.dram_tensor("ikT", [64, TT_], F32, kind="ExternalInput").ap()
    KT = nc.dram_tensor("kT", [128, TT_], F32, kind="ExternalInput").ap()
    VA = nc.dram_tensor("va", [128, NB * 129], F32, kind="ExternalInput").ap()
    NM = nc.dram_tensor("negmask", [128, 1024], F32, kind="ExternalInput").ap()
    P2 = nc.dram_tensor("pow2", [128, NIT], F32, kind="ExternalInput").ap()
    ID = nc.dram_tensor("ident", [128, 128], F32, kind="ExternalInput").ap()
    Y = nc.dram_tensor("Y", [NJ, 128, 512], F32, kind="ExternalOutput").ap()
    scale = float(128 ** -0.5)
    with contextlib.ExitStack() as es:
        S = Sched(nc, es)
        idb, b_idb = load_const(S, "id", ID, [128, 128], cast=BF16)
        nm, b_nm = load_const(S, "nm", NM, [128, 1024])
        p2, b_p2 = load_const(S, "p2", P2, [128, NIT])
        stages = [S.sb("stg%d" % k, [128, 2064], F32) for k in range(2)]
        ikT, b_ikT = S.sb("ikT", [64, TT_], BF16)
        kT, b_kT = S.sb("kT", [128, TT_], BF16)
        va, b_va = S.sb("va", [128, NB * 129], BF16)
        _load_cast_cols(S, ikT, b_ikT, IKT, 64, TT_, stages)
        _load_cast_cols(S, kT, b_kT, KT, 128, TT_, stages)
        _load_cast_cols(S, va, b_va, VA, 128, NB * 129, stages, piece=2064)
        va3 = va[:].rearrange("p (b d) -> p b d", d=129)
        score, b_score = S.sb("score", [128, TT_], F32)
        maskq, b_maskq = S.sb("maskq", [128, TT_], BF16)
        maskT, b_maskT = S.sb("maskT", [128, NB, 128], BF16)
        iqf = [S.sb("iqf%d" % k, [64, 1024], F32) for k in range(2)]
        iqb = [S.sb("iqb%d" % k, [64, 1024], BF16) for k in range(2)]
        qf = [S.sb("qf%d" % k, [128, 512], F32) for k in range(2)]
        qb = [S.sb("qb%d" % k, [128, 512], BF16) for k in range(2)]
        iws = [S.sb("iw%d" % k, [128, 8], F32) for k in range(2)]
        rbuf = [S.sb("r%d" % k, [128, 512], F32) for k in range(3)]
        ebuf = [S.sb("e%d" % k, [128, 512], F32) for k in range(2)]
        pbuf = [S.sb("p%d" % k, [128, 4, 128], BF16) for k in range(2)]
        obuf = [S.sb("ob%d" % k, [128, 512], F32) for k in range(2)]
        bs_, b_bs = S.sb("bis", [128, 8], F32)
        hd, b_hd = S.sb("hd", [128, NIT], F32)
        cnt, b_cnt = S.sb("cnt", [128, NIT], F32)
        zz, b_zz = S.sb("zz", [128, 8], F32)
        sps = [S.ps("sps%d" % k, [128, 512], F32) for k in range(2)]
        stp = sps
        tps, b_tps = S.ps("tps", [128, 512], BF16)
        ops_, b_ops = S.ps("ops", [128, 4, 512], F32)
        nr = 0
        for j in range(NJ):
            NBj = 8 * j + 8
            L = NBj * 128
            NCH = NBj // 4
            iq_f, b_iqf = iqf[j % 2]
            iq_b, b_iqb = iqb[j % 2]
            q_f, b_qf = qf[j % 2]
            q_b, b_qb = qb[j % 2]
            iw, b_iw = iws[j % 2]
            S.dma("sp", iq_f[:], IQT[j], None, b_iqf)
            S.dma("act", q_f[:], QT[j], None, b_qf)
            S.dma("sp", iw[:], IW[j], None, b_iw)
            CP(S, "pool", iq_b[:], iq_f[:], [b_iqf], [b_iqb])
            CP(S, "pool", q_b[:], q_f[:], [b_qf], [b_qb])
            for ch in range(NCH):
                sc_ch = score[:, ch * 512:(ch + 1) * 512]
                for h in range(8):
                    ps, b_ps = sps[nr % 2]
                    r, b_r = rbuf[nr % 3]
                    nr += 1
                    MM(S, ps[:], iq_b[:, h * 128:(h + 1) * 128], ikT[:, ch * 512:(ch + 1) * 512], True, True,
                       [b_iqb, b_ikT], [b_ps])
                    ACT(S, r[:], ps[:], AF.Relu, [b_ps], [b_r], scale=0.125)
                    if h == 0:
                        TS(S, "dve", sc_ch, r[:], iw[:, 0:1], None, ALU.mult, R=[b_r, b_iw], W=[b_score])
                    else:
                        STT(S, "dve", sc_ch, r[:], iw[:, h:h + 1], sc_ch, ALU.mult, ALU.add,
                            [b_r, b_iw, b_score], [b_score])
            RED(S, "dve", bs_[:, 0:1], score[:, 0:L], ALU.max, [b_score], [b_bs])
            RED(S, "dve", bs_[:, 1:2], score[:, 0:L], ALU.min, [b_score], [b_bs])
            TT(S, "dve", score[:, L - 1024:L], score[:, L - 1024:L], nm[:], ALU.add, [b_score, b_nm], [b_score])
            TS(S, "dve", bs_[:, 2:3], bs_[:, 1:2], -1.0, None, ALU.add, R=[b_bs], W=[b_bs])
            STT(S, "dve", bs_[:, 3:4], bs_[:, 0:1], 2.0, bs_[:, 1:2], ALU.add, ALU.subtract, [b_bs], [b_bs])
            TS(S, "dve", hd[:], p2[:], bs_[:, 3:4], None, ALU.mult, R=[b_p2, b_bs], W=[b_hd])
            S.op("dve", lambda e: e.memset(cnt[:], 0.0), [], [b_cnt])
            for k in range(NIT):
                TT(S, "dve", bs_[:, 4:5], bs_[:, 2:3], hd[:, k:k + 1], ALU.add, [b_bs, b_hd], [b_bs])
                TS(S, "dve", maskq[:, 0:L], score[:, 0:L], bs_[:, 4:5], None, ALU.is_ge, ALU.add,
                   R=[b_score, b_bs, b_cnt], W=[b_maskq, b_cnt], accum=cnt[:, k:k + 1])
                TS(S, "dve", bs_[:, 5:6], cnt[:, k:k + 1], 255.5, hd[:, k:k + 1], ALU.is_gt, ALU.mult,
                   R=[b_cnt, b_hd], W=[b_bs])
                TT(S, "dve", bs_[:, 2:3], bs_[:, 2:3], bs_[:, 5:6], ALU.add, [b_bs], [b_bs])
            TS(S, "dve", maskq[:, 0:L], score[:, 0:L], bs_[:, 2:3], None, ALU.is_ge, R=[b_score, b_bs], W=[b_maskq])
            for g4 in range(NBj // 4):
                for t in range(4):
                    kb = g4 * 4 + t
                    TR(S, tps[:, t * 128:(t + 1) * 128], maskq[:, kb * 128:(kb + 1) * 128], idb[:],
                       [b_maskq, b_idb], [b_tps], acc=(t > 0))
                CP(S, "act", maskT[:, g4 * 4:(g4 + 1) * 4, :], tps[:].rearrange("p (a b) -> p a b", a=4),
                   [b_tps], [b_maskT])
            for kb in range(NBj):
                st_, b_st = stp[kb % 2]
                e_, b_e = ebuf[kb % 2]
                p_, b_p = pbuf[kb % 2]
                MM(S, st_[:], kT[:, kb * 128:(kb + 1) * 128], q_b[:], True, True, [b_kT, b_qb], [b_st])
                ACT(S, e_[:], st_[:], AF.Exp, [b_st], [b_e], scale=scale)
                TT(S, "dve", p_[:], e_[:].rearrange("p (h q) -> p h q", h=4),
                   maskT[:, kb, :].unsqueeze(1).to_broadcast([128, 4, 128]), ALU.mult, [b_e, b_maskT], [b_p])
                for h in range(4):
                    MM(S, ops_[:, h, 0:129], p_[:, h, :], va3[:, kb, :], kb == 0, kb == NBj - 1,
                       [b_p, b_va], [b_ops])
            o_, b_o = obuf[j % 2]
            TS(S, "dve", zz[:, 0:4], ops_[:, :, 128], 1e-30, None, ALU.max, R=[b_ops], W=[b_zz])
            S.op("dve", lambda e: e.reciprocal(out=zz[:, 4:8], in_=zz[:, 0:4]), [b_zz], [b_zz])
            TT(S, "dve", o_[:].rearrange("p (h d) -> p h d", h=4), ops_[:, :, 0:128],
               zz[:, 4:8].unsqueeze(2).to_broadcast([128, 4, 128]), ALU.mult, [b_ops, b_zz], [b_o])
            S.dma("pool", Y[j], o_[:], b_o, None, sem_on="src")
        S.finish([(b.dsem, b.dcnt) for _, b in obuf])
    return nc


def causal_negmask(c):
    m = np.zeros((128, 8, 128), np.float32)
    q = np.arange(128)
    for r in range(8):
        if r == c:
            m[:, r, :] = np.where(q[None, :] <= q[:, None], 0.0, NEGBIG)
        elif r > c:
            m[:, r, :] = NEGBIG
    return m.reshape(128, 1024)


def own_tiles_T(a, H, D, c, NJ):
    out = []
    for j in range(NJ):
        g = 8 * j + c
        t = a[g * 128:(g + 1) * 128].reshape(128, H, D)
        out.append(np.transpose(t, (2, 1, 0)).reshape(D, H * 128))
    return np.ascontiguousarray(np.stack(out))


def v_aug(v):
    NB = v.shape[0] // 128
    t = np.ones((128, NB, 129), np.float32)
    t[:, :, 0:128] = np.transpose(v.reshape(NB, 128, 128), (1, 0, 2))
    return t.reshape(128, NB * 129)


def run_dsa(Yp, vd):
    Ttot = Yp.shape[0]
    NJ = Ttot // (128 * NCORES)
    nc = _get(("dsa", NJ), lambda: build_dsa(NJ))
    ikT = np.ascontiguousarray(Yp[:, 2432:2496].T)
    kT = np.ascontiguousarray(Yp[:, 1792:1920].T)
    va = v_aug(vd)
    pow2 = rep128(2.0 ** -(np.arange(NIT, dtype=np.float32) + 1))
    maps = []
    for c in range(NCORES):
        own = [8 * j + c for j in range(NJ)]
        maps.append({"iqT": own_tiles_T(Yp[:, 1920:2432], 8, 64, c, NJ),
                     "iw": np.ascontiguousarray(np.stack([Yp[g * 128:(g + 1) * 128, 2508:2516] for g in own])),
                     "qT": own_tiles_T(Yp[:, 1280:1792], 4, 128, c, NJ),
                     "ikT": ikT, "kT": kT, "va": va, "negmask": causal_negmask(c), "pow2": pow2, "ident": IDENT})
    res = _run(nc, maps)
    out = np.zeros((Ttot, 512), np.float32)
    for c in range(NCORES):
        for j in range(NJ):
            g = 8 * j + c
            out[g * 128:(g + 1) * 128] = res[c]["Y"][j]
    return out


GELU_C = 1.5957691216057308


def MMG(S, out, lhsT, rhs, first, R, W):
    return S.op("pe", lambda e: e.matmul(out, lhsT, rhs, start=first, stop=False, skip_group_check=True),
                R, W, accumulate=True)


def build_nsa(NJ):
    nc = bass.Bass("TRN2", target_bir_lowering=False)
    NB = NJ * 8
    TT_ = NB * 128
    NCMP = (TT_ - 32) // 16 + 1
    NCC = (NCMP + 127) // 128
    NCP = NCC * 128
    dr = lambda name, shape: nc.dram_tensor(name, shape, F32, kind="ExternalInput").ap()
    QNT = dr("qnT", [NJ, 128, 512])
    QRT = dr("qrT", [NJ, 128, 512])
    GATES = dr("gates", [NJ, 128, 12])
    CMASK = dr("cmask", [NJ, 128, 4 * 128])
    SELB = dr("selb", [NJ, 128, 128])
    KCT = dr("kcmpT", [128, TT_])
    VCT = dr("vcmpT", [128, TT_])
    W1 = dr("w1", [128, 2 * 32 * 128])
    POST = dr("posT", [128, 64])
    W2 = dr("w2", [128, 256])
    KG0 = dr("kg0", [128, 128])
    KST = dr("ksT", [128, TT_])
    KWT = dr("kwT", [128, TT_])
    VSA = dr("vsa", [128, NB * 129])
    VWA = dr("vwa", [128, NB * 129])
    OV = dr("ov", [128, 512])
    EX = dr("expE", [128, NB * 128])
    CAUS = dr("causT", [128, 1024])
    WINM = dr("winT", [128, 1536])
    ID = dr("ident", [128, 128])
    Y = nc.dram_tensor("Y", [NJ, 128, 512], F32, kind="ExternalOutput").ap()
    scale = float(128 ** -0.5)
    skip = set(os.environ.get("NSA_SKIP", "").split(","))
    with contextlib.ExitStack() as es:
        S = Sched(nc, es)
        idf, b_idf = load_const(S, "id", ID, [128, 128])
        kg0, b_kg0 = load_const(S, "kg0", KG0, [128, 128])
        stages = [S.sb("stg%d" % k, [128, 2064], F32) for k in range(2)]

        def bf_const(name, ap, ncols, piece=2048):
            t, b = S.sb(name, [128, ncols], BF16)
            _load_cast_cols(S, t, b, ap, 128, ncols, stages, piece=piece)
            return t, b
        w1, b_w1 = bf_const("w1", W1, 8192)
        posT, b_posT = bf_const("posT", POST, 64)
        w2, b_w2 = bf_const("w2", W2, 256)
        kcx, b_kcx = bf_const("kcx", KCT, TT_)
        vcx, b_vcx = bf_const("vcx", VCT, TT_)
        ksT, b_ksT = bf_const("ksT", KST, TT_)
        kwT, b_kwT = bf_const("kwT", KWT, TT_)
        vsa, b_vsa = bf_const("vsa", VSA, NB * 129, piece=2064)
        vwa, b_vwa = bf_const("vwa", VWA, NB * 129, piece=2064)
        ov, b_ov = bf_const("ov", OV, 512)
        exE, b_exE = bf_const("exE", EX, NB * 128)
        caus, b_caus = bf_const("caus", CAUS, 1024)
        winm, b_winm = bf_const("winm", WINM, 1536)
        vsa3 = vsa[:].rearrange("p (b d) -> p b d", d=129)
        vwa3 = vwa[:].rearrange("p (b d) -> p b d", d=129)
        w1v = w1[:].rearrange("p (x l j) -> p x l j", x=2, l=32)
        A = [S.ps("A%d" % k, [128, 512], F32) for k in range(2)]
        O, b_O = S.ps("O", [128, 4, 512], F32)
        IMP, b_IMP = S.ps("IMP", [128, 4, 128], F32)
        Mk, b_Mk = S.ps("Mk", [128, 512], F32)
        stop_at = os.environ.get("NSA_STOP", "")
        if stop_at == "c0":
            S.finish([])
            return nc
        kcT, b_kcT = S.sb("kcT", [128, NCP], BF16)
        vca, b_vca = S.sb("vca", [128, NCC, 129], BF16)
        S.op("dve", lambda e: e.memset(vca[:], 1.0), [], [b_vca])
        hs, b_hs = S.sb("hs", [128, NCP], F32)
        t1, b_t1 = S.sb("t1", [128, NCP], F32)
        t2, b_t2 = S.sb("t2", [128, NCP], F32)
        G, b_G = S.sb("G", [128, NCP], BF16)
        cb, b_cb = S.sb("cb", [128, 8], F32)
        kcs, b_kcs = S.sb("kcs", [128, 128], F32)
        jk, b_jk = S.sb("jk", [128, 128], F32)
        for X in range(2):
            src = kcx if X == 0 else vcx
            b_src = b_kcx if X == 0 else b_vcx
            xv = src[:].rearrange("p (n s) -> p s n", s=16)
            pc_, b_pc = A[0]
            ph_, b_ph = A[1]
            for l in range(32):
                if "cb" in skip:
                    continue
                MM(S, pc_[:, 0:1], w1v[:, X, l, :], posT[:, X * 32 + l:X * 32 + l + 1], l == 0, l == 31,
                   [b_w1, b_posT], [b_pc])
            CP(S, "dve", cb[:, X:X + 1], pc_[:, 0:1], [b_pc], [b_cb])
            for l in range(32):
                rhs = xv[:, l, 0:NCMP] if l < 16 else xv[:, l - 16, 1:1 + NCMP]
                if "ht" in skip:
                    continue
                MM(S, ph_[:, 0:NCMP], w1v[:, X, l, :], rhs, l == 0, l == 31, [b_w1, b_src], [b_ph])
            S.op("dve", lambda e: e.memset(hs[:], 0.0), [], [b_hs])
            ACT(S, hs[:, 0:NCMP], ph_[:, 0:NCMP], AF.Identity, [b_ph, b_cb], [b_hs], bias=cb[:, X:X + 1])
            TT(S, "dve", t1[:], hs[:], hs[:], ALU.mult, [b_hs], [b_t1])
            TS(S, "dve", t1[:], t1[:], 0.044715, 1.0, ALU.mult, ALU.add, R=[b_t1], W=[b_t1])
            TT(S, "dve", t1[:], t1[:], hs[:], ALU.mult, [b_t1, b_hs], [b_t1])
            ACT(S, t2[:], t1[:], AF.Sigmoid, [b_t1], [b_t2], scale=GELU_C)
            TT(S, "dve", G[:], hs[:], t2[:], ALU.mult, [b_hs, b_t2], [b_G])
            for ch in range(NCC):
                po_, b_po = A[ch % 2]
                MM(S, po_[:, 0:128], G[:, ch * 128:(ch + 1) * 128], w2[:, X * 128:(X + 1) * 128], True, True,
                   [b_G, b_w2], [b_po])
                if X == 0:
                    ACT(S, jk[:], po_[:, 0:128], AF.Square, [b_po], [b_jk, b_cb], accum=cb[:, 2:3])
                    TS(S, "dve", cb[:, 3:4], cb[:, 2:3], 1.0 / 128, EPS, ALU.mult, ALU.add, R=[b_cb], W=[b_cb])
                    RSTD(S, cb[:, 4:5], cb[:, 5:6], cb[:, 3:4], [b_cb], [b_cb])
                    STT(S, "dve", kcs[:], po_[:, 0:128], cb[:, 4:5], kg0[:], ALU.mult, ALU.mult,
                        [b_po, b_cb, b_kg0], [b_kcs])
                    TR(S, Mk[:, 256:384], kcs[:], idf[:], [b_kcs, b_idf], [b_Mk])
                    CP(S, "act", kcT[:, ch * 128:(ch + 1) * 128], Mk[:, 256:384], [b_Mk], [b_kcT])
                else:
                    CP(S, "act", vca[:, ch, 0:128], po_[:, 0:128], [b_po], [b_vca])
        if stop_at == "c1":
            S.finish([])
            return nc
        qnf = [S.sb("qnf%d" % k, [128, 512], F32) for k in range(2)]
        qrf = [S.sb("qrf%d" % k, [128, 512], F32) for k in range(2)]
        qnb = [S.sb("qnb%d" % k, [128, 512], BF16) for k in range(2)]
        qrb = [S.sb("qrb%d" % k, [128, 512], BF16) for k in range(2)]
        gts = [S.sb("gt%d" % k, [128, 12], F32) for k in range(2)]
        cms = [S.sb("cm%d" % k, [128, 512], F32) for k in range(2)]
        sbs = [S.sb("sb%d" % k, [128, 128], F32) for k in range(2)]
        ebuf = [S.sb("e%d" % k, [128, 512], F32) for k in range(2)]
        pbuf = [S.sb("p%d" % k, [128, 4, 128], BF16) for k in range(2)]
        obuf = [S.sb("ob%d" % k, [128, 512], F32) for k in range(2)]
        ocmp, b_ocmp = S.sb("ocmp", [128, 4, 128], F32)
        oslc, b_oslc = S.sb("oslc", [128, 4, 128], F32)
        imp, b_imp = S.sb("imp", [128, 128], F32)
        imp2, b_imp2 = S.sb("imp2", [128, 128], F32)
        self_, b_self = S.sb("self", [128, 128], F32)
        selT, b_selT = S.sb("selT", [128, 128], BF16)
        zz, b_zz = S.sb("zz", [128, 48], F32)
        cf, b_cf = S.sb("cf", [128, 12], F32)
        ne = 0

        def attend(kT_ap, q_b, b_q, b_kT, mask_ap, mask_bufs, extra, v_ap, b_v, first, last):
            nonlocal ne
            a_, b_a = A[ne % 2]
            e_, b_e = ebuf[ne % 2]
            p_, b_p = pbuf[ne % 2]
            ne += 1
            MM(S, a_[:], kT_ap, q_b[:], True, True, [b_kT, b_q], [b_a])
            ACT(S, e_[:], a_[:], AF.Exp, [b_a], [b_e], scale=scale)
            TT(S, "dve", p_[:], e_[:].rearrange("p (h q) -> p h q", h=4),
               mask_ap.unsqueeze(1).to_broadcast([128, 4, 128]), ALU.mult, [b_e] + mask_bufs, [b_p])
            if extra is not None:
                TT(S, "pool", p_[:], p_[:], extra[0].unsqueeze(1).to_broadcast([128, 4, 128]), ALU.mult,
                   [b_p, extra[1]], [b_p])
            for h in range(4):
                MM(S, O[:, h, 0:129], p_[:, h, :], v_ap, first, last, [b_p, b_v], [b_O])
            return p_, b_p

        def finish_branch(zoff, dst, b_dst):
            TS(S, "dve", zz[:, zoff:zoff + 4], O[:, :, 128], 1e-30, None, ALU.max, R=[b_O], W=[b_zz])
            S.op("dve", lambda e: e.reciprocal(out=zz[:, zoff + 4:zoff + 8], in_=zz[:, zoff:zoff + 4]), [b_zz], [b_zz])
            if dst is not None:
                CP(S, "dve", dst[:], O[:, :, 0:128], [b_O], [b_dst])

        for j in range(NJ):
            NBj = 8 * j + 8
            NCj = min(NCC, (64 * j + 62) // 128 + 1)
            qn_f, b_qnf = qnf[j % 2]
            qr_f, b_qrf = qrf[j % 2]
            qn_b, b_qnb = qnb[j % 2]
            qr_b, b_qrb = qrb[j % 2]
            gt, b_gt = gts[j % 2]
            cm, b_cm = cms[j % 2]
            sbi, b_sbi = sbs[j % 2]
            S.dma("sp", qn_f[:], QNT[j], None, b_qnf)
            S.dma("act", qr_f[:], QRT[j], None, b_qrf)
            S.dma("sp", gt[:], GATES[j], None, b_gt)
            S.dma("act", cm[:], CMASK[j], None, b_cm)
            S.dma("sp", sbi[:], SELB[j], None, b_sbi)
            CP(S, "pool", qn_b[:], qn_f[:], [b_qnf], [b_qnb])
            CP(S, "pool", qr_b[:], qr_f[:], [b_qrf], [b_qrb])
            for ch in range(NCj):
                p_, b_p = attend(kcT[:, ch * 128:(ch + 1) * 128], qn_b, b_qnb, b_kcT, cm[:, ch * 128:(ch + 1) * 128],
                                 [b_cm], None, vca[:, ch, :], b_vca, ch == 0, ch == NCj - 1)
                for h in range(4):
                    if "imp" in skip:
                        continue
                    MMG(S, IMP[:, h, :], p_[:, h, :], ov[:, ch * 128:(ch + 1) * 128], ch == 0 and h == 0,
                        [b_p, b_ov], [b_IMP])
            finish_branch(0, ocmp, b_ocmp)
            TS(S, "dve", imp[:], IMP[:, 0, :], zz[:, 4:5], None, ALU.mult, R=[b_IMP, b_zz], W=[b_imp])
            for h in range(1, 4):
                STT(S, "dve", imp[:], IMP[:, h, :], zz[:, 4 + h:5 + h], imp[:], ALU.mult, ALU.add,
                    [b_IMP, b_zz, b_imp], [b_imp])
            TT(S, "dve", imp[:], imp[:], sbi[:], ALU.add, [b_imp, b_sbi], [b_imp])
            if "top" not in skip:
                S.op("dve", lambda e: e.max(out=zz[:, 24:32], in_=imp[:]), [b_imp], [b_zz])
                S.op("dve", lambda e: e.match_replace(out=imp2[:], in_to_replace=zz[:, 24:32], in_values=imp[:],
                                                      imm_value=-3.0e38), [b_imp, b_zz], [b_imp2])
                S.op("dve", lambda e: e.max(out=zz[:, 32:40], in_=imp2[:]), [b_imp2], [b_zz])
            RED(S, "dve", zz[:, 40:41], zz[:, 32:40], ALU.min, [b_zz], [b_zz])
            TS(S, "dve", self_[:], imp[:], zz[:, 40:41], None, ALU.is_ge, R=[b_imp, b_zz], W=[b_self])
            TR(S, Mk[:, 256:384], self_[:], idf[:], [b_self, b_idf], [b_Mk])
            CP(S, "act", selT[:], Mk[:, 256:384], [b_Mk], [b_selT])
            for kb in range(NBj):
                if "slc" in skip:
                    continue
                slot = (kb % 2) * 128
                MM(S, Mk[:, slot:slot + 128], exE[:, kb * 128:(kb + 1) * 128], selT[:], True, True,
                   [b_exE, b_selT], [b_Mk])
                extra = None
                if kb >= NBj - 8 and "extra" not in skip:
                    r = kb - (NBj - 8)
                    extra = (caus[:, r * 128:(r + 1) * 128], b_caus)
                attend(ksT[:, kb * 128:(kb + 1) * 128], qr_b, b_qrb, b_ksT, Mk[:, slot:slot + 128], [b_Mk], extra,
                       vsa3[:, kb, :], b_vsa, kb == 0, kb == NBj - 1)
            finish_branch(8, oslc, b_oslc)
            blks = [(r, 8 * j - 4 + r) for r in range(12) if 8 * j - 4 + r >= 0]
            for n_, (r, blk) in enumerate(blks):
                if "win" in skip:
                    continue
                attend(kwT[:, blk * 128:(blk + 1) * 128], qr_b, b_qrb, b_kwT, winm[:, r * 128:(r + 1) * 128],
                       [b_winm], None, vwa3[:, blk, :], b_vwa, n_ == 0, n_ == len(blks) - 1)
            finish_branch(16, None, None)
            gv = gt[:].rearrange("p (h b) -> p h b", b=3)
            for b_i, zo in enumerate((4, 12, 20)):
                TT(S, "dve", cf[:, b_i * 4:(b_i + 1) * 4], gv[:, :, b_i], zz[:, zo:zo + 4], ALU.mult,
                   [b_gt, b_zz], [b_cf])
            o_, b_o = obuf[j % 2]
            for h in range(4):
                oh = o_[:, h * 128:(h + 1) * 128]
                TS(S, "dve", oh, ocmp[:, h, :], cf[:, h:h + 1], None, ALU.mult, R=[b_ocmp, b_cf], W=[b_o])
                STT(S, "dve", oh, oslc[:, h, :], cf[:, 4 + h:5 + h], oh, ALU.mult, ALU.add,
                    [b_oslc, b_cf, b_o], [b_o])
                STT(S, "dve", oh, O[:, h, 0:128], cf[:, 8 + h:9 + h], oh, ALU.mult, ALU.add,
                    [b_O, b_cf, b_o], [b_o])
            S.dma("pool", Y[j], o_[:], b_o, None, sem_on="src")
        S.finish([(b.dsem, b.dcnt) for _, b in obuf])
    return nc


def run_nsa(Yp, pc, cmp_pos, cmp_w1, cmp_w2, k_g0):
    Ttot = Yp.shape[0]
    NJ = Ttot // (128 * NCORES)
    NB = NJ * 8
    nc = _get(("nsa", NJ), lambda: build_nsa(NJ))
    NCMP = (Ttot - 32) // 16 + 1
    NSEL = Ttot // 64
    w1 = np.ascontiguousarray(np.transpose(cmp_w1.reshape(2, 32, 128, 128), (2, 0, 1, 3))).reshape(128, 8192)
    posT = np.ascontiguousarray(np.transpose(cmp_pos, (2, 0, 1))).reshape(128, 64)
    w2 = np.ascontiguousarray(np.transpose(cmp_w2, (1, 0, 2))).reshape(128, 256)
    n_all = np.arange(512)
    jb = np.arange(128)
    ovm = ((n_all[:, None] >= 4 * jb[None, :] - 1) & (n_all[:, None] <= 4 * jb[None, :] + 3)
           & (n_all[:, None] < NCMP) & (jb[None, :] < NSEL)).astype(np.float32)
    ov = np.ascontiguousarray(np.transpose(ovm.reshape(4, 128, 128), (1, 0, 2))).reshape(128, 512)
    s_ = np.arange(128)
    exE = np.zeros((128, NB, 128), np.float32)
    for kb in range(NB):
        exE[2 * kb + s_ // 64, kb, s_] = 1.0 if True else 0.0
    exE = exE[:128].reshape(128, NB * 128) if 2 * NB <= 128 else exE.reshape(128, NB * 128)
    tri = (s_[:, None] <= s_[None, :]).astype(np.float32)
    common = {"kcmpT": np.ascontiguousarray(pc[:, 512:640].T), "vcmpT": np.ascontiguousarray(pc[:, 640:768].T),
              "w1": w1, "posT": posT, "w2": w2, "kg0": rep128(k_g0),
              "ksT": np.ascontiguousarray(Yp[:, 1024:1152].T), "kwT": np.ascontiguousarray(Yp[:, 1152:1280].T),
              "vsa": v_aug(np.ascontiguousarray(pc[:, 896:1024])), "vwa": v_aug(np.ascontiguousarray(pc[:, 1152:1280])),
              "ov": ov, "expE": exE, "ident": IDENT}
    maps = []
    for c in range(NCORES):
        own = [8 * j + c for j in range(NJ)]
        caus = np.zeros((128, 8, 128), np.float32)
        for r in range(8):
            if r < c:
                caus[:, r, :] = 1.0
            elif r == c:
                caus[:, r, :] = tri
        winT = np.zeros((128, 12, 128), np.float32)
        for r in range(12):
            d = r - 4 - c
            if d == 0:
                winT[:, r, :] = tri
            elif d in (-1, -2, -3):
                winT[:, r, :] = 1.0
            elif d == -4:
                winT[:, r, :] = 1.0 - tri
        cmask = np.zeros((NJ, 128, 4, 128), np.float32)
        selb = np.zeros((NJ, 128, 128), np.float32)
        for j, g in enumerate(own):
            t = g * 128 + s_
            nn = np.arange(512).reshape(4, 128)
            ok = (16 * nn[:, :, None] + 31 <= t[None, None, :]) & (nn[:, :, None] < NCMP)
            cmask[j] = np.transpose(ok, (1, 0, 2)).astype(np.float32)
            cur = t // 64
            forced = (jb[None, :] == 0) | (jb[None, :] == cur[:, None]) | (jb[None, :] == cur[:, None] - 1)
            future = jb[None, :] > cur[:, None]
            selb[j] = np.where(forced, 1.0e30, np.where(future, NEGBIG, 0.0)).astype(np.float32)
        m = dict(common)
        m.update({"qnT": own_tiles_T(Yp[:, 0:512], 4, 128, c, NJ), "qrT": own_tiles_T(Yp[:, 512:1024], 4, 128, c, NJ),
                  "gates": np.ascontiguousarray(np.stack([Yp[g * 128:(g + 1) * 128, 2496:2508] for g in own])),
                  "cmask": cmask.reshape(NJ, 128, 512), "selb": selb,
                  "causT": caus.reshape(128, 1024), "winT": winT.reshape(128, 1536)})
        maps.append(m)
    res = _run(nc, maps)
    out = np.zeros((Ttot, 512), np.float32)
    for c in range(NCORES):
        for j in range(NJ):
            g = 8 * j + c
            out[g * 128:(g + 1) * 128] = res[c]["Y"][j]
    return out


def build_peer(NTL, NCH_E=128):
    nc = bass.Bass("TRN2", target_bir_lowering=False)
    T = NTL * 128
    TW = T
    K = 2048
    KC = 16
    dr = lambda name, shape: nc.dram_tensor(name, shape, F32, kind="ExternalInput").ap()
    X = dr("X", [T, K])
    G_ = dr("pg", [128, K])
    SC = dr("psc", [128, K])
    SH = dr("psh", [128, K])
    GATE = dr("gate", [128, K])
    WQ = dr("wq", [128, KC * 1024])
    SK = dr("skT", [128, 1024])
    UT = dr("UT", [NCH_E, 128, KC * 128])
    V = dr("V", [NCH_E * 128, K])
    ID = dr("ident", [128, 128])
    Y = nc.dram_tensor("Y", [T, K], F32, kind="ExternalOutput").ap()
    with contextlib.ExitStack() as es:
        S = Sched(nc, es)
        idb, b_idb = load_const(S, "id", ID, [128, 128], cast=BF16)
        A, b_A = load_const(S, "A", SC, [128, K])
        B, b_B = load_const(S, "B", SH, [128, K])
        gtmp, b_gtmp = load_const(S, "gtmp", G_, [128, K])
        STT(S, "dve", A[:], A[:], 1.0, gtmp[:], ALU.add, ALU.mult, [b_A, b_gtmp], [b_A])
        xs = [S.sb("x%d" % j, [128, K], F32) for j in range(2)]
        tmp, b_tmp = S.sb("tmp", [128, K], F32)
        hb, b_hb = S.sb("hb", [128, K], BF16)
        st, b_st = S.sb("st", [128, 8], F32)
        hnT, b_hnT = S.sb("hnT", [128, KC, TW], BF16)
        accw, b_accw = S.sb("accw", [128, NTL * K], F32)
        wqb = accw[:].bitcast(BF16)
        assert 2 * NTL * K >= KC * 1024
        skb, b_skb = S.sb("skb", [128, 1024], BF16)
        PA = [S.ps("PA%d" % k, [128, 512], F32) for k in range(4)]
        PV = [S.ps("PV%d" % k, [128, 512], F32) for k in range(4)]
        stg = [(xs[0][0], xs[0][1]), (xs[1][0], xs[1][1])]
        _load_cast_cols(S, accw[:].bitcast(BF16), b_accw, WQ, 128, KC * 1024, stg)
        _load_cast_cols(S, skb, b_skb, SK, 128, 1024, stg)
        s2s = [S.sb("s2_%d" % i, [128, 8, 128], F32) for i in range(NTL)]
        THR = [S.sb("thr_%d" % i, [128, 8, 128], F32) for i in range(NTL)]
        BIA = [S.sb("bia_%d" % i, [128, 8, 128], F32) for i in range(NTL)]
        qT, b_qT = S.sb("qT", [128, 8, 128], BF16)
        sv, b_sv = S.sb("sv", [128, 8, 2, 16], F32)
        mr, b_mr = S.sb("mr", [128, 256], F32)
        cand, b_cand = S.sb("cand", [128, 8, 256], F32)
        cv, b_cv = S.sb("cv", [128, 8, 16], F32)
        sm, b_sm = S.sb("sm", [128, 6, 8], F32)
        jk16, b_jk16 = S.sb("jk16", [128, 16], F32)
        for i in range(NTL):
            x, b_x = xs[i % 2]
            S.dma("sp", x[:], X[i * 128:(i + 1) * 128, :], None, b_x)
            ACT(S, tmp[:], x[:], AF.Square, [b_x], [b_tmp, b_st], accum=st[:, 0:1])
            TS(S, "dve", st[:, 1:2], st[:, 0:1], 1.0 / K, EPS, ALU.mult, ALU.add, R=[b_st], W=[b_st])
            RSTD(S, st[:, 2:3], st[:, 3:4], st[:, 1:2], [b_st], [b_st])
            STT(S, "dve", tmp[:], x[:], st[:, 2:3], A[:], ALU.mult, ALU.mult, [b_x, b_st, b_A], [b_tmp])
            TT(S, "dve", hb[:], tmp[:], B[:], ALU.add, [b_tmp, b_B], [b_hb])
            for q4 in range(KC // 4):
                pt_, b_pt = PA[q4 % 2]
                ptb = pt_[:].bitcast(BF16)
                for t in range(4):
                    kc = q4 * 4 + t
                    TR(S, ptb[:, t * 128:(t + 1) * 128], hb[:, kc * 128:(kc + 1) * 128], idb[:], [b_hb, b_idb],
                       [b_pt], acc=(t > 0))
                CP(S, "act" if q4 % 2 == 0 else "dve", hnT[:, q4 * 4:(q4 + 1) * 4, i * 128:(i + 1) * 128],
                   ptb[:, 0:512].rearrange("p (a b) -> p a b", a=4), [b_pt], [b_hnT])
            for h in range(8):
                pq, b_pq = PA[2 + h % 2]
                for kc in range(KC):
                    MM(S, pq[:, 0:128], wqb[:, kc * 1024 + h * 128:kc * 1024 + (h + 1) * 128],
                       hnT[:, kc, i * 128:(i + 1) * 128], kc == 0, kc == KC - 1, [b_accw, b_hnT], [b_pq])
                CP(S, "act", qT[:, h, :], pq[:, 0:128], [b_pq], [b_qT])
            s2, b_s2 = s2s[i]
            s1, b_s1 = BIA[i]
            for p_ in range(2):
                dst, b_dst = (s1, b_s1) if p_ == 0 else (s2, b_s2)
                lo, hi = p_ * 64, (p_ + 1) * 64
                for hh in range(2):
                    ps_, b_ps = PA[hh]
                    for h4 in range(4):
                        h = hh * 4 + h4
                        MM(S, ps_[:, h4 * 128:(h4 + 1) * 128], qT[lo:hi, h, :], skb[lo:hi, h * 128:(h + 1) * 128],
                           True, True, [b_qT, b_skb], [b_ps])
                    CP(S, "act", dst[:, hh * 4:(hh + 1) * 4, :], ps_[:].rearrange("p (a b) -> p a b", a=4),
                       [b_ps], [b_dst])
            for h in range(8):
                for p_ in range(2):
                    src = s1[:, h, :] if p_ == 0 else s2[:, h, :]
                    b_src = b_s1 if p_ == 0 else b_s2
                    S.op("dve", lambda e, src=src, h=h, p_=p_: e.max(out=sv[:, h, p_, 0:8], in_=src), [b_src], [b_sv])
                    S.op("dve", lambda e, src=src, h=h, p_=p_: e.match_replace(
                        out=mr[:, 0:128], in_to_replace=sv[:, h, p_, 0:8], in_values=src, imm_value=-3.0e38),
                        [b_src, b_sv], [b_mr])
                    S.op("dve", lambda e, h=h, p_=p_: e.max(out=sv[:, h, p_, 8:16], in_=mr[:, 0:128]), [b_mr], [b_sv])
            for h in range(8):
                TT(S, "dve", cand[:, h, :].rearrange("p (a b) -> p a b", a=16),
                   sv[:, h, 0, :].unsqueeze(2).to_broadcast([128, 16, 16]),
                   sv[:, h, 1, :].unsqueeze(1).to_broadcast([128, 16, 16]), ALU.add, [b_sv], [b_cand])
                S.op("dve", lambda e, h=h: e.max(out=cv[:, h, 0:8], in_=cand[:, h, :]), [b_cand], [b_cv])
                S.op("dve", lambda e, h=h: e.match_replace(out=mr[:], in_to_replace=cv[:, h, 0:8],
                                                            in_values=cand[:, h, :], imm_value=-3.0e38),
                     [b_cand, b_cv], [b_mr])
                S.op("dve", lambda e, h=h: e.max(out=cv[:, h, 8:16], in_=mr[:]), [b_mr], [b_cv])
            RED(S, "dve", sm[:, 0, :], cv[:], ALU.min, [b_cv], [b_sm])
            RED(S, "dve", sm[:, 1, :], cv[:], ALU.max, [b_cv], [b_sm])
            TS(S, "dve", sm[:, 2, :], sm[:, 1, :], -1.0, None, ALU.mult, R=[b_sm], W=[b_sm])
            S.op("dve", lambda e: e.memset(sm[:, 3, :], 0.0), [b_sm], [b_sm])
            for h in range(8):
                ACT(S, jk16[:], cv[:, h, :], AF.Exp, [b_cv, b_sm], [b_jk16, b_sm], bias=sm[:, 2, h:h + 1],
                    accum=sm[:, 3, h:h + 1])
            ACT(S, sm[:, 4, :], sm[:, 3, :], AF.Ln, [b_sm], [b_sm])
            STT(S, "dve", sm[:, 5, :], sm[:, 4, :], -1.0, sm[:, 2, :], ALU.mult, ALU.add, [b_sm], [b_sm])
            TS(S, "dve", sm[:, 0, :], sm[:, 0, :], -1.0e-5, None, ALU.add, R=[b_sm], W=[b_sm])
            thr_, b_thr = THR[i]
            TT(S, "dve", thr_[:], sm[:, 0, :].unsqueeze(2).to_broadcast([128, 8, 128]), s1[:], ALU.subtract,
               [b_sm, b_s1], [b_thr])
            TT(S, "dve", s1[:], s1[:], sm[:, 5, :].unsqueeze(2).to_broadcast([128, 8, 128]), ALU.add,
               [b_s1, b_sm], [b_s1])
        utf = [xs[0], xs[1]]
        vtf = [(tmp, b_tmp), (gtmp, b_gtmp)]
        utb = [S.sb("utb%d" % k, [128, KC * 128], BF16) for k in range(2)]
        vtb = [S.sb("vtb%d" % k, [128, K], BF16) for k in range(2)]
        ssb = [S.sb("ssb%d" % k, [128, TW], F32) for k in range(2)]
        g1_ = [S.sb("g1_%d" % k, [128, TW], F32) for k in range(2)]
        g2_ = [S.sb("g2_%d" % k, [128, TW], F32) for k in range(2)]
        PTs = [S.sb("PT%d" % k, [128, TW], BF16) for k in range(2)]
        exs = [S.sb("ex%d" % k, [128, 128], F32) for k in range(4)]
        wms = [S.sb("wm%d" % k, [128, 128], BF16) for k in range(4)]
        acc3 = accw[:].rearrange("p (i d) -> p i d", i=NTL)
        nw = 0
        npv = 0
        for sc in range(NCH_E // 2):
            for cc in range(2):
                c = 2 * sc + cc
                uf, b_uf = utf[cc]
                vf, b_vf = vtf[cc]
                ub, b_ub = utb[cc]
                vb, b_vb = vtb[cc]
                S.dma("sp", uf[:], UT[c], None, b_uf)
                S.dma("act", vf[:], V[c * 128:(c + 1) * 128, :], None, b_vf)
                CP(S, "pool", ub[:], uf[:], [b_uf], [b_ub])
                CP(S, "pool", vb[:], vf[:], [b_vf], [b_vb])
                su, b_su = PA[cc]
                at, b_at = PA[2 + cc]
                for kc in range(KC):
                    MM(S, su[:, 0:TW], ub[:, kc * 128:(kc + 1) * 128], hnT[:, kc, :], kc == 0, kc == KC - 1,
                       [b_ub, b_hnT], [b_su])
                s_sb, b_ssb = ssb[cc]
                ga, b_ga = g1_[cc]
                gb, b_gb = g2_[cc]
                CP(S, "act", s_sb[:], su[:, 0:TW], [b_su], [b_ssb])
                TT(S, "pool", ga[:], s_sb[:], s_sb[:], ALU.mult, [b_ssb], [b_ga])
                TS(S, "pool", ga[:], ga[:], 0.044715, 1.0, ALU.mult, ALU.add, R=[b_ga], W=[b_ga])
                TT(S, "pool", ga[:], ga[:], s_sb[:], ALU.mult, [b_ga, b_ssb], [b_ga])
                ACT(S, gb[:], ga[:], AF.Sigmoid, [b_ga], [b_gb], scale=GELU_C)
                TT(S, "pool", gb[:], gb[:], s_sb[:], ALU.mult, [b_gb, b_ssb], [b_gb])
                for i in range(NTL):
                    s2, b_s2 = s2s[i]
                    for h in range(8):
                        ex, b_ex = exs[nw % 4]
                        wm, b_wm = wms[nw % 4]
                        nw += 1
                        ACT(S, ex[:], s2[:, h, :], AF.Exp, [b_s2, BIA[i][1]], [b_ex], bias=BIA[i][0][:, h, c:c + 1])
                        STT(S, "dve", wm[:], s2[:, h, :], THR[i][0][:, h, c:c + 1], ex[:], ALU.is_ge, ALU.mult,
                            [b_s2, THR[i][1], b_ex], [b_wm])
                        MMG(S, at[:, i * 128:(i + 1) * 128], wm[:], idb[:], i == 0 and h == 0, [b_wm, b_idb], [b_at])
                pt_, b_pt = PTs[cc]
                TT(S, "dve", pt_[:], at[:, 0:TW], gb[:], ALU.mult, [b_at, b_gb], [b_pt])
            for i in range(NTL):
                for dq in range(4):
                    pv, b_pv = PV[npv % 4]
                    npv += 1
                    MM(S, pv[:], PTs[0][0][:, i * 128:(i + 1) * 128], vtb[0][0][:, dq * 512:(dq + 1) * 512], True, False,
                       [PTs[0][1], vtb[0][1]], [b_pv])
                    MM(S, pv[:], PTs[1][0][:, i * 128:(i + 1) * 128], vtb[1][0][:, dq * 512:(dq + 1) * 512], False, True,
                       [PTs[1][1], vtb[1][1]], [b_pv])
                    dst = acc3[:, i, dq * 512:(dq + 1) * 512]
                    if sc == 0:
                        CP(S, "dve", dst, pv[:], [b_pv], [b_accw])
                    else:
                        TT(S, "dve", dst, dst, pv[:], ALU.add, [b_accw, b_pv], [b_accw])
        gate, b_gate = A, b_A
        S.dma("sp", gate[:], GATE, None, b_gate)
        for i in range(NTL):
            x, b_x = xs[i % 2]
            S.dma("sp", x[:], X[i * 128:(i + 1) * 128, :], None, b_x)
            TT(S, "dve", acc3[:, i, :], acc3[:, i, :], gate[:], ALU.mult, [b_accw, b_gate], [b_accw])
            TT(S, "pool", x[:], x[:], acc3[:, i, :], ALU.add, [b_x, b_accw], [b_x])
            S.dma("pool", Y[i * 128:(i + 1) * 128, :], x[:], b_x, None, sem_on="src")
        S.finish([(b.dsem, b.dcnt) for _, b in xs])
    return nc


def peer_weights(peer_wq, peer_subkeys, peer_u):
    wq = np.ascontiguousarray(np.transpose(peer_wq.reshape(16, 128, 1024), (1, 0, 2))).reshape(128, 16 * 1024)
    skT = np.ascontiguousarray(np.transpose(peer_subkeys, (1, 3, 0, 2))).reshape(128, 1024)
    UT = np.ascontiguousarray(np.transpose(peer_u.reshape(128, 128, 16, 128), (0, 3, 2, 1))).reshape(128, 128, 2048)
    return wq, skT, UT


def run_peer(x1, norm2_g, sc2, sh2, g2, wq, skT, UT, peer_v, NTL=4):
    Ttot = x1.shape[0]
    nc = _get(("peer", NTL), lambda: build_peer(NTL))
    T = NTL * 128
    per = T * NCORES
    outs = []
    pg, psc, psh, gate = rep128(norm2_g), rep128(sc2), rep128(sh2), rep128(g2)
    for r0 in range(0, Ttot, per):
        maps = []
        for c in range(NCORES):
            maps.append({"X": np.ascontiguousarray(x1[r0 + c * T:r0 + (c + 1) * T]), "pg": pg, "psc": psc, "psh": psh,
                         "gate": gate, "wq": wq, "skT": skT, "UT": UT, "V": peer_v, "ident": IDENT})
        res = _run(nc, maps)
        outs.append(np.concatenate([r["Y"] for r in res], axis=0))
    return np.concatenate(outs, axis=0)


def run_ada(c, ada_w, ada_b):
    L, K, N = ada_w.shape
    NC = L * N // NCORES
    nc = _get(("ada", K, NC), lambda: build_gemm(128, K, NC, False, True, silu=True))
    X = np.ascontiguousarray(np.broadcast_to(c.reshape(1, K), (128, K))).astype(np.float32)
    ones = np.ones((128, NC), np.float32)
    maps = []
    for core in range(NCORES):
        l, c0 = divmod(core * NC, N)
        maps.append({"X": X, "W": np.ascontiguousarray(ada_w[l][:, c0:c0 + NC]), "ident": IDENT,
                     "resid": rep128(ada_b[l][c0:c0 + NC]), "gate": ones})
    res = _run(nc, maps)
    flat = np.concatenate([r["Y"][0] for r in res])
    return flat.reshape(L, N)


def kernel(x, c, positions, ada_w, ada_b, norm1_g, norm2_g, w_in, w_out,
           conv_w, conv_b, conv_ln_g, conv_ln_b, sgu_ln_g, sgu_ln_b, sgu_w, sgu_b,
           nsa_q_g, nsa_k_g, nsa_cmp_pos, nsa_cmp_w1, nsa_cmp_w2, dsa_q_g, dsa_k_g,
           peer_wq, peer_subkeys, peer_u, peer_v):
    f = lambda a: np.asarray(a, dtype=np.float32)
    x = f(x)[0]
    pos = np.asarray(positions)[0]
    ada = run_ada(f(c)[0], f(ada_w), f(ada_b))
    for i in range(ada.shape[0]):
        sh1, sc1, g1, sh2, sc2, g2 = np.split(ada[i], 6)
        proj = gemm(x, f(w_in[i]), pro=(rep128(f(norm1_g[i])), rep128(sc1), rep128(sh1)))
        ya = run_conv(proj[:, 0:1024], f(conv_w[i]), f(conv_b[i]), f(conv_ln_g[i]), f(conv_ln_b[i]))
        yb = run_sgu(proj[:, 1024:2048], f(sgu_ln_g[i]), f(sgu_ln_b[i]), f(sgu_w[i]), f(sgu_b[i]))
        pc = proj[:, 2048:2048 + 1292]
        pd = proj[:, 2048 + 1292:]
        Yp = run_prep(proj[:, 2048:], pos, f(nsa_q_g[i]), f(nsa_k_g[i]), f(dsa_q_g[i]), f(dsa_k_g[i]))
        yc = run_nsa(Yp, pc, f(nsa_cmp_pos[i]), f(nsa_cmp_w1[i]), f(nsa_cmp_w2[i]), f(nsa_k_g[i][0]))
        yd = run_dsa(Yp, np.ascontiguousarray(pd[:, 640:768]))
        ycat = np.concatenate([ya, yb, yc, yd], axis=1)
        x1 = gemm(ycat, f(w_out[i]), epi=(x, rep128(g1)))
        wq, skT, UT = peer_weights(f(peer_wq[i]), f(peer_subkeys[i]), f(peer_u[i]))
        x = run_peer(x1, f(norm2_g[i]), sc2, sh2, g2, wq, skT, UT, f(peer_v[i]))
    return x[None].astype(np.float32)
```

```python
import contextlib
import os
import numpy as np
import concourse.bass as bass
import concourse.mybir as mybir
from concourse.bass_utils import run_bass_kernel_spmd

F32 = mybir.dt.float32
BF16 = mybir.dt.bfloat16
ALU = mybir.AluOpType
AF = mybir.ActivationFunctionType
AX = mybir.AxisListType
NCORES = 8
EPS = 1e-6


class Buf:
    __slots__ = ("name", "w", "r", "dsem", "dcnt")

    def __init__(self, name):
        self.name = name
        self.w = None
        self.r = []
        self.dsem = None
        self.dcnt = 0


class Sched:
    ENG = ("pe", "act", "dve", "pool", "sp")

    def __init__(self, nc, es):
        self.nc = nc
        self.es = es
        self.sem = {e: es.enter_context(nc.semaphore("sem_" + e)) for e in self.ENG}
        self.cnt = {e: 0 for e in self.ENG}
        self.waited = {e: {} for e in self.ENG}
        self.prog = {e: [] for e in self.ENG}
        self.bufs = []
        self.nsem = 0

    def buf(self, name):
        b = Buf(name)
        self.bufs.append(b)
        return b

    def sb(self, name, shape, dtype):
        t = self.es.enter_context(self.nc.sbuf_tensor("sb_" + name, list(shape), dtype))
        return t, self.buf(name)

    def ps(self, name, shape, dtype=F32):
        t = self.es.enter_context(self.nc.psum_tensor("ps_" + name, list(shape), dtype))
        return t, self.buf(name)

    def _dsem(self, b):
        if b.dsem is None:
            b.dsem = self.es.enter_context(self.nc.semaphore("d_%d_%s" % (self.nsem, b.name)))
            self.nsem += 1
        return b.dsem

    def _waits(self, eng, reads, writes, skip_self_w=False):
        need = {}
        for b in reads:
            if b.w is not None:
                need[b.w[0]] = max(need.get(b.w[0], 0), b.w[1])
        for b in writes:
            if b.w is not None and not (skip_self_w and b.w[0] is self.sem[eng]):
                need[b.w[0]] = max(need.get(b.w[0], 0), b.w[1])
            for tok in b.r:
                need[tok[0]] = max(need.get(tok[0], 0), tok[1])
        out = []
        wd = self.waited[eng]
        for s, v in need.items():
            if wd.get(s, 0) < v:
                wd[s] = v
                out.append((s, v))
        return out

    def op(self, eng, fn, reads=(), writes=(), accumulate=False):
        waits = self._waits(eng, reads, writes, skip_self_w=accumulate)
        self.cnt[eng] += 1
        tok = (self.sem[eng], self.cnt[eng])
        self.prog[eng].append((waits, fn, (self.sem[eng], 1)))
        for b in reads:
            b.r.append(tok)
        for b in writes:
            b.w = tok
            b.r = []
        return tok

    def dma(self, eng, out_ap, in_ap, src, dst, sem_on="dst", **kw):
        reads = [src] if src is not None else []
        writes = [dst] if dst is not None else []
        waits = self._waits(eng, reads, writes)
        sb = dst if sem_on == "dst" else src
        sem = self._dsem(sb)
        sb.dcnt += 16
        tok = (sem, sb.dcnt)

        def fn(e, out_ap=out_ap, in_ap=in_ap, kw=kw):
            return e.dma_start(out=out_ap, in_=in_ap, **kw)

        self.prog[eng].append((waits, fn, (sem, 16)))
        if src is not None:
            src.r.append(tok)
        if dst is not None:
            dst.w = tok
            dst.r = []
        return tok

    def finish(self, final_tokens):
        nc = self.nc
        need = {}
        for s, v in final_tokens:
            if s is None:
                continue
            need[s] = max(need.get(s, 0), v)
        fin = list(need.items())
        with nc.Block() as block:
            def mk(ename):
                def body(e):
                    for waits, fn, inc in self.prog[ename]:
                        for s, v in waits:
                            e.wait_ge(s, v)
                        ins = fn(e)
                        ins.then_inc(inc[0], inc[1])
                    if ename == "sp":
                        for s, v in fin:
                            e.wait_ge(s, v)
                return body
            block.tensor(mk("pe"))
            block.scalar(mk("act"))
            block.vector(mk("dve"))
            block.gpsimd(mk("pool"))
            block.sync(mk("sp"))


def _run(nc, in_maps):
    if os.environ.get("KTRACE"):
        res = run_bass_kernel_spmd(nc, in_maps, core_ids=list(range(NCORES)), trace=True)
        print("KTRACE exec_time_ns", res.exec_time_ns, flush=True)
        return res.results
    res = run_bass_kernel_spmd(nc, in_maps, core_ids=list(range(NCORES)))
    return res.results


def rep128(v):
    v = np.asarray(v, dtype=np.float32).reshape(1, -1)
    return np.ascontiguousarray(np.broadcast_to(v, (128, v.shape[1])))


IDENT = np.eye(128, dtype=np.float32)


_GEMM_CACHE = {}


def build_gemm(T, K, N, prologue, epilogue, silu=False):
    nc = bass.Bass("TRN2", target_bir_lowering=False)
    NT = T // 128
    KC = K // 128
    CG = 512
    ncg = (N + CG - 1) // CG
    X = nc.dram_tensor("X", [T, K], F32, kind="ExternalInput").ap()
    W = nc.dram_tensor("W", [K, N], F32, kind="ExternalInput").ap()
    ID = nc.dram_tensor("ident", [128, 128], F32, kind="ExternalInput").ap()
    if prologue:
        G = nc.dram_tensor("pg", [128, K], F32, kind="ExternalInput").ap()
        SC = nc.dram_tensor("psc", [128, K], F32, kind="ExternalInput").ap()
        SH = nc.dram_tensor("psh", [128, K], F32, kind="ExternalInput").ap()
    if epilogue:
        R = nc.dram_tensor("resid", [T, N], F32, kind="ExternalInput").ap()
        GT = nc.dram_tensor("gate", [128, N], F32, kind="ExternalInput").ap()
    Y = nc.dram_tensor("Y", [T, N], F32, kind="ExternalOutput").ap()
    Wv = W.rearrange("(kc p) n -> p kc n", p=128)

    with contextlib.ExitStack() as es:
        S = Sched(nc, es)
        idf, b_idf = S.sb("idf", [128, 128], F32)
        idb, b_idb = S.sb("idb", [128, 128], BF16)
        S.dma("sp", idf[:], ID, None, b_idf)
        S.op("dve", lambda e: e.tensor_copy(out=idb[:], in_=idf[:]), [b_idf], [b_idb])
        if prologue:
            A, b_A = S.sb("A", [128, K], F32)
            B, b_B = S.sb("B", [128, K], F32)
            gt_, b_gt = S.sb("gtmp", [128, K], F32)
            S.dma("sp", A[:], SC, None, b_A)
            S.dma("sp", gt_[:], G, None, b_gt)
            S.dma("sp", B[:], SH, None, b_B)
            S.op("dve", lambda e: e.scalar_tensor_tensor(out=A[:], in0=A[:], scalar=1.0, in1=gt_[:],
                                                         op0=ALU.add, op1=ALU.mult), [b_A, b_gt], [b_A])
        if epilogue:
            gate, b_gate = S.sb("gate", [128, N], F32)
            S.dma("sp", gate[:], GT, None, b_gate)
        hnT, b_hnT = [], []
        for i in range(NT):
            t, b = S.sb("hnT%d" % i, [128, KC, 128], BF16)
            hnT.append(t)
            b_hnT.append(b)
        xs = [S.sb("x%d" % j, [128, K], F32) for j in range(2)]
        hb = [S.sb("hb%d" % j, [128, K], BF16) for j in range(2)]
        tmp, b_tmp = S.sb("tmp", [128, K], F32)
        st = [S.sb("st%d" % j, [128, 4], F32) for j in range(2)]
        pT = [S.ps("pT%d" % j, [128, 512], BF16) for j in range(2)]
        for i in range(NT):
            x, b_x = xs[i % 2]
            h, b_h = hb[i % 2]
            s_, b_s = st[i % 2]
            S.dma("sp", x[:], X[i * 128:(i + 1) * 128, :], None, b_x)
            if prologue:
                S.op("act", lambda e, x=x, s_=s_: e.activation(out=tmp[:], in_=x[:], func=AF.Square,
                                                               accum_out=s_[:, 0:1]), [b_x], [b_tmp, b_s])
                S.op("dve", lambda e, s_=s_: e.tensor_scalar(out=s_[:, 1:2], in0=s_[:, 0:1], scalar1=1.0 / K,
                                                             scalar2=EPS, op0=ALU.mult, op1=ALU.add), [b_s], [b_s])
                S.op("dve", lambda e, s_=s_: e.reciprocal(out=s_[:, 3:4], in_=s_[:, 1:2]), [b_s], [b_s])
                S.op("act", lambda e, s_=s_: e.sqrt(out=s_[:, 2:3], in_=s_[:, 3:4]), [b_s], [b_s])
                S.op("dve", lambda e, x=x, s_=s_: e.scalar_tensor_tensor(out=tmp[:], in0=x[:], scalar=s_[:, 2:3],
                                                                         in1=A[:], op0=ALU.mult, op1=ALU.mult),
                     [b_x, b_s, b_A], [b_tmp])
                S.op("dve", lambda e, h=h: e.tensor_tensor(out=h[:], in0=tmp[:], in1=B[:], op=ALU.add),
                     [b_tmp, b_B], [b_h])
            elif silu:
                S.op("act", lambda e, x=x: e.activation(out=tmp[:], in_=x[:], func=AF.Sigmoid), [b_x], [b_tmp])
                S.op("dve", lambda e, x=x, h=h: e.tensor_tensor(out=h[:], in0=x[:], in1=tmp[:], op=ALU.mult),
                     [b_x, b_tmp], [b_h])
            else:
                S.op("dve", lambda e, x=x, h=h: e.tensor_copy(out=h[:], in_=x[:]), [b_x], [b_h])
            for q in range(KC // 4):
                p, b_p = pT[q % 2]
                for j in range(4):
                    kc = q * 4 + j
                    S.op("pe", lambda e, p=p, h=h, kc=kc, j=j: e.transpose(
                        out=p[:, j * 128:(j + 1) * 128], in_=h[:, kc * 128:(kc + 1) * 128], identity=idb[:]),
                        [b_h, b_idb], [b_p])
                eng = "act" if q % 2 == 0 else "dve"
                if eng == "act":
                    S.op("act", lambda e, p=p, i=i, q=q: e.copy(
                        out=hnT[i][:, q * 4:(q + 1) * 4, :], in_=p[:].rearrange("p (a b) -> p a b", a=4)),
                        [b_p], [b_hnT[i]])
                else:
                    S.op("dve", lambda e, p=p, i=i, q=q: e.tensor_copy(
                        out=hnT[i][:, q * 4:(q + 1) * 4, :], in_=p[:].rearrange("p (a b) -> p a b", a=4)),
                        [b_p], [b_hnT[i]])
        wb = [S.sb("wb%d" % j, [128, KC, CG], BF16) for j in range(2)]
        po = [S.ps("po%d" % j, [128, CG], F32) for j in range(2)]
        ot = [S.sb("ot%d" % j, [128, CG], F32) for j in range(3)]
        if epilogue:
            rt = [S.sb("rt%d" % j, [128, CG], F32) for j in range(2)]
        n_out = 0
        for cg in range(ncg):
            c0 = cg * CG
            cw = min(CG, N - c0)
            w_b, b_wb = wb[cg % 2]
            half = KC // 2
            S.dma("pool", w_b[:, 0:half, 0:cw], Wv[:, 0:half, c0:c0 + cw], None, b_wb)
            S.dma("pool", w_b[:, half:KC, 0:cw], Wv[:, half:KC, c0:c0 + cw], None, b_wb)
            for i in range(NT):
                p, b_p = po[n_out % 2]
                o, b_o = ot[n_out % 3]
                if epilogue:
                    r, b_r = rt[n_out % 2]
                    S.dma("sp", r[:, 0:cw], R[i * 128:(i + 1) * 128, c0:c0 + cw], None, b_r)
                for kc in range(KC):
                    S.op("pe", lambda e, p=p, i=i, kc=kc, w_b=w_b, cw=cw: e.matmul(
                        p[:, 0:cw], hnT[i][:, kc, :], w_b[:, kc, 0:cw], start=(kc == 0), stop=(kc == KC - 1)),
                        [b_hnT[i], b_wb], [b_p], accumulate=(kc > 0))
                if epilogue:
                    S.op("dve", lambda e, o=o, p=p, c0=c0, cw=cw: e.tensor_tensor(
                        out=o[:, 0:cw], in0=p[:, 0:cw], in1=gate[:, c0:c0 + cw], op=ALU.mult),
                        [b_p, b_gate], [b_o])
                    S.op("pool", lambda e, o=o, r=r, cw=cw: e.tensor_tensor(
                        out=o[:, 0:cw], in0=o[:, 0:cw], in1=r[:, 0:cw], op=ALU.add), [b_o, b_r], [b_o])
                else:
                    if n_out % 2 == 0:
                        S.op("act", lambda e, o=o, p=p, cw=cw: e.copy(out=o[:, 0:cw], in_=p[:, 0:cw]),
                             [b_p], [b_o])
                    else:
                        S.op("dve", lambda e, o=o, p=p, cw=cw: e.tensor_copy(out=o[:, 0:cw], in_=p[:, 0:cw]),
                             [b_p], [b_o])
                S.dma("pool", Y[i * 128:(i + 1) * 128, c0:c0 + cw], o[:, 0:cw], b_o, None, sem_on="src")
                n_out += 1
        fin = [(b.dsem, b.dcnt) for _, b in ot]
        S.finish(fin)
    return nc


def gemm(X, W, pro=None, epi=None):
    Ttot, K = X.shape
    N = W.shape[1]
    T = Ttot // NCORES
    key = (T, K, N, pro is not None, epi is not None)
    if key not in _GEMM_CACHE:
        _GEMM_CACHE[key] = build_gemm(T, K, N, pro is not None, epi is not None)
    nc = _GEMM_CACHE[key]
    maps = []
    for c in range(NCORES):
        m = {"X": np.ascontiguousarray(X[c * T:(c + 1) * T]), "W": W, "ident": IDENT}
        if pro is not None:
            m["pg"], m["psc"], m["psh"] = pro
        if epi is not None:
            m["resid"] = np.ascontiguousarray(epi[0][c * T:(c + 1) * T])
            m["gate"] = epi[1]
        maps.append(m)
    res = _run(nc, maps)
    return np.concatenate([r["Y"] for r in res], axis=0)


def TT(S, eng, out, in0, in1, op, R, W):
    return S.op(eng, lambda e: e.tensor_tensor(out=out, in0=in0, in1=in1, op=op), R, W)


def TS(S, eng, out, in0, s1, s2, op0, op1=None, R=(), W=(), accum=None):
    if op1 is None:
        return S.op(eng, lambda e: e.tensor_scalar(out=out, in0=in0, scalar1=s1, scalar2=None, op0=op0), R, W)
    if accum is None:
        return S.op(eng, lambda e: e.tensor_scalar(out=out, in0=in0, scalar1=s1, scalar2=s2, op0=op0, op1=op1), R, W)
    return S.op(eng, lambda e: e.tensor_scalar(out=out, in0=in0, scalar1=s1, scalar2=s2, op0=op0, op1=op1,
                                               accum_out=accum), R, W)


def STT(S, eng, out, in0, scalar, in1, op0, op1, R, W):
    return S.op(eng, lambda e: e.scalar_tensor_tensor(out=out, in0=in0, scalar=scalar, in1=in1, op0=op0, op1=op1),
                R, W)


def ACT(S, out, in_, func, R, W, bias=None, scale=1.0, accum=None):
    kw = {}
    if bias is not None:
        kw["bias"] = bias
    if accum is not None:
        kw["accum_out"] = accum
    return S.op("act", lambda e: e.activation(out=out, in_=in_, func=func, scale=scale, **kw), R, W)


def CP(S, eng, out, in_, R, W):
    if eng == "act":
        return S.op("act", lambda e: e.copy(out=out, in_=in_), R, W)
    return S.op(eng, lambda e: e.tensor_copy(out=out, in_=in_), R, W)


def MM(S, out, lhsT, rhs, start, stop, R, W):
    return S.op("pe", lambda e: e.matmul(out, lhsT, rhs, start=start, stop=stop), R, W, accumulate=not start)


def TR(S, out, in_, ident, R, W, acc=False):
    return S.op("pe", lambda e: e.transpose(out=out, in_=in_, identity=ident), R, W, accumulate=acc)


def RED(S, eng, out, in_, op, R, W):
    return S.op(eng, lambda e: e.tensor_reduce(out=out, in_=in_, axis=AX.X, op=op), R, W)


def RSTD(S, out, tmp, ms, R, W):
    S.op("dve", lambda e: e.reciprocal(out=tmp, in_=ms), R, W)
    S.op("act", lambda e: e.sqrt(out=out, in_=tmp), W, W)


def load_const(S, name, dram_ap, shape, dtype=F32, q="sp", cast=None):
    t, b = S.sb(name, shape, dtype)
    S.dma(q, t[:], dram_ap, None, b)
    if cast is not None:
        t2, b2 = S.sb(name + "_c", shape, cast)
        CP(S, "dve", t2[:], t[:], [b], [b2])
        return t2, b2
    return t, b


def layer_norm_rows(S, pfx, dst, src, src_bufs, dst_bufs, D, g_t, b_t, g_b, b_b, st, b_st, junk, b_junk, tmp, b_tmp):
    ACT(S, tmp, src, AF.Identity, src_bufs + [b_st], [b_tmp, b_st], accum=st[:, 0:1])
    ACT(S, junk, src, AF.Square, src_bufs + [b_st], [b_junk, b_st], accum=st[:, 1:2])
    TS(S, "dve", st[:, 2:3], st[:, 0:1], 1.0 / D, None, ALU.mult, R=[b_st], W=[b_st])
    TT(S, "dve", st[:, 3:4], st[:, 2:3], st[:, 2:3], ALU.mult, [b_st], [b_st])
    STT(S, "dve", st[:, 4:5], st[:, 1:2], 1.0 / D, st[:, 3:4], ALU.mult, ALU.subtract, [b_st], [b_st])
    TS(S, "dve", st[:, 4:5], st[:, 4:5], EPS, None, ALU.add, R=[b_st], W=[b_st])
    RSTD(S, st[:, 5:6], st[:, 6:7], st[:, 4:5], [b_st], [b_st])
    TS(S, "dve", tmp, tmp, st[:, 2:3], st[:, 5:6], ALU.subtract, ALU.mult, R=[b_tmp, b_st], W=[b_tmp])
    TT(S, "dve", tmp, tmp, g_t, ALU.mult, [b_tmp, g_b], [b_tmp])
    TT(S, "dve", dst, tmp, b_t, ALU.add, [b_tmp, b_b], dst_bufs)


def build_conv(T):
    nc = bass.Bass("TRN2", target_bir_lowering=False)
    NT = T // 128
    TH = T + 32
    AT = nc.dram_tensor("aT", [4, 128, TH], F32, kind="ExternalInput").ap()
    GT = nc.dram_tensor("gT", [4, 128, TH], F32, kind="ExternalInput").ap()
    CW = nc.dram_tensor("cw", [4, 128, 32], F32, kind="ExternalInput").ap()
    LG = nc.dram_tensor("lng", [128, 512], F32, kind="ExternalInput").ap()
    LB = nc.dram_tensor("lnb", [128, 512], F32, kind="ExternalInput").ap()
    ID = nc.dram_tensor("ident", [128, 128], F32, kind="ExternalInput").ap()
    Y = nc.dram_tensor("Y", [T, 512], F32, kind="ExternalOutput").ap()
    with contextlib.ExitStack() as es:
        S = Sched(nc, es)
        idf, b_idf = load_const(S, "idf", ID, [128, 128])
        lg, b_lg = load_const(S, "lg", LG, [128, 512])
        lb, b_lb = load_const(S, "lb", LB, [128, 512])
        pz = [S.ps("pz%d" % i, [128, 512], F32) for i in range(NT)]
        for cc in range(4):
            a, b_a = S.sb("a%d" % cc, [128, TH], F32)
            g, b_g = S.sb("g%d" % cc, [128, TH], F32)
            w, b_w = S.sb("w%d" % cc, [128, 32], F32)
            S.dma("sp", a[:], AT[cc], None, b_a)
            S.dma("act", g[:], GT[cc], None, b_g)
            S.dma("sp", w[:], CW[cc], None, b_w)
            ACT(S, g[:], g[:], AF.Sigmoid, [b_g], [b_g])
            TT(S, "dve", a[:], a[:], g[:], ALU.mult, [b_a, b_g], [b_a])
            accA, b_accA = S.sb("accA%d" % cc, [128, T], F32)
            TS(S, "dve", accA[:], a[:, 2:2 + T], w[:, 0:1], w[:, 31:32], ALU.mult, ALU.add, R=[b_a, b_w], W=[b_accA])
            for k in range(1, 31):
                STT(S, "dve", accA[:], a[:, 2 + k:2 + k + T], w[:, k:k + 1], accA[:], ALU.mult, ALU.add,
                    [b_a, b_w, b_accA], [b_accA])
            for i in range(NT):
                TR(S, pz[i][0][:, cc * 128:(cc + 1) * 128], accA[:, i * 128:(i + 1) * 128], idf[:],
                   [b_accA, b_idf], [pz[i][1]], acc=(cc > 0))
        outs = [S.sb("o%d" % j, [128, 512], F32) for j in range(2)]
        tmps = [S.sb("t%d" % j, [128, 512], F32) for j in range(2)]
        junk, b_junk = S.sb("junk", [128, 512], F32)
        sts = [S.sb("st%d" % j, [128, 8], F32) for j in range(2)]
        for i in range(NT):
            o, b_o = outs[i % 2]
            t, b_t = tmps[i % 2]
            st, b_st = sts[i % 2]
            layer_norm_rows(S, "c", t[:], pz[i][0][:], [pz[i][1]], [b_t], 512, lg[:], lb[:], b_lg, b_lb,
                            st, b_st, junk[:], b_junk, t[:], b_t)
            ACT(S, junk[:], t[:], AF.Sigmoid, [b_t], [b_junk])
            TT(S, "dve", o[:], t[:], junk[:], ALU.mult, [b_t, b_junk], [b_o])
            S.dma("pool", Y[i * 128:(i + 1) * 128, :], o[:], b_o, None, sem_on="src")
        S.finish([(b.dsem, b.dcnt) for _, b in outs])
    return nc


def build_sgu(T):
    nc = bass.Bass("TRN2", target_bir_lowering=False)
    NT = T // 128
    U = nc.dram_tensor("u", [T, 512], F32, kind="ExternalInput").ap()
    V = nc.dram_tensor("v", [T, 512], F32, kind="ExternalInput").ap()
    WT = nc.dram_tensor("wT", [128, 4, 128], F32, kind="ExternalInput").ap()
    MK = nc.dram_tensor("mask", [128, 4, 128], F32, kind="ExternalInput").ap()
    BS = nc.dram_tensor("bs", [128, 4], F32, kind="ExternalInput").ap()
    LG = nc.dram_tensor("lng", [128, 512], F32, kind="ExternalInput").ap()
    LB = nc.dram_tensor("lnb", [128, 512], F32, kind="ExternalInput").ap()
    Y = nc.dram_tensor("Y", [T, 512], F32, kind="ExternalOutput").ap()
    with contextlib.ExitStack() as es:
        S = Sched(nc, es)
        lg, b_lg = load_const(S, "lg", LG, [128, 512])
        lb, b_lb = load_const(S, "lb", LB, [128, 512])
        wt, b_wt = load_const(S, "wt", WT, [128, 4, 128])
        mk, b_mk = load_const(S, "mk", MK, [128, 4, 128])
        bs, b_bs = load_const(S, "bs", BS, [128, 4])
        wm, b_wm = S.sb("wm", [128, 4, 128], BF16)
        TT(S, "dve", wm[:], wt[:], mk[:], ALU.mult, [b_wt, b_mk], [b_wm])
        us = [S.sb("u%d" % j, [128, 512], F32) for j in range(2)]
        vs = [S.sb("v%d" % j, [128, 512], F32) for j in range(2)]
        vn = [S.sb("vn%d" % j, [128, 512], BF16) for j in range(2)]
        tmps = [S.sb("t%d" % j, [128, 512], F32) for j in range(2)]
        outs = [S.sb("o%d" % j, [128, 512], F32) for j in range(2)]
        junk, b_junk = S.sb("junk", [128, 512], F32)
        sts = [S.sb("st%d" % j, [128, 8], F32) for j in range(2)]
        pss = [S.ps("ps%d" % j, [128, 512], F32) for j in range(2)]
        for i in range(NT):
            u, b_u = us[i % 2]
            v, b_v = vs[i % 2]
            n, b_n = vn[i % 2]
            t, b_t = tmps[i % 2]
            o, b_o = outs[i % 2]
            st, b_st = sts[i % 2]
            ps, b_ps = pss[i % 2]
            S.dma("sp", u[:], U[i * 128:(i + 1) * 128, :], None, b_u)
            S.dma("act", v[:], V[i * 128:(i + 1) * 128, :], None, b_v)
            layer_norm_rows(S, "s", n[:], v[:], [b_v], [b_n], 512, lg[:], lb[:], b_lg, b_lb,
                            st, b_st, junk[:], b_junk, t[:], b_t)
            for h in range(4):
                MM(S, ps[:, h * 128:(h + 1) * 128], wm[:, h, :], n[:, h * 128:(h + 1) * 128], True, True,
                   [b_wm, b_n], [b_ps])
            for h in range(4):
                STT(S, "dve", o[:, h * 128:(h + 1) * 128], ps[:, h * 128:(h + 1) * 128], bs[:, h:h + 1],
                    u[:, h * 128:(h + 1) * 128], ALU.add, ALU.mult, [b_ps, b_bs, b_u], [b_o])
            S.dma("pool", Y[i * 128:(i + 1) * 128, :], o[:], b_o, None, sem_on="src")
        S.finish([(b.dsem, b.dcnt) for _, b in outs])
    return nc


_CACHE = {}


def _get(key, fn):
    if key not in _CACHE:
        _CACHE[key] = fn()
    return _CACHE[key]


def run_conv(pa, conv_w, conv_b, ln_g, ln_b):
    Ttot = pa.shape[0]
    T = Ttot // NCORES
    nc = _get(("conv", T), lambda: build_conv(T))
    aT = np.zeros((512, Ttot + 32), np.float32)
    gT = np.zeros((512, Ttot + 32), np.float32)
    aT[:, 32:] = pa[:, 0:512].T
    gT[:, 32:] = pa[:, 512:1024].T
    cw = np.zeros((512, 32), np.float32)
    cw[:, 0:31] = conv_w.T
    cw[:, 31] = conv_b
    cw = cw.reshape(4, 128, 32)
    lg, lb = rep128(ln_g), rep128(ln_b)
    maps = []
    for c in range(NCORES):
        maps.append({"aT": np.ascontiguousarray(aT[:, c * T:c * T + T + 32]).reshape(4, 128, T + 32),
                     "gT": np.ascontiguousarray(gT[:, c * T:c * T + T + 32]).reshape(4, 128, T + 32),
                     "cw": cw, "lng": lg, "lnb": lb, "ident": IDENT})
    res = _run(nc, maps)
    return np.concatenate([r["Y"] for r in res], axis=0)


def run_sgu(pb, ln_g, ln_b, sgu_w, sgu_b):
    Ttot = pb.shape[0]
    T = Ttot // NCORES
    nc = _get(("sgu", T), lambda: build_sgu(T))
    wT = np.ascontiguousarray(np.transpose(sgu_w, (2, 0, 1)))
    jj = np.arange(128)
    mask = np.ascontiguousarray(np.broadcast_to((jj[:, None] <= jj[None, :]).astype(np.float32)[:, None, :],
                                                (128, 4, 128)))
    bs = np.ascontiguousarray(sgu_b.T)
    lg, lb = rep128(ln_g), rep128(ln_b)
    maps = []
    for c in range(NCORES):
        maps.append({"u": np.ascontiguousarray(pb[c * T:(c + 1) * T, 0:512]),
                     "v": np.ascontiguousarray(pb[c * T:(c + 1) * T, 512:1024]),
                     "wT": wT, "mask": mask, "bs": bs, "lng": lg, "lnb": lb})
    res = _run(nc, maps)
    return np.concatenate([r["Y"] for r in res], axis=0)


PREP_IN = 2644
PREP_OUT = 2516
I32 = mybir.dt.int32
TWO_PI = float(2.0 * np.pi)


def _rope_inplace(S, Y, b_Y, H, half, c, s, b_sn, tmp, b_tmp):
    Y1 = Y[:, :, 0:half]
    Y2 = Y[:, :, half:2 * half]
    cb = c.unsqueeze(1).to_broadcast([128, H, half])
    sb_ = s.unsqueeze(1).to_broadcast([128, H, half])
    t = [tmp[:, k, 0:H * half].rearrange("p (h d) -> p h d", h=H) for k in range(4)]
    TT(S, "dve", t[0], Y1, cb, ALU.mult, [b_Y, b_sn], [b_tmp])
    TT(S, "dve", t[1], Y2, sb_, ALU.mult, [b_Y, b_sn], [b_tmp])
    TT(S, "dve", t[2], Y2, cb, ALU.mult, [b_Y, b_sn], [b_tmp])
    TT(S, "dve", t[3], Y1, sb_, ALU.mult, [b_Y, b_sn], [b_tmp])
    TT(S, "dve", Y1, t[0], t[1], ALU.subtract, [b_tmp], [b_Y])
    TT(S, "dve", Y2, t[2], t[3], ALU.add, [b_tmp], [b_Y])


def build_prep(T):
    nc = bass.Bass("TRN2", target_bir_lowering=False)
    NT = T // 128
    X = nc.dram_tensor("X", [T, PREP_IN], F32, kind="ExternalInput").ap()
    POS = nc.dram_tensor("pos", [T, 1], I32, kind="ExternalInput").ap()
    GN = nc.dram_tensor("gn", [128, 11, 128], F32, kind="ExternalInput").ap()
    INV = nc.dram_tensor("inv", [128, 48], F32, kind="ExternalInput").ap()
    PH = nc.dram_tensor("ph", [128, 48], F32, kind="ExternalInput").ap()
    Y = nc.dram_tensor("Y", [T, PREP_OUT], F32, kind="ExternalOutput").ap()
    with contextlib.ExitStack() as es:
        S = Sched(nc, es)
        gn, b_gn = load_const(S, "gn", GN, [128, 11, 128])
        inv, b_inv = load_const(S, "inv", INV, [128, 48])
        ph, b_ph = load_const(S, "ph", PH, [128, 48])
        xs = [S.sb("x%d" % j, [128, PREP_IN], F32) for j in range(2)]
        os_ = [S.sb("o%d" % j, [128, PREP_OUT], F32) for j in range(2)]
        pis = [S.sb("pi%d" % j, [128, 1], I32) for j in range(2)]
        sq, b_sq = S.sb("sq", [128, 5, 128], F32)
        st, b_st = S.sb("st", [128, 4, 8], F32)
        ang, b_ang = S.sb("ang", [128, 4, 48], F32)
        ki, b_ki = S.sb("ki", [128, 48], I32)
        sn, b_sn = S.sb("sn", [128, 48], F32)
        rt, b_rt = S.sb("rt", [128, 4, 96], F32)
        for i in range(NT):
            x, b_x = xs[i % 2]
            o, b_o = os_[i % 2]
            pi_, b_pi = pis[i % 2]
            S.dma("sp", x[:], X[i * 128:(i + 1) * 128, :], None, b_x)
            S.dma("act", pi_[:], POS[i * 128:(i + 1) * 128, :], None, b_pi)
            CP(S, "dve", ang[:, 3, 0:1], pi_[:], [b_pi], [b_ang])
            STT(S, "dve", ang[:, 0, :], inv[:], ang[:, 3, 0:1], ph[:], ALU.mult, ALU.add, [b_inv, b_ang, b_ph], [b_ang])
            TS(S, "dve", ki[:], ang[:, 0, :], 1.0 / TWO_PI, None, ALU.mult, R=[b_ang], W=[b_ki])
            CP(S, "dve", ang[:, 1, :], ki[:], [b_ki], [b_ang])
            STT(S, "dve", ang[:, 0, :], ang[:, 1, :], -TWO_PI, ang[:, 0, :], ALU.mult, ALU.add, [b_ang], [b_ang])
            TS(S, "dve", ang[:, 1, :], ang[:, 0, :], float(np.pi), -TWO_PI, ALU.is_gt, ALU.mult, R=[b_ang], W=[b_ang])
            TT(S, "dve", ang[:, 0, :], ang[:, 0, :], ang[:, 1, :], ALU.add, [b_ang], [b_ang])
            TS(S, "dve", ang[:, 1, :], ang[:, 0, :], float(-np.pi), TWO_PI, ALU.is_lt, ALU.mult, R=[b_ang], W=[b_ang])
            TT(S, "dve", ang[:, 0, :], ang[:, 0, :], ang[:, 1, :], ALU.add, [b_ang], [b_ang])
            TS(S, "dve", ang[:, 0, :], ang[:, 0, :], -3.14159, 3.14159, ALU.max, ALU.min, R=[b_ang], W=[b_ang])
            ACT(S, sn[:], ang[:, 0, :], AF.Sin, [b_ang], [b_sn])
            sin16, sin8, cos16, cos8 = sn[:, 0:16], sn[:, 16:24], sn[:, 24:40], sn[:, 40:48]
            groups = [
                (x[:, 0:512].rearrange("p (h d) -> p h d", h=4), 4, gn[:, 0:4, :],
                 o[:, 0:512].rearrange("p (h d) -> p h d", h=4)),
                (x[:, 768:1280].rearrange("p (a b) -> p a b", b=256)[:, :, 0:128], 2, gn[:, 4:6, :],
                 o[:, 1024:1280].rearrange("p (h d) -> p h d", h=2)),
                (x[:, 1292:1932].rearrange("p (h d) -> p h d", h=5), 5, gn[:, 6:11, :],
                 o[:, 1280:1920].rearrange("p (h d) -> p h d", h=5)),
            ]
            for gi, (src, H, gain, dst) in enumerate(groups):
                TT(S, "dve", sq[:, 0:H, :], src, src, ALU.mult, [b_x], [b_sq])
                RED(S, "dve", st[:, 0, 0:H], sq[:, 0:H, :], ALU.add, [b_sq], [b_st])
                TS(S, "dve", st[:, 1, 0:H], st[:, 0, 0:H], 1.0 / 128, EPS, ALU.mult, ALU.add, R=[b_st], W=[b_st])
                RSTD(S, st[:, 2, 0:H], st[:, 3, 0:H], st[:, 1, 0:H], [b_st], [b_st])
                TT(S, "dve", dst, src, st[:, 2, 0:H].unsqueeze(2).to_broadcast([128, H, 128]), ALU.mult,
                   [b_x, b_st], [b_o])
                TT(S, "dve", dst, dst, gain, ALU.mult, [b_o, b_gn], [b_o])
            CP(S, "pool", o[:, 512:1024], o[:, 0:512], [b_o], [b_o])
            _rope_inplace(S, o[:, 512:1024].rearrange("p (h d) -> p h d", h=4), b_o, 4, 16, cos16, sin16, b_sn, rt, b_rt)
            _rope_inplace(S, o[:, 1024:1280].rearrange("p (h d) -> p h d", h=2), b_o, 2, 16, cos16, sin16, b_sn, rt, b_rt)
            _rope_inplace(S, o[:, 1280:1920].rearrange("p (h d) -> p h d", h=5), b_o, 5, 16, cos16, sin16, b_sn, rt, b_rt)
            CP(S, "pool", o[:, 1920:2496], x[:, 2060:2636], [b_x], [b_o])
            _rope_inplace(S, o[:, 1920:2496].rearrange("p (h d) -> p h d", h=9), b_o, 9, 8, cos8, sin8, b_sn, rt, b_rt)
            ACT(S, o[:, 2496:2508], x[:, 1280:1292], AF.Sigmoid, [b_x], [b_o])
            TS(S, "dve", o[:, 2508:2516], x[:, 2636:2644], float(8 ** -0.5), None, ALU.mult, R=[b_x], W=[b_o])
            S.dma("pool", Y[i * 128:(i + 1) * 128, :], o[:], b_o, None, sem_on="src")
        S.finish([(b.dsem, b.dcnt) for _, b in os_])
    return nc


def run_prep(pcd, positions, nsa_q_g, nsa_k_g, dsa_q_g, dsa_k_g):
    Ttot = pcd.shape[0]
    T = Ttot // NCORES
    nc = _get(("prep", T), lambda: build_prep(T))
    gl = [nsa_q_g] * 4 + [nsa_k_g[1], nsa_k_g[2]] + [dsa_q_g] * 4 + [dsa_k_g]
    gn = np.ascontiguousarray(np.broadcast_to(np.stack(gl)[None], (128, 11, 128))).astype(np.float32)
    inv16 = (500000.0 ** (-np.arange(16, dtype=np.float32) / np.float32(16))).astype(np.float32)
    inv8 = (500000.0 ** (-np.arange(8, dtype=np.float32) / np.float32(8))).astype(np.float32)
    inv = rep128(np.concatenate([inv16, inv8, inv16, inv8]))
    ph = rep128(np.concatenate([np.zeros(24, np.float32), np.full(24, np.pi / 2, np.float32)]))
    pos = positions.reshape(-1, 1).astype(np.int32)
    maps = []
    for c in range(NCORES):
        maps.append({"X": np.ascontiguousarray(pcd[c * T:(c + 1) * T]), "pos": np.ascontiguousarray(pos[c * T:(c + 1) * T]),
                     "gn": gn, "inv": inv, "ph": ph})
    res = _run(nc, maps)
    return np.concatenate([r["Y"] for r in res], axis=0)


NIT = 15
NEGBIG = -1.0e30


def _load_cast_cols(S, dst, b_dst, src_ap, P, ncols, stages=None, piece=4096, tag=""):
    for c0 in range(0, ncols, piece):
        cw = min(piece, ncols - c0)
        S.dma("pool", dst[0:P, c0:c0 + cw], src_ap[:, c0:c0 + cw], None, b_dst)


def build_dsa(NJ):
    nc = bass.Bass("TRN2", target_bir_lowering=False)
    NB = NJ * 8
    TT_ = NB * 128
    IQT = nc.dram_tensor("iqT", [NJ, 64, 8 * 128], F32, kind="ExternalInput").ap()
    IW = nc.dram_tensor("iw", [NJ, 128, 8], F32, kind="ExternalInput").ap()
    QT = nc.dram_tensor("qT", [NJ, 128, 512], F32, kind="ExternalInput").ap()
    IKT = nc.dram_tensor("ikT", [64, TT_], F32, kind="ExternalInput").ap()
    KT = nc.dram_tensor("kT", [128, TT_], F32, kind="ExternalInput").ap()
    VA = nc.dram_tensor("va", [128, NB * 129], F32, kind="ExternalInput").ap()
    NM = nc.dram_tensor("negmask", [128, 1024], F32, kind="ExternalInput").ap()
    P2 = nc.dram_tensor("pow2", [128, NIT], F32, kind="ExternalInput").ap()
    ID = nc.dram_tensor("ident", [128, 128], F32, kind="ExternalInput").ap()
    Y = nc.dram_tensor("Y", [NJ, 128, 512], F32, kind="ExternalOutput").ap()
    scale = float(128 ** -0.5)
    with contextlib.ExitStack() as es:
        S = Sched(nc, es)
        idb, b_idb = load_const(S, "id", ID, [128, 128], cast=BF16)
        nm, b_nm = load_const(S, "nm", NM, [128, 1024])
        p2, b_p2 = load_const(S, "p2", P2, [128, NIT])
        stages = [S.sb("stg%d" % k, [128, 2064], F32) for k in range(2)]
        ikT, b_ikT = S.sb("ikT", [64, TT_], BF16)
        kT, b_kT = S.sb("kT", [128, TT_], BF16)
        va, b_va = S.sb("va", [128, NB * 129], BF16)
        _load_cast_cols(S, ikT, b_ikT, IKT, 64, TT_, stages)
        _load_cast_cols(S, kT, b_kT, KT, 128, TT_, stages)
        _load_cast_cols(S, va, b_va, VA, 128, NB * 129, stages, piece=2064)
        va3 = va[:].rearrange("p (b d) -> p b d", d=129)
        score, b_score = S.sb("score", [128, TT_], F32)
        maskq, b_maskq = S.sb("maskq", [128, TT_], BF16)
        maskT, b_maskT = S.sb("maskT", [128, NB, 128], BF16)
        iqf = [S.sb("iqf%d" % k, [64, 1024], F32) for k in range(2)]
        iqb = [S.sb("iqb%d" % k, [64, 1024], BF16) for k in range(2)]
        qf = [S.sb("qf%d" % k, [128, 512], F32) for k in range(2)]
        qb = [S.sb("qb%d" % k, [128, 512], BF16) for k in range(2)]
        iws = [S.sb("iw%d" % k, [128, 8], F32) for k in range(2)]
        rbuf = [S.sb("r%d" % k, [128, 512], F32) for k in range(3)]
        ebuf = [S.sb("e%d" % k, [128, 512], F32) for k in range(2)]
        pbuf = [S.sb("p%d" % k, [128, 4, 128], BF16) for k in range(2)]
        obuf = [S.sb("ob%d" % k, [128, 512], F32) for k in range(2)]
        bs_, b_bs = S.sb("bis", [128, 8], F32)
        hd, b_hd = S.sb("hd", [128, NIT], F32)
        cnt, b_cnt = S.sb("cnt", [128, NIT], F32)
        zz, b_zz = S.sb("zz", [128, 8], F32)
        sps = [S.ps("sps%d" % k, [128, 512], F32) for k in range(2)]
        stp = sps
        tps, b_tps = S.ps("tps", [128, 512], BF16)
        ops_, b_ops = S.ps("ops", [128, 4, 512], F32)
        nr = 0
        for j in range(NJ):
            NBj = 8 * j + 8
            L = NBj * 128
            NCH = NBj // 4
            iq_f, b_iqf = iqf[j % 2]
            iq_b, b_iqb = iqb[j % 2]
            q_f, b_qf = qf[j % 2]
            q_b, b_qb = qb[j % 2]
            iw, b_iw = iws[j % 2]
            S.dma("sp", iq_f[:], IQT[j], None, b_iqf)
            S.dma("act", q_f[:], QT[j], None, b_qf)
            S.dma("sp", iw[:], IW[j], None, b_iw)
            CP(S, "pool", iq_b[:], iq_f[:], [b_iqf], [b_iqb])
            CP(S, "pool", q_b[:], q_f[:], [b_qf], [b_qb])
            for ch in range(NCH):
                sc_ch = score[:, ch * 512:(ch + 1) * 512]
                for h in range(8):
                    ps, b_ps = sps[nr % 2]
                    r, b_r = rbuf[nr % 3]
                    nr += 1
                    MM(S, ps[:], iq_b[:, h * 128:(h + 1) * 128], ikT[:, ch * 512:(ch + 1) * 512], True, True,
                       [b_iqb, b_ikT], [b_ps])
                    ACT(S, r[:], ps[:], AF.Relu, [b_ps], [b_r], scale=0.125)
                    if h == 0:
                        TS(S, "dve", sc_ch, r[:], iw[:, 0:1], None, ALU.mult, R=[b_r, b_iw], W=[b_score])
                    else:
                        STT(S, "dve", sc_ch, r[:], iw[:, h:h + 1], sc_ch, ALU.mult, ALU.add,
                            [b_r, b_iw, b_score], [b_score])
            RED(S, "dve", bs_[:, 0:1], score[:, 0:L], ALU.max, [b_score], [b_bs])
            RED(S, "dve", bs_[:, 1:2], score[:, 0:L], ALU.min, [b_score], [b_bs])
            TT(S, "dve", score[:, L - 1024:L], score[:, L - 1024:L], nm[:], ALU.add, [b_score, b_nm], [b_score])
            TS(S, "dve", bs_[:, 2:3], bs_[:, 1:2], -1.0, None, ALU.add, R=[b_bs], W=[b_bs])
            STT(S, "dve", bs_[:, 3:4], bs_[:, 0:1], 2.0, bs_[:, 1:2], ALU.add, ALU.subtract, [b_bs], [b_bs])
            TS(S, "dve", hd[:], p2[:], bs_[:, 3:4], None, ALU.mult, R=[b_p2, b_bs], W=[b_hd])
            S.op("dve", lambda e: e.memset(cnt[:], 0.0), [], [b_cnt])
            for k in range(NIT):
                TT(S, "dve", bs_[:, 4:5], bs_[:, 2:3], hd[:, k:k + 1], ALU.add, [b_bs, b_hd], [b_bs])
                TS(S, "dve", maskq[:, 0:L], score[:, 0:L], bs_[:, 4:5], None, ALU.is_ge, ALU.add,
                   R=[b_score, b_bs, b_cnt], W=[b_maskq, b_cnt], accum=cnt[:, k:k + 1])
                TS(S, "dve", bs_[:, 5:6], cnt[:, k:k + 1], 255.5, hd[:, k:k + 1], ALU.is_gt, ALU.mult,
                   R=[b_cnt, b_hd], W=[b_bs])
                TT(S, "dve", bs_[:, 2:3], bs_[:, 2:3], bs_[:, 5:6], ALU.add, [b_bs], [b_bs])
            TS(S, "dve", maskq[:, 0:L], score[:, 0:L], bs_[:, 2:3], None, ALU.is_ge, R=[b_score, b_bs], W=[b_maskq])
            for g4 in range(NBj // 4):
                for t in range(4):
                    kb = g4 * 4 + t
                    TR(S, tps[:, t * 128:(t + 1) * 128], maskq[:, kb * 128:(kb + 1) * 128], idb[:],
                       [b_maskq, b_idb], [b_tps], acc=(t > 0))
                CP(S, "act", maskT[:, g4 * 4:(g4 + 1) * 4, :], tps[:].rearrange("p (a b) -> p a b", a=4),
                   [b_tps], [b_maskT])
            for kb in range(NBj):
                st_, b_st = stp[kb % 2]
                e_, b_e = ebuf[kb % 2]
                p_, b_p = pbuf[kb % 2]
                MM(S, st_[:], kT[:, kb * 128:(kb + 1) * 128], q_b[:], True, True, [b_kT, b_qb], [b_st])
                ACT(S, e_[:], st_[:], AF.Exp, [b_st], [b_e], scale=scale)
                TT(S, "dve", p_[:], e_[:].rearrange("p (h q) -> p h q", h=4),
                   maskT[:, kb, :].unsqueeze(1).to_broadcast([128, 4, 128]), ALU.mult, [b_e, b_maskT], [b_p])
                for h in range(4):
                    MM(S, ops_[:, h, 0:129], p_[:, h, :], va3[:, kb, :], kb == 0, kb == NBj - 1,
                       [b_p, b_va], [b_ops])
            o_, b_o = obuf[j % 2]
            TS(S, "dve", zz[:, 0:4], ops_[:, :, 128], 1e-30, None, ALU.max, R=[b_ops], W=[b_zz])
            S.op("dve", lambda e: e.reciprocal(out=zz[:, 4:8], in_=zz[:, 0:4]), [b_zz], [b_zz])
            TT(S, "dve", o_[:].rearrange("p (h d) -> p h d", h=4), ops_[:, :, 0:128],
               zz[:, 4:8].unsqueeze(2).to_broadcast([128, 4, 128]), ALU.mult, [b_ops, b_zz], [b_o])
            S.dma("pool", Y[j], o_[:], b_o, None, sem_on="src")
        S.finish([(b.dsem, b.dcnt) for _, b in obuf])
    return nc


def causal_negmask(c):
    m = np.zeros((128, 8, 128), np.float32)
    q = np.arange(128)
    for r in range(8):
        if r == c:
            m[:, r, :] = np.where(q[None, :] <= q[:, None], 0.0, NEGBIG)
        elif r > c:
            m[:, r, :] = NEGBIG
    return m.reshape(128, 1024)


def own_tiles_T(a, H, D, c, NJ):
    out = []
    for j in range(NJ):
        g = 8 * j + c
        t = a[g * 128:(g + 1) * 128].reshape(128, H, D)
        out.append(np.transpose(t, (2, 1, 0)).reshape(D, H * 128))
    return np.ascontiguousarray(np.stack(out))


def v_aug(v):
    NB = v.shape[0] // 128
    t = np.ones((128, NB, 129), np.float32)
    t[:, :, 0:128] = np.transpose(v.reshape(NB, 128, 128), (1, 0, 2))
    return t.reshape(128, NB * 129)


def run_dsa(Yp, vd):
    Ttot = Yp.shape[0]
    NJ = Ttot // (128 * NCORES)
    nc = _get(("dsa", NJ), lambda: build_dsa(NJ))
    ikT = np.ascontiguousarray(Yp[:, 2432:2496].T)
    kT = np.ascontiguousarray(Yp[:, 1792:1920].T)
    va = v_aug(vd)
    pow2 = rep128(2.0 ** -(np.arange(NIT, dtype=np.float32) + 1))
    maps = []
    for c in range(NCORES):
        own = [8 * j + c for j in range(NJ)]
        maps.append({"iqT": own_tiles_T(Yp[:, 1920:2432], 8, 64, c, NJ),
                     "iw": np.ascontiguousarray(np.stack([Yp[g * 128:(g + 1) * 128, 2508:2516] for g in own])),
                     "qT": own_tiles_T(Yp[:, 1280:1792], 4, 128, c, NJ),
                     "ikT": ikT, "kT": kT, "va": va, "negmask": causal_negmask(c), "pow2": pow2, "ident": IDENT})
    res = _run(nc, maps)
    out = np.zeros((Ttot, 512), np.float32)
    for c in range(NCORES):
        for j in range(NJ):
            g = 8 * j + c
            out[g * 128:(g + 1) * 128] = res[c]["Y"][j]
    return out


GELU_C = 1.5957691216057308


def MMG(S, out, lhsT, rhs, first, R, W):
    return S.op("pe", lambda e: e.matmul(out, lhsT, rhs, start=first, stop=False, skip_group_check=True),
                R, W, accumulate=True)


def build_nsa(NJ):
    nc = bass.Bass("TRN2", target_bir_lowering=False)
    NB = NJ * 8
    TT_ = NB * 128
    NCMP = (TT_ - 32) // 16 + 1
    NCC = (NCMP + 127) // 128
    NCP = NCC * 128
    dr = lambda name, shape: nc.dram_tensor(name, shape, F32, kind="ExternalInput").ap()
    QNT = dr("qnT", [NJ, 128, 512])
    QRT = dr("qrT", [NJ, 128, 512])
    GATES = dr("gates", [NJ, 128, 12])
    CMASK = dr("cmask", [NJ, 128, 4 * 128])
    SELB = dr("selb", [NJ, 128, 128])
    KCT = dr("kcmpT", [128, TT_])
    VCT = dr("vcmpT", [128, TT_])
    W1 = dr("w1", [128, 2 * 32 * 128])
    POST = dr("posT", [128, 64])
    W2 = dr("w2", [128, 256])
    KG0 = dr("kg0", [128, 128])
    KST = dr("ksT", [128, TT_])
    KWT = dr("kwT", [128, TT_])
    VSA = dr("vsa", [128, NB * 129])
    VWA = dr("vwa", [128, NB * 129])
    OV = dr("ov", [128, 512])
    EX = dr("expE", [128, NB * 128])
    CAUS = dr("causT", [128, 1024])
    WINM = dr("winT", [128, 1536])
    ID = dr("ident", [128, 128])
    Y = nc.dram_tensor("Y", [NJ, 128, 512], F32, kind="ExternalOutput").ap()
    scale = float(128 ** -0.5)
    skip = set(os.environ.get("NSA_SKIP", "").split(","))
    with contextlib.ExitStack() as es:
        S = Sched(nc, es)
        idf, b_idf = load_const(S, "id", ID, [128, 128])
        kg0, b_kg0 = load_const(S, "kg0", KG0, [128, 128])
        stages = [S.sb("stg%d" % k, [128, 2064], F32) for k in range(2)]

        def bf_const(name, ap, ncols, piece=2048):
            t, b = S.sb(name, [128, ncols], BF16)
            _load_cast_cols(S, t, b, ap, 128, ncols, stages, piece=piece)
            return t, b
        w1, b_w1 = bf_const("w1", W1, 8192)
        posT, b_posT = bf_const("posT", POST, 64)
        w2, b_w2 = bf_const("w2", W2, 256)
        kcx, b_kcx = bf_const("kcx", KCT, TT_)
        vcx, b_vcx = bf_const("vcx", VCT, TT_)
        ksT, b_ksT = bf_const("ksT", KST, TT_)
        kwT, b_kwT = bf_const("kwT", KWT, TT_)
        vsa, b_vsa = bf_const("vsa", VSA, NB * 129, piece=2064)
        vwa, b_vwa = bf_const("vwa", VWA, NB * 129, piece=2064)
        ov, b_ov = bf_const("ov", OV, 512)
        exE, b_exE = bf_const("exE", EX, NB * 128)
        caus, b_caus = bf_const("caus", CAUS, 1024)
        winm, b_winm = bf_const("winm", WINM, 1536)
        vsa3 = vsa[:].rearrange("p (b d) -> p b d", d=129)
        vwa3 = vwa[:].rearrange("p (b d) -> p b d", d=129)
        w1v = w1[:].rearrange("p (x l j) -> p x l j", x=2, l=32)
        A = [S.ps("A%d" % k, [128, 512], F32) for k in range(2)]
        O, b_O = S.ps("O", [128, 4, 512], F32)
        IMP, b_IMP = S.ps("IMP", [128, 4, 128], F32)
        Mk, b_Mk = S.ps("Mk", [128, 512], F32)
        stop_at = os.environ.get("NSA_STOP", "")
        if stop_at == "c0":
            S.finish([])
            return nc
        kcT, b_kcT = S.sb("kcT", [128, NCP], BF16)
        vca, b_vca = S.sb("vca", [128, NCC, 129], BF16)
        S.op("dve", lambda e: e.memset(vca[:], 1.0), [], [b_vca])
        hs, b_hs = S.sb("hs", [128, NCP], F32)
        t1, b_t1 = S.sb("t1", [128, NCP], F32)
        t2, b_t2 = S.sb("t2", [128, NCP], F32)
        G, b_G = S.sb("G", [128, NCP], BF16)
        cb, b_cb = S.sb("cb", [128, 8], F32)
        kcs, b_kcs = S.sb("kcs", [128, 128], F32)
        jk, b_jk = S.sb("jk", [128, 128], F32)
        for X in range(2):
            src = kcx if X == 0 else vcx
            b_src = b_kcx if X == 0 else b_vcx
            xv = src[:].rearrange("p (n s) -> p s n", s=16)
            pc_, b_pc = A[0]
            ph_, b_ph = A[1]
            for l in range(32):
                if "cb" in skip:
                    continue
                MM(S, pc_[:, 0:1], w1v[:, X, l, :], posT[:, X * 32 + l:X * 32 + l + 1], l == 0, l == 31,
                   [b_w1, b_posT], [b_pc])
            CP(S, "dve", cb[:, X:X + 1], pc_[:, 0:1], [b_pc], [b_cb])
            for l in range(32):
                rhs = xv[:, l, 0:NCMP] if l < 16 else xv[:, l - 16, 1:1 + NCMP]
                if "ht" in skip:
                    continue
                MM(S, ph_[:, 0:NCMP], w1v[:, X, l, :], rhs, l == 0, l == 31, [b_w1, b_src], [b_ph])
            S.op("dve", lambda e: e.memset(hs[:], 0.0), [], [b_hs])
            ACT(S, hs[:, 0:NCMP], ph_[:, 0:NCMP], AF.Identity, [b_ph, b_cb], [b_hs], bias=cb[:, X:X + 1])
            TT(S, "dve", t1[:], hs[:], hs[:], ALU.mult, [b_hs], [b_t1])
            TS(S, "dve", t1[:], t1[:], 0.044715, 1.0, ALU.mult, ALU.add, R=[b_t1], W=[b_t1])
            TT(S, "dve", t1[:], t1[:], hs[:], ALU.mult, [b_t1, b_hs], [b_t1])
            ACT(S, t2[:], t1[:], AF.Sigmoid, [b_t1], [b_t2], scale=GELU_C)
            TT(S, "dve", G[:], hs[:], t2[:], ALU.mult, [b_hs, b_t2], [b_G])
            for ch in range(NCC):
                po_, b_po = A[ch % 2]
                MM(S, po_[:, 0:128], G[:, ch * 128:(ch + 1) * 128], w2[:, X * 128:(X + 1) * 128], True, True,
                   [b_G, b_w2], [b_po])
                if X == 0:
                    ACT(S, jk[:], po_[:, 0:128], AF.Square, [b_po], [b_jk, b_cb], accum=cb[:, 2:3])
                    TS(S, "dve", cb[:, 3:4], cb[:, 2:3], 1.0 / 128, EPS, ALU.mult, ALU.add, R=[b_cb], W=[b_cb])
                    RSTD(S, cb[:, 4:5], cb[:, 5:6], cb[:, 3:4], [b_cb], [b_cb])
                    STT(S, "dve", kcs[:], po_[:, 0:128], cb[:, 4:5], kg0[:], ALU.mult, ALU.mult,
                        [b_po, b_cb, b_kg0], [b_kcs])
                    TR(S, Mk[:, 256:384], kcs[:], idf[:], [b_kcs, b_idf], [b_Mk])
                    CP(S, "act", kcT[:, ch * 128:(ch + 1) * 128], Mk[:, 256:384], [b_Mk], [b_kcT])
                else:
                    CP(S, "act", vca[:, ch, 0:128], po_[:, 0:128], [b_po], [b_vca])
        if stop_at == "c1":
            S.finish([])
            return nc
        qnf = [S.sb("qnf%d" % k, [128, 512], F32) for k in range(2)]
        qrf = [S.sb("qrf%d" % k, [128, 512], F32) for k in range(2)]
        qnb = [S.sb("qnb%d" % k, [128, 512], BF16) for k in range(2)]
        qrb = [S.sb("qrb%d" % k, [128, 512], BF16) for k in range(2)]
        gts = [S.sb("gt%d" % k, [128, 12], F32) for k in range(2)]
        cms = [S.sb("cm%d" % k, [128, 512], F32) for k in range(2)]
        sbs = [S.sb("sb%d" % k, [128, 128], F32) for k in range(2)]
        ebuf = [S.sb("e%d" % k, [128, 512], F32) for k in range(2)]
        pbuf = [S.sb("p%d" % k, [128, 4, 128], BF16) for k in range(2)]
        obuf = [S.sb("ob%d" % k, [128, 512], F32) for k in range(2)]
        ocmp, b_ocmp = S.sb("ocmp", [128, 4, 128], F32)
        oslc, b_oslc = S.sb("oslc", [128, 4, 128], F32)
        imp, b_imp = S.sb("imp", [128, 128], F32)
        imp2, b_imp2 = S.sb("imp2", [128, 128], F32)
        self_, b_self = S.sb("self", [128, 128], F32)
        selT, b_selT = S.sb("selT", [128, 128], BF16)
        zz, b_zz = S.sb("zz", [128, 48], F32)
        cf, b_cf = S.sb("cf", [128, 12], F32)
        ne = 0

        def attend(kT_ap, q_b, b_q, b_kT, mask_ap, mask_bufs, extra, v_ap, b_v, first, last):
            nonlocal ne
            a_, b_a = A[ne % 2]
            e_, b_e = ebuf[ne % 2]
            p_, b_p = pbuf[ne % 2]
            ne += 1
            MM(S, a_[:], kT_ap, q_b[:], True, True, [b_kT, b_q], [b_a])
            ACT(S, e_[:], a_[:], AF.Exp, [b_a], [b_e], scale=scale)
            TT(S, "dve", p_[:], e_[:].rearrange("p (h q) -> p h q", h=4),
               mask_ap.unsqueeze(1).to_broadcast([128, 4, 128]), ALU.mult, [b_e] + mask_bufs, [b_p])
            if extra is not None:
                TT(S, "pool", p_[:], p_[:], extra[0].unsqueeze(1).to_broadcast([128, 4, 128]), ALU.mult,
                   [b_p, extra[1]], [b_p])
            for h in range(4):
                MM(S, O[:, h, 0:129], p_[:, h, :], v_ap, first, last, [b_p, b_v], [b_O])
            return p_, b_p

        def finish_branch(zoff, dst, b_dst):
            TS(S, "dve", zz[:, zoff:zoff + 4], O[:, :, 128], 1e-30, None, ALU.max, R=[b_O], W=[b_zz])
            S.op("dve", lambda e: e.reciprocal(out=zz[:, zoff + 4:zoff + 8], in_=zz[:, zoff:zoff + 4]), [b_zz], [b_zz])
            if dst is not None:
                CP(S, "dve", dst[:], O[:, :, 0:128], [b_O], [b_dst])

        for j in range(NJ):
            NBj = 8 * j + 8
            NCj = min(NCC, (64 * j + 62) // 128 + 1)
            qn_f, b_qnf = qnf[j % 2]
            qr_f, b_qrf = qrf[j % 2]
            qn_b, b_qnb = qnb[j % 2]
            qr_b, b_qrb = qrb[j % 2]
            gt, b_gt = gts[j % 2]
            cm, b_cm = cms[j % 2]
            sbi, b_sbi = sbs[j % 2]
            S.dma("sp", qn_f[:], QNT[j], None, b_qnf)
            S.dma("act", qr_f[:], QRT[j], None, b_qrf)
            S.dma("sp", gt[:], GATES[j], None, b_gt)
            S.dma("act", cm[:], CMASK[j], None, b_cm)
            S.dma("sp", sbi[:], SELB[j], None, b_sbi)
            CP(S, "pool", qn_b[:], qn_f[:], [b_qnf], [b_qnb])
            CP(S, "pool", qr_b[:], qr_f[:], [b_qrf], [b_qrb])
            for ch in range(NCj):
                p_, b_p = attend(kcT[:, ch * 128:(ch + 1) * 128], qn_b, b_qnb, b_kcT, cm[:, ch * 128:(ch + 1) * 128],
                                 [b_cm], None, vca[:, ch, :], b_vca, ch == 0, ch == NCj - 1)
                for h in range(4):
                    if "imp" in skip:
                        continue
                    MMG(S, IMP[:, h, :], p_[:, h, :], ov[:, ch * 128:(ch + 1) * 128], ch == 0 and h == 0,
                        [b_p, b_ov], [b_IMP])
            finish_branch(0, ocmp, b_ocmp)
            TS(S, "dve", imp[:], IMP[:, 0, :], zz[:, 4:5], None, ALU.mult, R=[b_IMP, b_zz], W=[b_imp])
            for h in range(1, 4):
                STT(S, "dve", imp[:], IMP[:, h, :], zz[:, 4 + h:5 + h], imp[:], ALU.mult, ALU.add,
                    [b_IMP, b_zz, b_imp], [b_imp])
            TT(S, "dve", imp[:], imp[:], sbi[:], ALU.add, [b_imp, b_sbi], [b_imp])
            if "top" not in skip:
                S.op("dve", lambda e: e.max(out=zz[:, 24:32], in_=imp[:]), [b_imp], [b_zz])
                S.op("dve", lambda e: e.match_replace(out=imp2[:], in_to_replace=zz[:, 24:32], in_values=imp[:],
                                                      imm_value=-3.0e38), [b_imp, b_zz], [b_imp2])
                S.op("dve", lambda e: e.max(out=zz[:, 32:40], in_=imp2[:]), [b_imp2], [b_zz])
            RED(S, "dve", zz[:, 40:41], zz[:, 32:40], ALU.min, [b_zz], [b_zz])
            TS(S, "dve", self_[:], imp[:], zz[:, 40:41], None, ALU.is_ge, R=[b_imp, b_zz], W=[b_self])
            TR(S, Mk[:, 256:384], self_[:], idf[:], [b_self, b_idf], [b_Mk])
            CP(S, "act", selT[:], Mk[:, 256:384], [b_Mk], [b_selT])
            for kb in range(NBj):
                if "slc" in skip:
                    continue
                slot = (kb % 2) * 128
                MM(S, Mk[:, slot:slot + 128], exE[:, kb * 128:(kb + 1) * 128], selT[:], True, True,
                   [b_exE, b_selT], [b_Mk])
                extra = None
                if kb >= NBj - 8 and "extra" not in skip:
                    r = kb - (NBj - 8)
                    extra = (caus[:, r * 128:(r + 1) * 128], b_caus)
                attend(ksT[:, kb * 128:(kb + 1) * 128], qr_b, b_qrb, b_ksT, Mk[:, slot:slot + 128], [b_Mk], extra,
                       vsa3[:, kb, :], b_vsa, kb == 0, kb == NBj - 1)
            finish_branch(8, oslc, b_oslc)
            blks = [(r, 8 * j - 4 + r) for r in range(12) if 8 * j - 4 + r >= 0]
            for n_, (r, blk) in enumerate(blks):
                if "win" in skip:
                    continue
                attend(kwT[:, blk * 128:(blk + 1) * 128], qr_b, b_qrb, b_kwT, winm[:, r * 128:(r + 1) * 128],
                       [b_winm], None, vwa3[:, blk, :], b_vwa, n_ == 0, n_ == len(blks) - 1)
            finish_branch(16, None, None)
            gv = gt[:].rearrange("p (h b) -> p h b", b=3)
            for b_i, zo in enumerate((4, 12, 20)):
                TT(S, "dve", cf[:, b_i * 4:(b_i + 1) * 4], gv[:, :, b_i], zz[:, zo:zo + 4], ALU.mult,
                   [b_gt, b_zz], [b_cf])
            o_, b_o = obuf[j % 2]
            for h in range(4):
                oh = o_[:, h * 128:(h + 1) * 128]
                TS(S, "dve", oh, ocmp[:, h, :], cf[:, h:h + 1], None, ALU.mult, R=[b_ocmp, b_cf], W=[b_o])
                STT(S, "dve", oh, oslc[:, h, :], cf[:, 4 + h:5 + h], oh, ALU.mult, ALU.add,
                    [b_oslc, b_cf, b_o], [b_o])
                STT(S, "dve", oh, O[:, h, 0:128], cf[:, 8 + h:9 + h], oh, ALU.mult, ALU.add,
                    [b_O, b_cf, b_o], [b_o])
            S.dma("pool", Y[j], o_[:], b_o, None, sem_on="src")
        S.finish([(b.dsem, b.dcnt) for _, b in obuf])
    return nc


def run_nsa(Yp, pc, cmp_pos, cmp_w1, cmp_w2, k_g0):
    Ttot = Yp.shape[0]
    NJ = Ttot // (128 * NCORES)
    NB = NJ * 8
    nc = _get(("nsa", NJ), lambda: build_nsa(NJ))
    NCMP = (Ttot - 32) // 16 + 1
    NSEL = Ttot // 64
    w1 = np.ascontiguousarray(np.transpose(cmp_w1.reshape(2, 32, 128, 128), (2, 0, 1, 3))).reshape(128, 8192)
    posT = np.ascontiguousarray(np.transpose(cmp_pos, (2, 0, 1))).reshape(128, 64)
    w2 = np.ascontiguousarray(np.transpose(cmp_w2, (1, 0, 2))).reshape(128, 256)
    n_all = np.arange(512)
    jb = np.arange(128)
    ovm = ((n_all[:, None] >= 4 * jb[None, :] - 1) & (n_all[:, None] <= 4 * jb[None, :] + 3)
           & (n_all[:, None] < NCMP) & (jb[None, :] < NSEL)).astype(np.float32)
    ov = np.ascontiguousarray(np.transpose(ovm.reshape(4, 128, 128), (1, 0, 2))).reshape(128, 512)
    s_ = np.arange(128)
    exE = np.zeros((128, NB, 128), np.float32)
    for kb in range(NB):
        exE[2 * kb + s_ // 64, kb, s_] = 1.0 if True else 0.0
    exE = exE[:128].reshape(128, NB * 128) if 2 * NB <= 128 else exE.reshape(128, NB * 128)
    tri = (s_[:, None] <= s_[None, :]).astype(np.float32)
    common = {"kcmpT": np.ascontiguousarray(pc[:, 512:640].T), "vcmpT": np.ascontiguousarray(pc[:, 640:768].T),
              "w1": w1, "posT": posT, "w2": w2, "kg0": rep128(k_g0),
              "ksT": np.ascontiguousarray(Yp[:, 1024:1152].T), "kwT": np.ascontiguousarray(Yp[:, 1152:1280].T),
              "vsa": v_aug(np.ascontiguousarray(pc[:, 896:1024])), "vwa": v_aug(np.ascontiguousarray(pc[:, 1152:1280])),
              "ov": ov, "expE": exE, "ident": IDENT}
    maps = []
    for c in range(NCORES):
        own = [8 * j + c for j in range(NJ)]
        caus = np.zeros((128, 8, 128), np.float32)
        for r in range(8):
            if r < c:
                caus[:, r, :] = 1.0
            elif r == c:
                caus[:, r, :] = tri
        winT = np.zeros((128, 12, 128), np.float32)
        for r in range(12):
            d = r - 4 - c
            if d == 0:
                winT[:, r, :] = tri
            elif d in (-1, -2, -3):
                winT[:, r, :] = 1.0
            elif d == -4:
                winT[:, r, :] = 1.0 - tri
        cmask = np.zeros((NJ, 128, 4, 128), np.float32)
        selb = np.zeros((NJ, 128, 128), np.float32)
        for j, g in enumerate(own):
            t = g * 128 + s_
            nn = np.arange(512).reshape(4, 128)
            ok = (16 * nn[:, :, None] + 31 <= t[None, None, :]) & (nn[:, :, None] < NCMP)
            cmask[j] = np.transpose(ok, (1, 0, 2)).astype(np.float32)
            cur = t // 64
            forced = (jb[None, :] == 0) | (jb[None, :] == cur[:, None]) | (jb[None, :] == cur[:, None] - 1)
            future = jb[None, :] > cur[:, None]
            selb[j] = np.where(forced, 1.0e30, np.where(future, NEGBIG, 0.0)).astype(np.float32)
        m = dict(common)
        m.update({"qnT": own_tiles_T(Yp[:, 0:512], 4, 128, c, NJ), "qrT": own_tiles_T(Yp[:, 512:1024], 4, 128, c, NJ),
                  "gates": np.ascontiguousarray(np.stack([Yp[g * 128:(g + 1) * 128, 2496:2508] for g in own])),
                  "cmask": cmask.reshape(NJ, 128, 512), "selb": selb,
                  "causT": caus.reshape(128, 1024), "winT": winT.reshape(128, 1536)})
        maps.append(m)
    res = _run(nc, maps)
    out = np.zeros((Ttot, 512), np.float32)
    for c in range(NCORES):
        for j in range(NJ):
            g = 8 * j + c
            out[g * 128:(g + 1) * 128] = res[c]["Y"][j]
    return out


def build_peer(NTL, NCH_E=128):
    nc = bass.Bass("TRN2", target_bir_lowering=False)
    T = NTL * 128
    TW = T
    K = 2048
    KC = 16
    dr = lambda name, shape: nc.dram_tensor(name, shape, F32, kind="ExternalInput").ap()
    X = dr("X", [T, K])
    G_ = dr("pg", [128, K])
    SC = dr("psc", [128, K])
    SH = dr("psh", [128, K])
    GATE = dr("gate", [128, K])
    WQ = dr("wq", [128, KC * 1024])
    SK = dr("skT", [128, 1024])
    UT = dr("UT", [NCH_E, 128, KC * 128])
    V = dr("V", [NCH_E * 128, K])
    ID = dr("ident", [128, 128])
    Y = nc.dram_tensor("Y", [T, K], F32, kind="ExternalOutput").ap()
    with contextlib.ExitStack() as es:
        S = Sched(nc, es)
        idb, b_idb = load_const(S, "id", ID, [128, 128], cast=BF16)
        A, b_A = load_const(S, "A", SC, [128, K])
        B, b_B = load_const(S, "B", SH, [128, K])
        xs = [S.sb("x%d" % j, [128, K], F32) for j in range(2)]
        tmp, b_tmp = S.sb("tmp", [128, K], F32)
        S.dma("sp", tmp[:], G_, None, b_tmp)
        STT(S, "dve", A[:], A[:], 1.0, tmp[:], ALU.add, ALU.mult, [b_A, b_tmp], [b_A])
        hb, b_hb = S.sb("hb", [128, K], BF16)
        st, b_st = S.sb("st", [128, 8], F32)
        hnT, b_hnT = S.sb("hnT", [128, KC, TW], BF16)
        accw, b_accw = S.sb("accw", [128, NTL * K], F32)
        wqb = accw[:].bitcast(BF16)
        assert 2 * NTL * K >= KC * 1024
        skb, b_skb = S.sb("skb", [128, 1024], BF16)
        PA = [S.ps("PA%d" % k, [128, 512], F32) for k in range(4)]
        PV = [S.ps("PV%d" % k, [128, 512], F32) for k in range(4)]
        stg = [(xs[0][0], xs[0][1]), (xs[1][0], xs[1][1])]
        _load_cast_cols(S, accw[:].bitcast(BF16), b_accw, WQ, 128, KC * 1024, stg)
        _load_cast_cols(S, skb, b_skb, SK, 128, 1024, stg)
        s2s = [S.sb("s2_%d" % i, [128, 8, 128], F32) for i in range(NTL)]
        THR = [S.sb("thr_%d" % i, [128, 8, 128], F32) for i in range(NTL)]
        BIA = [S.sb("bia_%d" % i, [128, 8, 128], F32) for i in range(NTL)]
        qT, b_qT = S.sb("qT", [128, 8, 128], BF16)
        sv, b_sv = S.sb("sv", [128, 8, 2, 16], F32)
        mr, b_mr = S.sb("mr", [128, 256], F32)
        cand, b_cand = S.sb("cand", [128, 8, 256], F32)
        cv, b_cv = S.sb("cv", [128, 8, 16], F32)
        sm, b_sm = S.sb("sm", [128, 6, 8], F32)
        jk16, b_jk16 = S.sb("jk16", [128, 16], F32)
        for i in range(NTL):
            x, b_x = xs[i % 2]
            S.dma("sp", x[:], X[i * 128:(i + 1) * 128, :], None, b_x)
            ACT(S, tmp[:], x[:], AF.Square, [b_x], [b_tmp, b_st], accum=st[:, 0:1])
            TS(S, "dve", st[:, 1:2], st[:, 0:1], 1.0 / K, EPS, ALU.mult, ALU.add, R=[b_st], W=[b_st])
            RSTD(S, st[:, 2:3], st[:, 3:4], st[:, 1:2], [b_st], [b_st])
            STT(S, "dve", tmp[:], x[:], st[:, 2:3], A[:], ALU.mult, ALU.mult, [b_x, b_st, b_A], [b_tmp])
            TT(S, "dve", hb[:], tmp[:], B[:], ALU.add, [b_tmp, b_B], [b_hb])
            for q4 in range(KC // 4):
                pt_, b_pt = PA[q4 % 2]
                ptb = pt_[:].bitcast(BF16)
                for t in range(4):
                    kc = q4 * 4 + t
                    TR(S, ptb[:, t * 128:(t + 1) * 128], hb[:, kc * 128:(kc + 1) * 128], idb[:], [b_hb, b_idb],
                       [b_pt], acc=(t > 0))
                CP(S, "act" if q4 % 2 == 0 else "dve", hnT[:, q4 * 4:(q4 + 1) * 4, i * 128:(i + 1) * 128],
                   ptb[:, 0:512].rearrange("p (a b) -> p a b", a=4), [b_pt], [b_hnT])
            for h in range(8):
                pq, b_pq = PA[2 + h % 2]
                for kc in range(KC):
                    MM(S, pq[:, 0:128], wqb[:, kc * 1024 + h * 128:kc * 1024 + (h + 1) * 128],
                       hnT[:, kc, i * 128:(i + 1) * 128], kc == 0, kc == KC - 1, [b_accw, b_hnT], [b_pq])
                CP(S, "act", qT[:, h, :], pq[:, 0:128], [b_pq], [b_qT])
            s2, b_s2 = s2s[i]
            s1, b_s1 = BIA[i]
            for p_ in range(2):
                dst, b_dst = (s1, b_s1) if p_ == 0 else (s2, b_s2)
                lo, hi = p_ * 64, (p_ + 1) * 64
                for hh in range(2):
                    ps_, b_ps = PA[hh]
                    for h4 in range(4):
                        h = hh * 4 + h4
                        MM(S, ps_[:, h4 * 128:(h4 + 1) * 128], qT[lo:hi, h, :], skb[lo:hi, h * 128:(h + 1) * 128],
                           True, True, [b_qT, b_skb], [b_ps])
                    CP(S, "act", dst[:, hh * 4:(hh + 1) * 4, :], ps_[:].rearrange("p (a b) -> p a b", a=4),
                       [b_ps], [b_dst])
            for h in range(8):
                for p_ in range(2):
                    src = s1[:, h, :] if p_ == 0 else s2[:, h, :]
                    b_src = b_s1 if p_ == 0 else b_s2
                    S.op("dve", lambda e, src=src, h=h, p_=p_: e.max(out=sv[:, h, p_, 0:8], in_=src), [b_src], [b_sv])
                    S.op("dve", lambda e, src=src, h=h, p_=p_: e.match_replace(
                        out=mr[:, 0:128], in_to_replace=sv[:, h, p_, 0:8], in_values=src, imm_value=-3.0e38),
                        [b_src, b_sv], [b_mr])
                    S.op("dve", lambda e, h=h, p_=p_: e.max(out=sv[:, h, p_, 8:16], in_=mr[:, 0:128]), [b_mr], [b_sv])
            for h in range(8):
                TT(S, "dve", cand[:, h, :].rearrange("p (a b) -> p a b", a=16),
                   sv[:, h, 0, :].unsqueeze(2).to_broadcast([128, 16, 16]),
                   sv[:, h, 1, :].unsqueeze(1).to_broadcast([128, 16, 16]), ALU.add, [b_sv], [b_cand])
                S.op("dve", lambda e, h=h: e.max(out=cv[:, h, 0:8], in_=cand[:, h, :]), [b_cand], [b_cv])
                S.op("dve", lambda e, h=h: e.match_replace(out=mr[:], in_to_replace=cv[:, h, 0:8],
                                                            in_values=cand[:, h, :], imm_value=-3.0e38),
                     [b_cand, b_cv], [b_mr])
                S.op("dve", lambda e, h=h: e.max(out=cv[:, h, 8:16], in_=mr[:]), [b_mr], [b_cv])
            RED(S, "dve", sm[:, 0, :], cv[:], ALU.min, [b_cv], [b_sm])
            RED(S, "dve", sm[:, 1, :], cv[:], ALU.max, [b_cv], [b_sm])
            TS(S, "dve", sm[:, 2, :], sm[:, 1, :], -1.0, None, ALU.mult, R=[b_sm], W=[b_sm])
            S.op("dve", lambda e: e.memset(sm[:, 3, :], 0.0), [b_sm], [b_sm])
            for h in range(8):
                ACT(S, jk16[:], cv[:, h, :], AF.Exp, [b_cv, b_sm], [b_jk16, b_sm], bias=sm[:, 2, h:h + 1],
                    accum=sm[:, 3, h:h + 1])
            ACT(S, sm[:, 4, :], sm[:, 3, :], AF.Ln, [b_sm], [b_sm])
            STT(S, "dve", sm[:, 5, :], sm[:, 4, :], -1.0, sm[:, 2, :], ALU.mult, ALU.add, [b_sm], [b_sm])
            TS(S, "dve", sm[:, 0, :], sm[:, 0, :], -1.0e-5, None, ALU.add, R=[b_sm], W=[b_sm])
            thr_, b_thr = THR[i]
            TT(S, "dve", thr_[:], sm[:, 0, :].unsqueeze(2).to_broadcast([128, 8, 128]), s1[:], ALU.subtract,
               [b_sm, b_s1], [b_thr])
            TT(S, "dve", s1[:], s1[:], sm[:, 5, :].unsqueeze(2).to_broadcast([128, 8, 128]), ALU.add,
               [b_s1, b_sm], [b_s1])
        NWB = 4
        NWU = 3
        utb = [S.sb("utb%d" % k, [128, KC * 128], BF16) for k in range(NWU)]
        vtb = [S.sb("vtb%d" % k, [128, K], BF16) for k in range(NWB)]
        ssb = [S.sb("ssb%d" % k, [128, TW], F32) for k in range(2)]
        g1_ = [S.sb("g1_%d" % k, [128, TW], F32) for k in range(2)]
        g2_ = [S.sb("g2_%d" % k, [128, TW], F32) for k in range(2)]
        PTs = [S.sb("PT%d" % k, [128, TW], BF16) for k in range(2)]
        NEX = 8
        exs = [S.sb("ex%d" % k, [128, 128], F32) for k in range(NEX)]
        wms = [S.sb("wm%d" % k, [128, 128], BF16) for k in range(NEX)]
        acc3 = accw[:].rearrange("p (i d) -> p i d", i=NTL)
        nw = 0
        npv = 0

        def load_chunk(c):
            ub, b_ub = utb[c % NWU]
            vb, b_vb = vtb[c % NWB]
            S.dma("pool", ub[:], UT[c], None, b_ub)
            S.dma("pool", vb[:], V[c * 128:(c + 1) * 128, :], None, b_vb)

        def finish_chunk(c):
            nonlocal npv
            at, b_at = PA[2 + c % 2]
            gb, b_gb = g2_[c % 2]
            pt_, b_pt = PTs[c % 2]
            TT(S, "dve", pt_[:], at[:, 0:TW], gb[:], ALU.mult, [b_at, b_gb], [b_pt])
            if c % 2 == 1:
                c0 = c - 1
                for i in range(NTL):
                    for dq in range(4):
                        pv, b_pv = PV[npv % 4]
                        npv += 1
                        MM(S, pv[:], PTs[0][0][:, i * 128:(i + 1) * 128], vtb[c0 % NWB][0][:, dq * 512:(dq + 1) * 512],
                           True, False, [PTs[0][1], vtb[c0 % NWB][1]], [b_pv])
                        MM(S, pv[:], PTs[1][0][:, i * 128:(i + 1) * 128], vtb[c % NWB][0][:, dq * 512:(dq + 1) * 512],
                           False, True, [PTs[1][1], vtb[c % NWB][1]], [b_pv])
                        dst = acc3[:, i, dq * 512:(dq + 1) * 512]
                        if c0 == 0:
                            CP(S, "dve", dst, pv[:], [b_pv], [b_accw])
                        else:
                            TT(S, "dve", dst, dst, pv[:], ALU.add, [b_accw, b_pv], [b_accw])

        load_chunk(0)
        load_chunk(1)
        for c in range(NCH_E):
            ub, b_ub = utb[c % NWU]
            su, b_su = PA[c % 2]
            at, b_at = PA[2 + c % 2]
            for kc in range(KC):
                MM(S, su[:, 0:TW], ub[:, kc * 128:(kc + 1) * 128], hnT[:, kc, :], kc == 0, kc == KC - 1,
                   [b_ub, b_hnT], [b_su])
            s_sb, b_ssb = ssb[c % 2]
            ga, b_ga = g1_[c % 2]
            gb, b_gb = g2_[c % 2]
            CP(S, "act", s_sb[:], su[:, 0:TW], [b_su], [b_ssb])
            TT(S, "pool", ga[:], s_sb[:], s_sb[:], ALU.mult, [b_ssb], [b_ga])
            TS(S, "pool", ga[:], ga[:], 0.044715, 1.0, ALU.mult, ALU.add, R=[b_ga], W=[b_ga])
            TT(S, "pool", ga[:], ga[:], s_sb[:], ALU.mult, [b_ga, b_ssb], [b_ga])
            ACT(S, gb[:], ga[:], AF.Sigmoid, [b_ga], [b_gb], scale=GELU_C)
            TT(S, "pool", gb[:], gb[:], s_sb[:], ALU.mult, [b_gb, b_ssb], [b_gb])
            for i in range(NTL):
                s2, b_s2 = s2s[i]
                for h in range(8):
                    ex, b_ex = exs[nw % NEX]
                    wm, b_wm = wms[nw % NEX]
                    nw += 1
                    ACT(S, ex[:], s2[:, h, :], AF.Exp, [b_s2, BIA[i][1]], [b_ex], bias=BIA[i][0][:, h, c:c + 1])
                    STT(S, "dve", wm[:], s2[:, h, :], THR[i][0][:, h, c:c + 1], ex[:], ALU.is_ge, ALU.mult,
                        [b_s2, THR[i][1], b_ex], [b_wm])
                    MMG(S, at[:, i * 128:(i + 1) * 128], wm[:], idb[:], i == 0 and h == 0, [b_wm, b_idb], [b_at])
            if c >= 1:
                finish_chunk(c - 1)
            if c + 2 < NCH_E:
                load_chunk(c + 2)
        finish_chunk(NCH_E - 1)
        gate, b_gate = A, b_A
        S.dma("sp", gate[:], GATE, None, b_gate)
        for i in range(NTL):
            x, b_x = xs[i % 2]
            S.dma("sp", x[:], X[i * 128:(i + 1) * 128, :], None, b_x)
            TT(S, "dve", acc3[:, i, :], acc3[:, i, :], gate[:], ALU.mult, [b_accw, b_gate], [b_accw])
            TT(S, "pool", x[:], x[:], acc3[:, i, :], ALU.add, [b_x, b_accw], [b_x])
            S.dma("pool", Y[i * 128:(i + 1) * 128, :], x[:], b_x, None, sem_on="src")
        S.finish([(b.dsem, b.dcnt) for _, b in xs])
    return nc


def peer_weights(peer_wq, peer_subkeys, peer_u):
    wq = np.ascontiguousarray(np.transpose(peer_wq.reshape(16, 128, 1024), (1, 0, 2))).reshape(128, 16 * 1024)
    skT = np.ascontiguousarray(np.transpose(peer_subkeys, (1, 3, 0, 2))).reshape(128, 1024)
    UT = np.ascontiguousarray(np.transpose(peer_u.reshape(128, 128, 16, 128), (0, 3, 2, 1))).reshape(128, 128, 2048)
    return wq, skT, UT


def run_peer(x1, norm2_g, sc2, sh2, g2, wq, skT, UT, peer_v, NTL=4):
    Ttot = x1.shape[0]
    nc = _get(("peer", NTL), lambda: build_peer(NTL))
    T = NTL * 128
    per = T * NCORES
    outs = []
    pg, psc, psh, gate = rep128(norm2_g), rep128(sc2), rep128(sh2), rep128(g2)
    for r0 in range(0, Ttot, per):
        maps = []
        for c in range(NCORES):
            maps.append({"X": np.ascontiguousarray(x1[r0 + c * T:r0 + (c + 1) * T]), "pg": pg, "psc": psc, "psh": psh,
                         "gate": gate, "wq": wq, "skT": skT, "UT": UT, "V": peer_v, "ident": IDENT})
        res = _run(nc, maps)
        outs.append(np.concatenate([r["Y"] for r in res], axis=0))
    return np.concatenate(outs, axis=0)


def run_ada(c, ada_w, ada_b):
    L, K, N = ada_w.shape
    NC = L * N // NCORES
    nc = _get(("ada", K, NC), lambda: build_gemm(128, K, NC, False, True, silu=True))
    X = np.ascontiguousarray(np.broadcast_to(c.reshape(1, K), (128, K))).astype(np.float32)
    ones = np.ones((128, NC), np.float32)
    maps = []
    for core in range(NCORES):
        l, c0 = divmod(core * NC, N)
        maps.append({"X": X, "W": np.ascontiguousarray(ada_w[l][:, c0:c0 + NC]), "ident": IDENT,
                     "resid": rep128(ada_b[l][c0:c0 + NC]), "gate": ones})
    res = _run(nc, maps)
    flat = np.concatenate([r["Y"][0] for r in res])
    return flat.reshape(L, N)


def kernel(x, c, positions, ada_w, ada_b, norm1_g, norm2_g, w_in, w_out,
           conv_w, conv_b, conv_ln_g, conv_ln_b, sgu_ln_g, sgu_ln_b, sgu_w, sgu_b,
           nsa_q_g, nsa_k_g, nsa_cmp_pos, nsa_cmp_w1, nsa_cmp_w2, dsa_q_g, dsa_k_g,
           peer_wq, peer_subkeys, peer_u, peer_v):
    f = lambda a: np.asarray(a, dtype=np.float32)
    x = f(x)[0]
    pos = np.asarray(positions)[0]
    ada = run_ada(f(c)[0], f(ada_w), f(ada_b))
    for i in range(ada.shape[0]):
        sh1, sc1, g1, sh2, sc2, g2 = np.split(ada[i], 6)
        proj = gemm(x, f(w_in[i]), pro=(rep128(f(norm1_g[i])), rep128(sc1), rep128(sh1)))
        ya = run_conv(proj[:, 0:1024], f(conv_w[i]), f(conv_b[i]), f(conv_ln_g[i]), f(conv_ln_b[i]))
        yb = run_sgu(proj[:, 1024:2048], f(sgu_ln_g[i]), f(sgu_ln_b[i]), f(sgu_w[i]), f(sgu_b[i]))
        pc = proj[:, 2048:2048 + 1292]
        pd = proj[:, 2048 + 1292:]
        Yp = run_prep(proj[:, 2048:], pos, f(nsa_q_g[i]), f(nsa_k_g[i]), f(dsa_q_g[i]), f(dsa_k_g[i]))
        yc = run_nsa(Yp, pc, f(nsa_cmp_pos[i]), f(nsa_cmp_w1[i]), f(nsa_cmp_w2[i]), f(nsa_k_g[i][0]))
        yd = run_dsa(Yp, np.ascontiguousarray(pd[:, 640:768]))
        ycat = np.concatenate([ya, yb, yc, yd], axis=1)
        x1 = gemm(ycat, f(w_out[i]), epi=(x, rep128(g1)))
        wq, skT, UT = peer_weights(f(peer_wq[i]), f(peer_subkeys[i]), f(peer_u[i]))
        x = run_peer(x1, f(norm2_g[i]), sc2, sh2, g2, wq, skT, UT, f(peer_v[i]))
    return x[None].astype(np.float32)
```

```python
import contextlib
import os
import numpy as np
import concourse.bass as bass
import concourse.mybir as mybir
from concourse.bass_utils import run_bass_kernel_spmd

F32 = mybir.dt.float32
BF16 = mybir.dt.bfloat16
ALU = mybir.AluOpType
AF = mybir.ActivationFunctionType
AX = mybir.AxisListType
NCORES = 8
EPS = 1e-6


class Buf:
    __slots__ = ("name", "w", "r", "dsem", "dcnt")

    def __init__(self, name):
        self.name = name
        self.w = None
        self.r = []
        self.dsem = None
        self.dcnt = 0


class Sched:
    ENG = ("pe", "act", "dve", "pool", "sp")

    def __init__(self, nc, es):
        self.nc = nc
        self.es = es
        self.sem = {e: es.enter_context(nc.semaphore("sem_" + e)) for e in self.ENG}
        self.cnt = {e: 0 for e in self.ENG}
        self.waited = {e: {} for e in self.ENG}
        self.prog = {e: [] for e in self.ENG}
        self.bufs = []
        self.nsem = 0

    def buf(self, name):
        b = Buf(name)
        self.bufs.append(b)
        return b

    def sb(self, name, shape, dtype):
        t = self.es.enter_context(self.nc.sbuf_tensor("sb_" + name, list(shape), dtype))
        return t, self.buf(name)

    def ps(self, name, shape, dtype=F32):
        t = self.es.enter_context(self.nc.psum_tensor("ps_" + name, list(shape), dtype))
        return t, self.buf(name)

    def _dsem(self, b):
        if b.dsem is None:
            b.dsem = self.es.enter_context(self.nc.semaphore("d_%d_%s" % (self.nsem, b.name)))
            self.nsem += 1
        return b.dsem

    def _waits(self, eng, reads, writes, skip_self_w=False):
        need = {}
        for b in reads:
            if b.w is not None:
                need[b.w[0]] = max(need.get(b.w[0], 0), b.w[1])
        for b in writes:
            if b.w is not None and not (skip_self_w and b.w[0] is self.sem[eng]):
                need[b.w[0]] = max(need.get(b.w[0], 0), b.w[1])
            for tok in b.r:
                need[tok[0]] = max(need.get(tok[0], 0), tok[1])
        out = []
        wd = self.waited[eng]
        for s, v in need.items():
            if wd.get(s, 0) < v:
                wd[s] = v
                out.append((s, v))
        return out

    def op(self, eng, fn, reads=(), writes=(), accumulate=False):
        waits = self._waits(eng, reads, writes, skip_self_w=accumulate)
        self.cnt[eng] += 1
        tok = (self.sem[eng], self.cnt[eng])
        self.prog[eng].append((waits, fn, (self.sem[eng], 1)))
        for b in reads:
            b.r.append(tok)
        for b in writes:
            b.w = tok
            b.r = []
        return tok

    def dma(self, eng, out_ap, in_ap, src, dst, sem_on="dst", **kw):
        reads = [src] if src is not None else []
        writes = [dst] if dst is not None else []
        waits = self._waits(eng, reads, writes)
        sb = dst if sem_on == "dst" else src
        sem = self._dsem(sb)
        sb.dcnt += 16
        tok = (sem, sb.dcnt)

        def fn(e, out_ap=out_ap, in_ap=in_ap, kw=kw):
            return e.dma_start(out=out_ap, in_=in_ap, **kw)

        self.prog[eng].append((waits, fn, (sem, 16)))
        if src is not None:
            src.r.append(tok)
        if dst is not None:
            dst.w = tok
            dst.r = []
        return tok

    def finish(self, final_tokens):
        nc = self.nc
        need = {}
        for s, v in final_tokens:
            if s is None:
                continue
            need[s] = max(need.get(s, 0), v)
        fin = list(need.items())
        with nc.Block() as block:
            def mk(ename):
                def body(e):
                    for waits, fn, inc in self.prog[ename]:
                        for s, v in waits:
                            e.wait_ge(s, v)
                        ins = fn(e)
                        ins.then_inc(inc[0], inc[1])
                    if ename == "sp":
                        for s, v in fin:
                            e.wait_ge(s, v)
                return body
            block.tensor(mk("pe"))
            block.scalar(mk("act"))
            block.vector(mk("dve"))
            block.gpsimd(mk("pool"))
            block.sync(mk("sp"))


def _run(nc, in_maps):
    if os.environ.get("KTRACE"):
        res = run_bass_kernel_spmd(nc, in_maps, core_ids=list(range(NCORES)), trace=True)
        print("KTRACE exec_time_ns", res.exec_time_ns, flush=True)
        return res.results
    res = run_bass_kernel_spmd(nc, in_maps, core_ids=list(range(NCORES)))
    return res.results


def rep128(v):
    v = np.asarray(v, dtype=np.float32).reshape(1, -1)
    return np.ascontiguousarray(np.broadcast_to(v, (128, v.shape[1])))


IDENT = np.eye(128, dtype=np.float32)


_GEMM_CACHE = {}


def build_gemm(T, K, N, prologue, epilogue, silu=False):
    nc = bass.Bass("TRN2", target_bir_lowering=False)
    NT = T // 128
    KC = K // 128
    CG = 512
    ncg = (N + CG - 1) // CG
    X = nc.dram_tensor("X", [T, K], F32, kind="ExternalInput").ap()
    W = nc.dram_tensor("W", [K, N], F32, kind="ExternalInput").ap()
    ID = nc.dram_tensor("ident", [128, 128], F32, kind="ExternalInput").ap()
    if prologue:
        G = nc.dram_tensor("pg", [128, K], F32, kind="ExternalInput").ap()
        SC = nc.dram_tensor("psc", [128, K], F32, kind="ExternalInput").ap()
        SH = nc.dram_tensor("psh", [128, K], F32, kind="ExternalInput").ap()
    if epilogue:
        R = nc.dram_tensor("resid", [T, N], F32, kind="ExternalInput").ap()
        GT = nc.dram_tensor("gate", [128, N], F32, kind="ExternalInput").ap()
    Y = nc.dram_tensor("Y", [T, N], F32, kind="ExternalOutput").ap()
    Wv = W.rearrange("(kc p) n -> p kc n", p=128)

    with contextlib.ExitStack() as es:
        S = Sched(nc, es)
        idf, b_idf = S.sb("idf", [128, 128], F32)
        idb, b_idb = S.sb("idb", [128, 128], BF16)
        S.dma("sp", idf[:], ID, None, b_idf)
        S.op("dve", lambda e: e.tensor_copy(out=idb[:], in_=idf[:]), [b_idf], [b_idb])
        if prologue:
            A, b_A = S.sb("A", [128, K], F32)
            B, b_B = S.sb("B", [128, K], F32)
            gt_, b_gt = S.sb("gtmp", [128, K], F32)
            S.dma("sp", A[:], SC, None, b_A)
            S.dma("sp", gt_[:], G, None, b_gt)
            S.dma("sp", B[:], SH, None, b_B)
            S.op("dve", lambda e: e.scalar_tensor_tensor(out=A[:], in0=A[:], scalar=1.0, in1=gt_[:],
                                                         op0=ALU.add, op1=ALU.mult), [b_A, b_gt], [b_A])
        if epilogue:
            gate, b_gate = S.sb("gate", [128, N], F32)
            S.dma("sp", gate[:], GT, None, b_gate)
        hnT, b_hnT = [], []
        for i in range(NT):
            t, b = S.sb("hnT%d" % i, [128, KC, 128], BF16)
            hnT.append(t)
            b_hnT.append(b)
        xs = [S.sb("x%d" % j, [128, K], F32) for j in range(2)]
        hb = [S.sb("hb%d" % j, [128, K], BF16) for j in range(2)]
        tmp, b_tmp = S.sb("tmp", [128, K], F32)
        st = [S.sb("st%d" % j, [128, 4], F32) for j in range(2)]
        pT = [S.ps("pT%d" % j, [128, 512], BF16) for j in range(2)]
        for i in range(NT):
            x, b_x = xs[i % 2]
            h, b_h = hb[i % 2]
            s_, b_s = st[i % 2]
            S.dma("sp", x[:], X[i * 128:(i + 1) * 128, :], None, b_x)
            if prologue:
                S.op("act", lambda e, x=x, s_=s_: e.activation(out=tmp[:], in_=x[:], func=AF.Square,
                                                               accum_out=s_[:, 0:1]), [b_x], [b_tmp, b_s])
                S.op("dve", lambda e, s_=s_: e.tensor_scalar(out=s_[:, 1:2], in0=s_[:, 0:1], scalar1=1.0 / K,
                                                             scalar2=EPS, op0=ALU.mult, op1=ALU.add), [b_s], [b_s])
                S.op("dve", lambda e, s_=s_: e.reciprocal(out=s_[:, 3:4], in_=s_[:, 1:2]), [b_s], [b_s])
                S.op("act", lambda e, s_=s_: e.sqrt(out=s_[:, 2:3], in_=s_[:, 3:4]), [b_s], [b_s])
                S.op("dve", lambda e, x=x, s_=s_: e.scalar_tensor_tensor(out=tmp[:], in0=x[:], scalar=s_[:, 2:3],
                                                                         in1=A[:], op0=ALU.mult, op1=ALU.mult),
                     [b_x, b_s, b_A], [b_tmp])
                S.op("dve", lambda e, h=h: e.tensor_tensor(out=h[:], in0=tmp[:], in1=B[:], op=ALU.add),
                     [b_tmp, b_B], [b_h])
            elif silu:
                S.op("act", lambda e, x=x: e.activation(out=tmp[:], in_=x[:], func=AF.Sigmoid), [b_x], [b_tmp])
                S.op("dve", lambda e, x=x, h=h: e.tensor_tensor(out=h[:], in0=x[:], in1=tmp[:], op=ALU.mult),
                     [b_x, b_tmp], [b_h])
            else:
                S.op("dve", lambda e, x=x, h=h: e.tensor_copy(out=h[:], in_=x[:]), [b_x], [b_h])
            for q in range(KC // 4):
                p, b_p = pT[q % 2]
                for j in range(4):
                    kc = q * 4 + j
                    S.op("pe", lambda e, p=p, h=h, kc=kc, j=j: e.transpose(
                        out=p[:, j * 128:(j + 1) * 128], in_=h[:, kc * 128:(kc + 1) * 128], identity=idb[:]),
                        [b_h, b_idb], [b_p])
                eng = "act" if q % 2 == 0 else "dve"
                if eng == "act":
                    S.op("act", lambda e, p=p, i=i, q=q: e.copy(
                        out=hnT[i][:, q * 4:(q + 1) * 4, :], in_=p[:].rearrange("p (a b) -> p a b", a=4)),
                        [b_p], [b_hnT[i]])
                else:
                    S.op("dve", lambda e, p=p, i=i, q=q: e.tensor_copy(
                        out=hnT[i][:, q * 4:(q + 1) * 4, :], in_=p[:].rearrange("p (a b) -> p a b", a=4)),
                        [b_p], [b_hnT[i]])
        wb = [S.sb("wb%d" % j, [128, KC, CG], BF16) for j in range(2)]
        po = [S.ps("po%d" % j, [128, CG], F32) for j in range(2)]
        ot = [S.sb("ot%d" % j, [128, CG], F32) for j in range(3)]
        if epilogue:
            rt = [S.sb("rt%d" % j, [128, CG], F32) for j in range(2)]
        n_out = 0
        for cg in range(ncg):
            c0 = cg * CG
            cw = min(CG, N - c0)
            w_b, b_wb = wb[cg % 2]
            half = KC // 2
            S.dma("pool", w_b[:, 0:half, 0:cw], Wv[:, 0:half, c0:c0 + cw], None, b_wb)
            S.dma("pool", w_b[:, half:KC, 0:cw], Wv[:, half:KC, c0:c0 + cw], None, b_wb)
            for i in range(NT):
                p, b_p = po[n_out % 2]
                o, b_o = ot[n_out % 3]
                if epilogue:
                    r, b_r = rt[n_out % 2]
                    S.dma("sp", r[:, 0:cw], R[i * 128:(i + 1) * 128, c0:c0 + cw], None, b_r)
                for kc in range(KC):
                    S.op("pe", lambda e, p=p, i=i, kc=kc, w_b=w_b, cw=cw: e.matmul(
                        p[:, 0:cw], hnT[i][:, kc, :], w_b[:, kc, 0:cw], start=(kc == 0), stop=(kc == KC - 1)),
                        [b_hnT[i], b_wb], [b_p], accumulate=(kc > 0))
                if epilogue:
                    S.op("dve", lambda e, o=o, p=p, c0=c0, cw=cw: e.tensor_tensor(
                        out=o[:, 0:cw], in0=p[:, 0:cw], in1=gate[:, c0:c0 + cw], op=ALU.mult),
                        [b_p, b_gate], [b_o])
                    S.op("pool", lambda e, o=o, r=r, cw=cw: e.tensor_tensor(
                        out=o[:, 0:cw], in0=o[:, 0:cw], in1=r[:, 0:cw], op=ALU.add), [b_o, b_r], [b_o])
                else:
                    if n_out % 2 == 0:
                        S.op("act", lambda e, o=o, p=p, cw=cw: e.copy(out=o[:, 0:cw], in_=p[:, 0:cw]),
                             [b_p], [b_o])
                    else:
                        S.op("dve", lambda e, o=o, p=p, cw=cw: e.tensor_copy(out=o[:, 0:cw], in_=p[:, 0:cw]),
                             [b_p], [b_o])
                S.dma("pool", Y[i * 128:(i + 1) * 128, c0:c0 + cw], o[:, 0:cw], b_o, None, sem_on="src")
                n_out += 1
        fin = [(b.dsem, b.dcnt) for _, b in ot]
        S.finish(fin)
    return nc


def gemm(X, W, pro=None, epi=None):
    Ttot, K = X.shape
    N = W.shape[1]
    T = Ttot // NCORES
    key = (T, K, N, pro is not None, epi is not None)
    if key not in _GEMM_CACHE:
        _GEMM_CACHE[key] = build_gemm(T, K, N, pro is not None, epi is not None)
    nc = _GEMM_CACHE[key]
    maps = []
    for c in range(NCORES):
        m = {"X": np.ascontiguousarray(X[c * T:(c + 1) * T]), "W": W, "ident": IDENT}
        if pro is not None:
            m["pg"], m["psc"], m["psh"] = pro
        if epi is not None:
            m["resid"] = np.ascontiguousarray(epi[0][c * T:(c + 1) * T])
            m["gate"] = epi[1]
        maps.append(m)
    res = _run(nc, maps)
    return np.concatenate([r["Y"] for r in res], axis=0)


def TT(S, eng, out, in0, in1, op, R, W):
    return S.op(eng, lambda e: e.tensor_tensor(out=out, in0=in0, in1=in1, op=op), R, W)


def TS(S, eng, out, in0, s1, s2, op0, op1=None, R=(), W=(), accum=None):
    if op1 is None:
        return S.op(eng, lambda e: e.tensor_scalar(out=out, in0=in0, scalar1=s1, scalar2=None, op0=op0), R, W)
    if accum is None:
        return S.op(eng, lambda e: e.tensor_scalar(out=out, in0=in0, scalar1=s1, scalar2=s2, op0=op0, op1=op1), R, W)
    return S.op(eng, lambda e: e.tensor_scalar(out=out, in0=in0, scalar1=s1, scalar2=s2, op0=op0, op1=op1,
                                               accum_out=accum), R, W)


def STT(S, eng, out, in0, scalar, in1, op0, op1, R, W):
    return S.op(eng, lambda e: e.scalar_tensor_tensor(out=out, in0=in0, scalar=scalar, in1=in1, op0=op0, op1=op1),
                R, W)


def ACT(S, out, in_, func, R, W, bias=None, scale=1.0, accum=None):
    kw = {}
    if bias is not None:
        kw["bias"] = bias
    if accum is not None:
        kw["accum_out"] = accum
    return S.op("act", lambda e: e.activation(out=out, in_=in_, func=func, scale=scale, **kw), R, W)


def CP(S, eng, out, in_, R, W):
    if eng == "act":
        return S.op("act", lambda e: e.copy(out=out, in_=in_), R, W)
    return S.op(eng, lambda e: e.tensor_copy(out=out, in_=in_), R, W)


def MM(S, out, lhsT, rhs, start, stop, R, W):
    return S.op("pe", lambda e: e.matmul(out, lhsT, rhs, start=start, stop=stop), R, W, accumulate=not start)


def TR(S, out, in_, ident, R, W, acc=False):
    return S.op("pe", lambda e: e.transpose(out=out, in_=in_, identity=ident), R, W, accumulate=acc)


def RED(S, eng, out, in_, op, R, W):
    return S.op(eng, lambda e: e.tensor_reduce(out=out, in_=in_, axis=AX.X, op=op), R, W)


def RSTD(S, out, tmp, ms, R, W):
    S.op("dve", lambda e: e.reciprocal(out=tmp, in_=ms), R, W)
    S.op("act", lambda e: e.sqrt(out=out, in_=tmp), W, W)


def load_const(S, name, dram_ap, shape, dtype=F32, q="sp", cast=None):
    t, b = S.sb(name, shape, dtype)
    S.dma(q, t[:], dram_ap, None, b)
    if cast is not None:
        t2, b2 = S.sb(name + "_c", shape, cast)
        CP(S, "dve", t2[:], t[:], [b], [b2])
        return t2, b2
    return t, b


def layer_norm_rows(S, pfx, dst, src, src_bufs, dst_bufs, D, g_t, b_t, g_b, b_b, st, b_st, junk, b_junk, tmp, b_tmp):
    ACT(S, tmp, src, AF.Identity, src_bufs + [b_st], [b_tmp, b_st], accum=st[:, 0:1])
    ACT(S, junk, src, AF.Square, src_bufs + [b_st], [b_junk, b_st], accum=st[:, 1:2])
    TS(S, "dve", st[:, 2:3], st[:, 0:1], 1.0 / D, None, ALU.mult, R=[b_st], W=[b_st])
    TT(S, "dve", st[:, 3:4], st[:, 2:3], st[:, 2:3], ALU.mult, [b_st], [b_st])
    STT(S, "dve", st[:, 4:5], st[:, 1:2], 1.0 / D, st[:, 3:4], ALU.mult, ALU.subtract, [b_st], [b_st])
    TS(S, "dve", st[:, 4:5], st[:, 4:5], EPS, None, ALU.add, R=[b_st], W=[b_st])
    RSTD(S, st[:, 5:6], st[:, 6:7], st[:, 4:5], [b_st], [b_st])
    TS(S, "dve", tmp, tmp, st[:, 2:3], st[:, 5:6], ALU.subtract, ALU.mult, R=[b_tmp, b_st], W=[b_tmp])
    TT(S, "dve", tmp, tmp, g_t, ALU.mult, [b_tmp, g_b], [b_tmp])
    TT(S, "dve", dst, tmp, b_t, ALU.add, [b_tmp, b_b], dst_bufs)


def build_conv(T):
    nc = bass.Bass("TRN2", target_bir_lowering=False)
    NT = T // 128
    TH = T + 32
    AT = nc.dram_tensor("aT", [4, 128, TH], F32, kind="ExternalInput").ap()
    GT = nc.dram_tensor("gT", [4, 128, TH], F32, kind="ExternalInput").ap()
    CW = nc.dram_tensor("cw", [4, 128, 32], F32, kind="ExternalInput").ap()
    LG = nc.dram_tensor("lng", [128, 512], F32, kind="ExternalInput").ap()
    LB = nc.dram_tensor("lnb", [128, 512], F32, kind="ExternalInput").ap()
    ID = nc.dram_tensor("ident", [128, 128], F32, kind="ExternalInput").ap()
    Y = nc.dram_tensor("Y", [T, 512], F32, kind="ExternalOutput").ap()
    with contextlib.ExitStack() as es:
        S = Sched(nc, es)
        idf, b_idf = load_const(S, "idf", ID, [128, 128])
        lg, b_lg = load_const(S, "lg", LG, [128, 512])
        lb, b_lb = load_const(S, "lb", LB, [128, 512])
        pz = [S.ps("pz%d" % i, [128, 512], F32) for i in range(NT)]
        for cc in range(4):
            a, b_a = S.sb("a%d" % cc, [128, TH], F32)
            g, b_g = S.sb("g%d" % cc, [128, TH], F32)
            w, b_w = S.sb("w%d" % cc, [128, 32], F32)
            S.dma("sp", a[:], AT[cc], None, b_a)
            S.dma("act", g[:], GT[cc], None, b_g)
            S.dma("sp", w[:], CW[cc], None, b_w)
            ACT(S, g[:], g[:], AF.Sigmoid, [b_g], [b_g])
            TT(S, "dve", a[:], a[:], g[:], ALU.mult, [b_a, b_g], [b_a])
            accA, b_accA = S.sb("accA%d" % cc, [128, T], F32)
            TS(S, "dve", accA[:], a[:, 2:2 + T], w[:, 0:1], w[:, 31:32], ALU.mult, ALU.add, R=[b_a, b_w], W=[b_accA])
            for k in range(1, 31):
                STT(S, "dve", accA[:], a[:, 2 + k:2 + k + T], w[:, k:k + 1], accA[:], ALU.mult, ALU.add,
                    [b_a, b_w, b_accA], [b_accA])
            for i in range(NT):
                TR(S, pz[i][0][:, cc * 128:(cc + 1) * 128], accA[:, i * 128:(i + 1) * 128], idf[:],
                   [b_accA, b_idf], [pz[i][1]], acc=(cc > 0))
        outs = [S.sb("o%d" % j, [128, 512], F32) for j in range(2)]
        tmps = [S.sb("t%d" % j, [128, 512], F32) for j in range(2)]
        junk, b_junk = S.sb("junk", [128, 512], F32)
        sts = [S.sb("st%d" % j, [128, 8], F32) for j in range(2)]
        for i in range(NT):
            o, b_o = outs[i % 2]
            t, b_t = tmps[i % 2]
            st, b_st = sts[i % 2]
            layer_norm_rows(S, "c", t[:], pz[i][0][:], [pz[i][1]], [b_t], 512, lg[:], lb[:], b_lg, b_lb,
                            st, b_st, junk[:], b_junk, t[:], b_t)
            ACT(S, junk[:], t[:], AF.Sigmoid, [b_t], [b_junk])
            TT(S, "dve", o[:], t[:], junk[:], ALU.mult, [b_t, b_junk], [b_o])
            S.dma("pool", Y[i * 128:(i + 1) * 128, :], o[:], b_o, None, sem_on="src")
        S.finish([(b.dsem, b.dcnt) for _, b in outs])
    return nc


def build_sgu(T):
    nc = bass.Bass("TRN2", target_bir_lowering=False)
    NT = T // 128
    U = nc.dram_tensor("u", [T, 512], F32, kind="ExternalInput").ap()
    V = nc.dram_tensor("v", [T, 512], F32, kind="ExternalInput").ap()
    WT = nc.dram_tensor("wT", [128, 4, 128], F32, kind="ExternalInput").ap()
    MK = nc.dram_tensor("mask", [128, 4, 128], F32, kind="ExternalInput").ap()
    BS = nc.dram_tensor("bs", [128, 4], F32, kind="ExternalInput").ap()
    LG = nc.dram_tensor("lng", [128, 512], F32, kind="ExternalInput").ap()
    LB = nc.dram_tensor("lnb", [128, 512], F32, kind="ExternalInput").ap()
    Y = nc.dram_tensor("Y", [T, 512], F32, kind="ExternalOutput").ap()
    with contextlib.ExitStack() as es:
        S = Sched(nc, es)
        lg, b_lg = load_const(S, "lg", LG, [128, 512])
        lb, b_lb = load_const(S, "lb", LB, [128, 512])
        wt, b_wt = load_const(S, "wt", WT, [128, 4, 128])
        mk, b_mk = load_const(S, "mk", MK, [128, 4, 128])
        bs, b_bs = load_const(S, "bs", BS, [128, 4])
        wm, b_wm = S.sb("wm", [128, 4, 128], BF16)
        TT(S, "dve", wm[:], wt[:], mk[:], ALU.mult, [b_wt, b_mk], [b_wm])
        us = [S.sb("u%d" % j, [128, 512], F32) for j in range(2)]
        vs = [S.sb("v%d" % j, [128, 512], F32) for j in range(2)]
        vn = [S.sb("vn%d" % j, [128, 512], BF16) for j in range(2)]
        tmps = [S.sb("t%d" % j, [128, 512], F32) for j in range(2)]
        outs = [S.sb("o%d" % j, [128, 512], F32) for j in range(2)]
        junk, b_junk = S.sb("junk", [128, 512], F32)
        sts = [S.sb("st%d" % j, [128, 8], F32) for j in range(2)]
        pss = [S.ps("ps%d" % j, [128, 512], F32) for j in range(2)]
        for i in range(NT):
            u, b_u = us[i % 2]
            v, b_v = vs[i % 2]
            n, b_n = vn[i % 2]
            t, b_t = tmps[i % 2]
            o, b_o = outs[i % 2]
            st, b_st = sts[i % 2]
            ps, b_ps = pss[i % 2]
            S.dma("sp", u[:], U[i * 128:(i + 1) * 128, :], None, b_u)
            S.dma("act", v[:], V[i * 128:(i + 1) * 128, :], None, b_v)
            layer_norm_rows(S, "s", n[:], v[:], [b_v], [b_n], 512, lg[:], lb[:], b_lg, b_lb,
                            st, b_st, junk[:], b_junk, t[:], b_t)
            for h in range(4):
                MM(S, ps[:, h * 128:(h + 1) * 128], wm[:, h, :], n[:, h * 128:(h + 1) * 128], True, True,
                   [b_wm, b_n], [b_ps])
            for h in range(4):
                STT(S, "dve", o[:, h * 128:(h + 1) * 128], ps[:, h * 128:(h + 1) * 128], bs[:, h:h + 1],
                    u[:, h * 128:(h + 1) * 128], ALU.add, ALU.mult, [b_ps, b_bs, b_u], [b_o])
            S.dma("pool", Y[i * 128:(i + 1) * 128, :], o[:], b_o, None, sem_on="src")
        S.finish([(b.dsem, b.dcnt) for _, b in outs])
    return nc


_CACHE = {}


def _get(key, fn):
    if key not in _CACHE:
        _CACHE[key] = fn()
    return _CACHE[key]


def run_conv(pa, conv_w, conv_b, ln_g, ln_b):
    Ttot = pa.shape[0]
    T = Ttot // NCORES
    nc = _get(("conv", T), lambda: build_conv(T))
    aT = np.zeros((512, Ttot + 32), np.float32)
    gT = np.zeros((512, Ttot + 32), np.float32)
    aT[:, 32:] = pa[:, 0:512].T
    gT[:, 32:] = pa[:, 512:1024].T
    cw = np.zeros((512, 32), np.float32)
    cw[:, 0:31] = conv_w.T
    cw[:, 31] = conv_b
    cw = cw.reshape(4, 128, 32)
    lg, lb = rep128(ln_g), rep128(ln_b)
    maps = []
    for c in range(NCORES):
        maps.append({"aT": np.ascontiguousarray(aT[:, c * T:c * T + T + 32]).reshape(4, 128, T + 32),
                     "gT": np.ascontiguousarray(gT[:, c * T:c * T + T + 32]).reshape(4, 128, T + 32),
                     "cw": cw, "lng": lg, "lnb": lb, "ident": IDENT})
    res = _run(nc, maps)
    return np.concatenate([r["Y"] for r in res], axis=0)


def run_sgu(pb, ln_g, ln_b, sgu_w, sgu_b):
    Ttot = pb.shape[0]
    T = Ttot // NCORES
    nc = _get(("sgu", T), lambda: build_sgu(T))
    wT = np.ascontiguousarray(np.transpose(sgu_w, (2, 0, 1)))
    jj = np.arange(128)
    mask = np.ascontiguousarray(np.broadcast_to((jj[:, None] <= jj[None, :]).astype(np.float32)[:, None, :],
                                                (128, 4, 128)))
    bs = np.ascontiguousarray(sgu_b.T)
    lg, lb = rep128(ln_g), rep128(ln_b)
    maps = []
    for c in range(NCORES):
        maps.append({"u": np.ascontiguousarray(pb[c * T:(c + 1) * T, 0:512]),
                     "v": np.ascontiguousarray(pb[c * T:(c + 1) * T, 512:1024]),
                     "wT": wT, "mask": mask, "bs": bs, "lng": lg, "lnb": lb})
    res = _run(nc, maps)
    return np.concatenate([r["Y"] for r in res], axis=0)


PREP_IN = 2644
PREP_OUT = 2516
I32 = mybir.dt.int32
TWO_PI = float(2.0 * np.pi)


def _rope_inplace(S, Y, b_Y, H, half, c, s, b_sn, tmp, b_tmp):
    Y1 = Y[:, :, 0:half]
    Y2 = Y[:, :, half:2 * half]
    cb = c.unsqueeze(1).to_broadcast([128, H, half])
    sb_ = s.unsqueeze(1).to_broadcast([128, H, half])
    t = [tmp[:, k, 0:H * half].rearrange("p (h d) -> p h d", h=H) for k in range(4)]
    TT(S, "dve", t[0], Y1, cb, ALU.mult, [b_Y, b_sn], [b_tmp])
    TT(S, "dve", t[1], Y2, sb_, ALU.mult, [b_Y, b_sn], [b_tmp])
    TT(S, "dve", t[2], Y2, cb, ALU.mult, [b_Y, b_sn], [b_tmp])
    TT(S, "dve", t[3], Y1, sb_, ALU.mult, [b_Y, b_sn], [b_tmp])
    TT(S, "dve", Y1, t[0], t[1], ALU.subtract, [b_tmp], [b_Y])
    TT(S, "dve", Y2, t[2], t[3], ALU.add, [b_tmp], [b_Y])


def build_prep(T):
    nc = bass.Bass("TRN2", target_bir_lowering=False)
    NT = T // 128
    X = nc.dram_tensor("X", [T, PREP_IN], F32, kind="ExternalInput").ap()
    POS = nc.dram_tensor("pos", [T, 1], I32, kind="ExternalInput").ap()
    GN = nc.dram_tensor("gn", [128, 11, 128], F32, kind="ExternalInput").ap()
    INV = nc.dram_tensor("inv", [128, 48], F32, kind="ExternalInput").ap()
    PH = nc.dram_tensor("ph", [128, 48], F32, kind="ExternalInput").ap()
    Y = nc.dram_tensor("Y", [T, PREP_OUT], F32, kind="ExternalOutput").ap()
    with contextlib.ExitStack() as es:
        S = Sched(nc, es)
        gn, b_gn = load_const(S, "gn", GN, [128, 11, 128])
        inv, b_inv = load_const(S, "inv", INV, [128, 48])
        ph, b_ph = load_const(S, "ph", PH, [128, 48])
        xs = [S.sb("x%d" % j, [128, PREP_IN], F32) for j in range(2)]
        os_ = [S.sb("o%d" % j, [128, PREP_OUT], F32) for j in range(2)]
        pis = [S.sb("pi%d" % j, [128, 1], I32) for j in range(2)]
        sq, b_sq = S.sb("sq", [128, 5, 128], F32)
        st, b_st = S.sb("st", [128, 4, 8], F32)
        ang, b_ang = S.sb("ang", [128, 4, 48], F32)
        ki, b_ki = S.sb("ki", [128, 48], I32)
        sn, b_sn = S.sb("sn", [128, 48], F32)
        rt, b_rt = S.sb("rt", [128, 4, 96], F32)
        for i in range(NT):
            x, b_x = xs[i % 2]
            o, b_o = os_[i % 2]
            pi_, b_pi = pis[i % 2]
            S.dma("sp", x[:], X[i * 128:(i + 1) * 128, :], None, b_x)
            S.dma("act", pi_[:], POS[i * 128:(i + 1) * 128, :], None, b_pi)
            CP(S, "dve", ang[:, 3, 0:1], pi_[:], [b_pi], [b_ang])
            STT(S, "dve", ang[:, 0, :], inv[:], ang[:, 3, 0:1], ph[:], ALU.mult, ALU.add, [b_inv, b_ang, b_ph], [b_ang])
            TS(S, "dve", ki[:], ang[:, 0, :], 1.0 / TWO_PI, None, ALU.mult, R=[b_ang], W=[b_ki])
            CP(S, "dve", ang[:, 1, :], ki[:], [b_ki], [b_ang])
            STT(S, "dve", ang[:, 0, :], ang[:, 1, :], -TWO_PI, ang[:, 0, :], ALU.mult, ALU.add, [b_ang], [b_ang])
            TS(S, "dve", ang[:, 1, :], ang[:, 0, :], float(np.pi), -TWO_PI, ALU.is_gt, ALU.mult, R=[b_ang], W=[b_ang])
            TT(S, "dve", ang[:, 0, :], ang[:, 0, :], ang[:, 1, :], ALU.add, [b_ang], [b_ang])
            TS(S, "dve", ang[:, 1, :], ang[:, 0, :], float(-np.pi), TWO_PI, ALU.is_lt, ALU.mult, R=[b_ang], W=[b_ang])
            TT(S, "dve", ang[:, 0, :], ang[:, 0, :], ang[:, 1, :], ALU.add, [b_ang], [b_ang])
            TS(S, "dve", ang[:, 0, :], ang[:, 0, :], -3.14159, 3.14159, ALU.max, ALU.min, R=[b_ang], W=[b_ang])
            ACT(S, sn[:], ang[:, 0, :], AF.Sin, [b_ang], [b_sn])
            sin16, sin8, cos16, cos8 = sn[:, 0:16], sn[:, 16:24], sn[:, 24:40], sn[:, 40:48]
            groups = [
                (x[:, 0:512].rearrange("p (h d) -> p h d", h=4), 4, gn[:, 0:4, :],
                 o[:, 0:512].rearrange("p (h d) -> p h d", h=4)),
                (x[:, 768:1280].rearrange("p (a b) -> p a b", b=256)[:, :, 0:128], 2, gn[:, 4:6, :],
                 o[:, 1024:1280].rearrange("p (h d) -> p h d", h=2)),
                (x[:, 1292:1932].rearrange("p (h d) -> p h d", h=5), 5, gn[:, 6:11, :],
                 o[:, 1280:1920].rearrange("p (h d) -> p h d", h=5)),
            ]
            for gi, (src, H, gain, dst) in enumerate(groups):
                TT(S, "dve", sq[:, 0:H, :], src, src, ALU.mult, [b_x], [b_sq])
                RED(S, "dve", st[:, 0, 0:H], sq[:, 0:H, :], ALU.add, [b_sq], [b_st])
                TS(S, "dve", st[:, 1, 0:H], st[:, 0, 0:H], 1.0 / 128, EPS, ALU.mult, ALU.add, R=[b_st], W=[b_st])
                RSTD(S, st[:, 2, 0:H], st[:, 3, 0:H], st[:, 1, 0:H], [b_st], [b_st])
                TT(S, "dve", dst, src, st[:, 2, 0:H].unsqueeze(2).to_broadcast([128, H, 128]), ALU.mult,
                   [b_x, b_st], [b_o])
                TT(S, "dve", dst, dst, gain, ALU.mult, [b_o, b_gn], [b_o])
            CP(S, "pool", o[:, 512:1024], o[:, 0:512], [b_o], [b_o])
            _rope_inplace(S, o[:, 512:1024].rearrange("p (h d) -> p h d", h=4), b_o, 4, 16, cos16, sin16, b_sn, rt, b_rt)
            _rope_inplace(S, o[:, 1024:1280].rearrange("p (h d) -> p h d", h=2), b_o, 2, 16, cos16, sin16, b_sn, rt, b_rt)
            _rope_inplace(S, o[:, 1280:1920].rearrange("p (h d) -> p h d", h=5), b_o, 5, 16, cos16, sin16, b_sn, rt, b_rt)
            CP(S, "pool", o[:, 1920:2496], x[:, 2060:2636], [b_x], [b_o])
            _rope_inplace(S, o[:, 1920:2496].rearrange("p (h d) -> p h d", h=9), b_o, 9, 8, cos8, sin8, b_sn, rt, b_rt)
            ACT(S, o[:, 2496:2508], x[:, 1280:1292], AF.Sigmoid, [b_x], [b_o])
            TS(S, "dve", o[:, 2508:2516], x[:, 2636:2644], float(8 ** -0.5), None, ALU.mult, R=[b_x], W=[b_o])
            S.dma("pool", Y[i * 128:(i + 1) * 128, :], o[:], b_o, None, sem_on="src")
        S.finish([(b.dsem, b.dcnt) for _, b in os_])
    return nc


def run_prep(pcd, positions, nsa_q_g, nsa_k_g, dsa_q_g, dsa_k_g):
    Ttot = pcd.shape[0]
    T = Ttot // NCORES
    nc = _get(("prep", T), lambda: build_prep(T))
    gl = [nsa_q_g] * 4 + [nsa_k_g[1], nsa_k_g[2]] + [dsa_q_g] * 4 + [dsa_k_g]
    gn = np.ascontiguousarray(np.broadcast_to(np.stack(gl)[None], (128, 11, 128))).astype(np.float32)
    inv16 = (500000.0 ** (-np.arange(16, dtype=np.float32) / np.float32(16))).astype(np.float32)
    inv8 = (500000.0 ** (-np.arange(8, dtype=np.float32) / np.float32(8))).astype(np.float32)
    inv = rep128(np.concatenate([inv16, inv8, inv16, inv8]))
    ph = rep128(np.concatenate([np.zeros(24, np.float32), np.full(24, np.pi / 2, np.float32)]))
    pos = positions.reshape(-1, 1).astype(np.int32)
    maps = []
    for c in range(NCORES):
        maps.append({"X": np.ascontiguousarray(pcd[c * T:(c + 1) * T]), "pos": np.ascontiguousarray(pos[c * T:(c + 1) * T]),
                     "gn": gn, "inv": inv, "ph": ph})
    res = _run(nc, maps)
    return np.concatenate([r["Y"] for r in res], axis=0)


NIT = 15
NEGBIG = -1.0e30


def _load_cast_cols(S, dst, b_dst, src_ap, P, ncols, stages=None, piece=4096, tag=""):
    for c0 in range(0, ncols, piece):
        cw = min(piece, ncols - c0)
        S.dma("pool", dst[0:P, c0:c0 + cw], src_ap[:, c0:c0 + cw], None, b_dst)


def build_dsa(NJ):
    nc = bass.Bass("TRN2", target_bir_lowering=False)
    NB = NJ * 8
    TT_ = NB * 128
    IQT = nc.dram_tensor("iqT", [NJ, 64, 8 * 128], F32, kind="ExternalInput").ap()
    IW = nc.dram_tensor("iw", [NJ, 128, 8], F32, kind="ExternalInput").ap()
    QT = nc.dram_tensor("qT", [NJ, 128, 512], F32, kind="ExternalInput").ap()
    IKT = nc.dram_tensor("ikT", [64, TT_], F32, kind="ExternalInput").ap()
    KT = nc.dram_tensor("kT", [128, TT_], F32, kind="ExternalInput").ap()
    VA = nc.dram_tensor("va", [128, NB * 129], F32, kind="ExternalInput").ap()
    NM = nc.dram_tensor("negmask", [128, 1024], F32, kind="ExternalInput").ap()
    P2 = nc.dram_tensor("pow2", [128, NIT], F32, kind="ExternalInput").ap()
    ID = nc.dram_tensor("ident", [128, 128], F32, kind="ExternalInput").ap()
    Y = nc.dram_tensor("Y", [NJ, 128, 512], F32, kind="ExternalOutput").ap()
    scale = float(128 ** -0.5)
    with contextlib.ExitStack() as es:
        S = Sched(nc, es)
        idb, b_idb = load_const(S, "id", ID, [128, 128], cast=BF16)
        nm, b_nm = load_const(S, "nm", NM, [128, 1024])
        p2, b_p2 = load_const(S, "p2", P2, [128, NIT])
        stages = None
        ikT, b_ikT = S.sb("ikT", [64, TT_], BF16)
        kT, b_kT = S.sb("kT", [128, TT_], BF16)
        va, b_va = S.sb("va", [128, NB * 129], BF16)
        _load_cast_cols(S, ikT, b_ikT, IKT, 64, TT_, stages)
        _load_cast_cols(S, kT, b_kT, KT, 128, TT_, stages)
        _load_cast_cols(S, va, b_va, VA, 128, NB * 129, stages, piece=2064)
        va3 = va[:].rearrange("p (b d) -> p b d", d=129)
        score, b_score = S.sb("score", [128, TT_], F32)
        maskq, b_maskq = S.sb("maskq", [128, TT_], BF16)
        maskT, b_maskT = S.sb("maskT", [128, NB, 128], BF16)
        iqf = [S.sb("iqf%d" % k, [64, 1024], F32) for k in range(2)]
        iqb = [S.sb("iqb%d" % k, [64, 1024], BF16) for k in range(2)]
        qf = [S.sb("qf%d" % k, [128, 512], F32) for k in range(2)]
        qb = [S.sb("qb%d" % k, [128, 512], BF16) for k in range(2)]
        iws = [S.sb("iw%d" % k, [128, 8], F32) for k in range(2)]
        rbuf = [S.sb("r%d" % k, [128, 512], F32) for k in range(4)]
        ebuf = [S.sb("e%d" % k, [128, 512], F32) for k in range(4)]
        pbuf = [S.sb("pb%d" % k, [128, 4, 128], BF16) for k in range(4)]
        obuf = [S.sb("ob%d" % k, [128, 512], F32) for k in range(2)]
        bs_, b_bs = S.sb("bis", [128, 8], F32)
        hd, b_hd = S.sb("hd", [128, NIT], F32)
        cnt, b_cnt = S.sb("cnt", [128, NIT], F32)
        zz, b_zz = S.sb("zz", [128, 8], F32)
        sps = [S.ps("sps%d" % k, [128, 512], F32) for k in range(4)]
        stp = sps
        tps, b_tps = S.ps("tps", [128, 512], BF16)
        ops_, b_ops = S.ps("ops", [128, 4, 256], F32)
        nr = 0
        for j in range(NJ):
            NBj = 8 * j + 8
            L = NBj * 128
            NCH = NBj // 4
            iq_f, b_iqf = iqf[j % 2]
            iq_b, b_iqb = iqb[j % 2]
            q_f, b_qf = qf[j % 2]
            q_b, b_qb = qb[j % 2]
            iw, b_iw = iws[j % 2]
            S.dma("sp", iq_f[:], IQT[j], None, b_iqf)
            S.dma("act", q_f[:], QT[j], None, b_qf)
            S.dma("sp", iw[:], IW[j], None, b_iw)
            CP(S, "pool", iq_b[:], iq_f[:], [b_iqf], [b_iqb])
            CP(S, "pool", q_b[:], q_f[:], [b_qf], [b_qb])
            for ch in range(NCH):
                sc_ch = score[:, ch * 512:(ch + 1) * 512]
                for h in range(8):
                    ps, b_ps = sps[nr % 4]
                    r, b_r = rbuf[nr % 4]
                    nr += 1
                    MM(S, ps[:], iq_b[:, h * 128:(h + 1) * 128], ikT[:, ch * 512:(ch + 1) * 512], True, True,
                       [b_iqb, b_ikT], [b_ps])
                    ACT(S, r[:], ps[:], AF.Relu, [b_ps], [b_r], scale=0.125)
                    if h == 0:
                        TS(S, "dve", sc_ch, r[:], iw[:, 0:1], None, ALU.mult, R=[b_r, b_iw], W=[b_score])
                    else:
                        STT(S, "dve", sc_ch, r[:], iw[:, h:h + 1], sc_ch, ALU.mult, ALU.add,
                            [b_r, b_iw, b_score], [b_score])
            RED(S, "dve", bs_[:, 0:1], score[:, 0:L], ALU.max, [b_score], [b_bs])
            RED(S, "dve", bs_[:, 1:2], score[:, 0:L], ALU.min, [b_score], [b_bs])
            TT(S, "dve", score[:, L - 1024:L], score[:, L - 1024:L], nm[:], ALU.add, [b_score, b_nm], [b_score])
            TS(S, "dve", bs_[:, 2:3], bs_[:, 1:2], -1.0, None, ALU.add, R=[b_bs], W=[b_bs])
            STT(S, "dve", bs_[:, 3:4], bs_[:, 0:1], 2.0, bs_[:, 1:2], ALU.add, ALU.subtract, [b_bs], [b_bs])
            TS(S, "dve", hd[:], p2[:], bs_[:, 3:4], None, ALU.mult, R=[b_p2, b_bs], W=[b_hd])
            S.op("dve", lambda e: e.memset(cnt[:], 0.0), [], [b_cnt])
            for k in range(NIT):
                TT(S, "dve", bs_[:, 4:5], bs_[:, 2:3], hd[:, k:k + 1], ALU.add, [b_bs, b_hd], [b_bs])
                TS(S, "dve", maskq[:, 0:L], score[:, 0:L], bs_[:, 4:5], None, ALU.is_ge, ALU.add,
                   R=[b_score, b_bs, b_cnt], W=[b_maskq, b_cnt], accum=cnt[:, k:k + 1])
                TS(S, "dve", bs_[:, 5:6], cnt[:, k:k + 1], 255.5, hd[:, k:k + 1], ALU.is_gt, ALU.mult,
                   R=[b_cnt, b_hd], W=[b_bs])
                TT(S, "dve", bs_[:, 2:3], bs_[:, 2:3], bs_[:, 5:6], ALU.add, [b_bs], [b_bs])
            TS(S, "dve", maskq[:, 0:L], score[:, 0:L], bs_[:, 2:3], None, ALU.is_ge, R=[b_score, b_bs], W=[b_maskq])
            for g4 in range(NBj // 4):
                for t in range(4):
                    kb = g4 * 4 + t
                    TR(S, tps[:, t * 128:(t + 1) * 128], maskq[:, kb * 128:(kb + 1) * 128], idb[:],
                       [b_maskq, b_idb], [b_tps], acc=(t > 0))
                CP(S, "act", maskT[:, g4 * 4:(g4 + 1) * 4, :], tps[:].rearrange("p (a b) -> p a b", a=4),
                   [b_tps], [b_maskT])
            pend = []
            for kb in range(NBj):
                st_, b_st = stp[kb % 4]
                e_, b_e = ebuf[kb % 4]
                p_, b_p = pbuf[kb % 4]
                MM(S, st_[:], kT[:, kb * 128:(kb + 1) * 128], q_b[:], True, True, [b_kT, b_qb], [b_st])
                ACT(S, e_[:], st_[:], AF.Exp, [b_st], [b_e], scale=scale)
                TT(S, "dve", p_[:], e_[:].rearrange("p (h q) -> p h q", h=4),
                   maskT[:, kb, :].unsqueeze(1).to_broadcast([128, 4, 128]), ALU.mult, [b_e, b_maskT], [b_p])
                pend.append((kb, p_, b_p))
                if len(pend) > 2:
                    kb2, p2_, b_p2_ = pend.pop(0)
                    for h in range(4):
                        MMG(S, ops_[:, h, 0:129], p2_[:, h, :], va3[:, kb2, :], kb2 == 0 and h % 2 == 0,
                            [b_p2_, b_va], [b_ops])
            while pend:
                kb2, p2_, b_p2_ = pend.pop(0)
                for h in range(4):
                    MMG(S, ops_[:, h, 0:129], p2_[:, h, :], va3[:, kb2, :], kb2 == 0 and h % 2 == 0,
                        [b_p2_, b_va], [b_ops])
            o_, b_o = obuf[j % 2]
            TS(S, "dve", zz[:, 0:4], ops_[:, :, 128], 1e-30, None, ALU.max, R=[b_ops], W=[b_zz])
            S.op("dve", lambda e: e.reciprocal(out=zz[:, 4:8], in_=zz[:, 0:4]), [b_zz], [b_zz])
            TT(S, "dve", o_[:].rearrange("p (h d) -> p h d", h=4), ops_[:, :, 0:128],
               zz[:, 4:8].unsqueeze(2).to_broadcast([128, 4, 128]), ALU.mult, [b_ops, b_zz], [b_o])
            S.dma("pool", Y[j], o_[:], b_o, None, sem_on="src")
        S.finish([(b.dsem, b.dcnt) for _, b in obuf])
    return nc


def causal_negmask(c):
    m = np.zeros((128, 8, 128), np.float32)
    q = np.arange(128)
    for r in range(8):
        if r == c:
            m[:, r, :] = np.where(q[None, :] <= q[:, None], 0.0, NEGBIG)
        elif r > c:
            m[:, r, :] = NEGBIG
    return m.reshape(128, 1024)


def own_tiles_T(a, H, D, c, NJ):
    out = []
    for j in range(NJ):
        g = 8 * j + c
        t = a[g * 128:(g + 1) * 128].reshape(128, H, D)
        out.append(np.transpose(t, (2, 1, 0)).reshape(D, H * 128))
    return np.ascontiguousarray(np.stack(out))


def v_aug(v):
    NB = v.shape[0] // 128
    t = np.ones((128, NB, 129), np.float32)
    t[:, :, 0:128] = np.transpose(v.reshape(NB, 128, 128), (1, 0, 2))
    return t.reshape(128, NB * 129)


def run_dsa(Yp, vd):
    Ttot = Yp.shape[0]
    NJ = Ttot // (128 * NCORES)
    nc = _get(("dsa", NJ), lambda: build_dsa(NJ))
    ikT = np.ascontiguousarray(Yp[:, 2432:2496].T)
    kT = np.ascontiguousarray(Yp[:, 1792:1920].T)
    va = v_aug(vd)
    pow2 = rep128(2.0 ** -(np.arange(NIT, dtype=np.float32) + 1))
    maps = []
    for c in range(NCORES):
        own = [8 * j + c for j in range(NJ)]
        maps.append({"iqT": own_tiles_T(Yp[:, 1920:2432], 8, 64, c, NJ),
                     "iw": np.ascontiguousarray(np.stack([Yp[g * 128:(g + 1) * 128, 2508:2516] for g in own])),
                     "qT": own_tiles_T(Yp[:, 1280:1792], 4, 128, c, NJ),
                     "ikT": ikT, "kT": kT, "va": va, "negmask": causal_negmask(c), "pow2": pow2, "ident": IDENT})
    res = _run(nc, maps)
    out = np.zeros((Ttot, 512), np.float32)
    for c in range(NCORES):
        for j in range(NJ):
            g = 8 * j + c
            out[g * 128:(g + 1) * 128] = res[c]["Y"][j]
    return out


GELU_C = 1.5957691216057308


def MMG(S, out, lhsT, rhs, first, R, W):
    return S.op("pe", lambda e: e.matmul(out, lhsT, rhs, start=first, stop=False, skip_group_check=True),
                R, W, accumulate=True)


def build_nsa(NJ):
    nc = bass.Bass("TRN2", target_bir_lowering=False)
    NB = NJ * 8
    TT_ = NB * 128
    NCMP = (TT_ - 32) // 16 + 1
    NCC = (NCMP + 127) // 128
    NCP = NCC * 128
    dr = lambda name, shape: nc.dram_tensor(name, shape, F32, kind="ExternalInput").ap()
    QNT = dr("qnT", [NJ, 128, 512])
    QRT = dr("qrT", [NJ, 128, 512])
    GATES = dr("gates", [NJ, 128, 12])
    CMASK = dr("cmask", [NJ, 128, 4 * 128])
    SELB = dr("selb", [NJ, 128, 128])
    KCT = dr("kcmpT", [128, TT_])
    VCT = dr("vcmpT", [128, TT_])
    W1 = dr("w1", [128, 2 * 32 * 128])
    POST = dr("posT", [128, 64])
    W2 = dr("w2", [128, 256])
    KG0 = dr("kg0", [128, 128])
    KST = dr("ksT", [128, TT_])
    KWT = dr("kwT", [128, TT_])
    VSA = dr("vsa", [128, NB * 129])
    VWA = dr("vwa", [128, NB * 129])
    OV = dr("ov", [128, 512])
    EX = dr("expE", [128, NB * 128])
    CAUS = dr("causT", [128, 1024])
    WINM = dr("winT", [128, 1536])
    ID = dr("ident", [128, 128])
    Y = nc.dram_tensor("Y", [NJ, 128, 512], F32, kind="ExternalOutput").ap()
    scale = float(128 ** -0.5)
    skip = set(os.environ.get("NSA_SKIP", "").split(","))
    with contextlib.ExitStack() as es:
        S = Sched(nc, es)
        idf, b_idf = load_const(S, "id", ID, [128, 128])
        kg0, b_kg0 = load_const(S, "kg0", KG0, [128, 128])
        stages = None

        def bf_const(name, ap, ncols, piece=2048):
            t, b = S.sb(name, [128, ncols], BF16)
            _load_cast_cols(S, t, b, ap, 128, ncols, stages, piece=piece)
            return t, b
        w1, b_w1 = bf_const("w1", W1, 8192)
        posT, b_posT = bf_const("posT", POST, 64)
        w2, b_w2 = bf_const("w2", W2, 256)
        kcx, b_kcx = bf_const("kcx", KCT, TT_)
        vcx, b_vcx = bf_const("vcx", VCT, TT_)
        ksT, b_ksT = bf_const("ksT", KST, TT_)
        kwT, b_kwT = bf_const("kwT", KWT, TT_)
        vsa, b_vsa = bf_const("vsa", VSA, NB * 129, piece=2064)
        vwa, b_vwa = bf_const("vwa", VWA, NB * 129, piece=2064)
        ov, b_ov = bf_const("ov", OV, 512)
        exE, b_exE = bf_const("exE", EX, NB * 128)
        caus, b_caus = bf_const("caus", CAUS, 1024)
        winm, b_winm = bf_const("winm", WINM, 1536)
        vsa3 = vsa[:].rearrange("p (b d) -> p b d", d=129)
        vwa3 = vwa[:].rearrange("p (b d) -> p b d", d=129)
        w1v = w1[:].rearrange("p (x l j) -> p x l j", x=2, l=32)
        A = [S.ps("A%d" % k, [128, 512], F32) for k in range(3)]
        O, b_O = S.ps("O", [128, 4, 256], F32)
        IMP, b_IMP = S.ps("IMP", [128, 4, 128], F32)
        Mk0, b_Mk0 = S.ps("Mk0", [128, 512], F32)
        Mk, b_Mk = S.ps("Mk1", [128, 512], F32)
        Mks = [(Mk0, b_Mk0), (Mk, b_Mk)]
        stop_at = os.environ.get("NSA_STOP", "")
        if stop_at == "c0":
            S.finish([])
            return nc
        kcT, b_kcT = S.sb("kcT", [128, NCP], BF16)
        vca, b_vca = S.sb("vca", [128, NCC, 129], BF16)
        S.op("dve", lambda e: e.memset(vca[:], 1.0), [], [b_vca])
        hs, b_hs = S.sb("hs", [128, NCP], F32)
        t1, b_t1 = S.sb("t1", [128, NCP], F32)
        t2, b_t2 = S.sb("t2", [128, NCP], F32)
        G, b_G = S.sb("G", [128, NCP], BF16)
        cb, b_cb = S.sb("cb", [128, 8], F32)
        kcs, b_kcs = S.sb("kcs", [128, 128], F32)
        jk, b_jk = S.sb("jk", [128, 128], F32)
        for X in range(2):
            src = kcx if X == 0 else vcx
            b_src = b_kcx if X == 0 else b_vcx
            xv = src[:].rearrange("p (n s) -> p s n", s=16)
            pc_, b_pc = A[0]
            ph_, b_ph = A[1]
            for l in range(32):
                if "cb" in skip:
                    continue
                MM(S, pc_[:, 0:1], w1v[:, X, l, :], posT[:, X * 32 + l:X * 32 + l + 1], l == 0, l == 31,
                   [b_w1, b_posT], [b_pc])
            CP(S, "dve", cb[:, X:X + 1], pc_[:, 0:1], [b_pc], [b_cb])
            for l in range(32):
                rhs = xv[:, l, 0:NCMP] if l < 16 else xv[:, l - 16, 1:1 + NCMP]
                if "ht" in skip:
                    continue
                MM(S, ph_[:, 0:NCMP], w1v[:, X, l, :], rhs, l == 0, l == 31, [b_w1, b_src], [b_ph])
            S.op("dve", lambda e: e.memset(hs[:], 0.0), [], [b_hs])
            ACT(S, hs[:, 0:NCMP], ph_[:, 0:NCMP], AF.Identity, [b_ph, b_cb], [b_hs], bias=cb[:, X:X + 1])
            TT(S, "dve", t1[:], hs[:], hs[:], ALU.mult, [b_hs], [b_t1])
            TS(S, "dve", t1[:], t1[:], 0.044715, 1.0, ALU.mult, ALU.add, R=[b_t1], W=[b_t1])
            TT(S, "dve", t1[:], t1[:], hs[:], ALU.mult, [b_t1, b_hs], [b_t1])
            ACT(S, t2[:], t1[:], AF.Sigmoid, [b_t1], [b_t2], scale=GELU_C)
            TT(S, "dve", G[:], hs[:], t2[:], ALU.mult, [b_hs, b_t2], [b_G])
            for ch in range(NCC):
                po_, b_po = A[ch % 2]
                MM(S, po_[:, 0:128], G[:, ch * 128:(ch + 1) * 128], w2[:, X * 128:(X + 1) * 128], True, True,
                   [b_G, b_w2], [b_po])
                if X == 0:
                    ACT(S, jk[:], po_[:, 0:128], AF.Square, [b_po], [b_jk, b_cb], accum=cb[:, 2:3])
                    TS(S, "dve", cb[:, 3:4], cb[:, 2:3], 1.0 / 128, EPS, ALU.mult, ALU.add, R=[b_cb], W=[b_cb])
                    RSTD(S, cb[:, 4:5], cb[:, 5:6], cb[:, 3:4], [b_cb], [b_cb])
                    STT(S, "dve", kcs[:], po_[:, 0:128], cb[:, 4:5], kg0[:], ALU.mult, ALU.mult,
                        [b_po, b_cb, b_kg0], [b_kcs])
                    TR(S, Mk[:, 384:512], kcs[:], idf[:], [b_kcs, b_idf], [b_Mk])
                    CP(S, "act", kcT[:, ch * 128:(ch + 1) * 128], Mk[:, 384:512], [b_Mk], [b_kcT])
                else:
                    CP(S, "act", vca[:, ch, 0:128], po_[:, 0:128], [b_po], [b_vca])
        if stop_at == "c1":
            S.finish([])
            return nc
        qnf = [S.sb("qnf%d" % k, [128, 512], F32) for k in range(2)]
        qrf = [S.sb("qrf%d" % k, [128, 512], F32) for k in range(2)]
        qnb = [S.sb("qnb%d" % k, [128, 512], BF16) for k in range(2)]
        qrb = [S.sb("qrb%d" % k, [128, 512], BF16) for k in range(2)]
        gts = [S.sb("gt%d" % k, [128, 12], F32) for k in range(2)]
        cms = [S.sb("cm%d" % k, [128, 512], F32) for k in range(2)]
        sbs = [S.sb("sb%d" % k, [128, 128], F32) for k in range(2)]
        ebuf = [S.sb("e%d" % k, [128, 512], F32) for k in range(4)]
        pbuf = [S.sb("p%d" % k, [128, 4, 128], BF16) for k in range(4)]
        obuf = [S.sb("ob%d" % k, [128, 512], F32) for k in range(2)]
        ocmp, b_ocmp = S.sb("ocmp", [128, 4, 128], F32)
        oslc, b_oslc = S.sb("oslc", [128, 4, 128], F32)
        imp, b_imp = S.sb("imp", [128, 128], F32)
        imp2, b_imp2 = S.sb("imp2", [128, 128], F32)
        self_, b_self = S.sb("self", [128, 128], F32)
        selT, b_selT = S.sb("selT", [128, 128], BF16)
        zz, b_zz = S.sb("zz", [128, 48], F32)
        cf, b_cf = S.sb("cf", [128, 12], F32)
        ne = 0

        pending = []
        SKEW = 2

        def flush(keep=0):
            while len(pending) > keep:
                pending.pop(0)()

        def attend(kT_ap, q_b, b_q, b_kT, mask_ap, mask_bufs, extra, v_ap, b_v, first, last, imp_ch=None):
            nonlocal ne
            a_, b_a = A[ne % 3]
            e_, b_e = ebuf[ne % 4]
            p_, b_p = pbuf[ne % 4]
            ne += 1
            MM(S, a_[:], kT_ap, q_b[:], True, True, [b_kT, b_q], [b_a])
            ACT(S, e_[:], a_[:], AF.Exp, [b_a], [b_e], scale=scale)
            TT(S, "dve", p_[:], e_[:].rearrange("p (h q) -> p h q", h=4),
               mask_ap.unsqueeze(1).to_broadcast([128, 4, 128]), ALU.mult, [b_e] + mask_bufs, [b_p])
            if extra is not None:
                TT(S, "pool", p_[:], p_[:], extra[0].unsqueeze(1).to_broadcast([128, 4, 128]), ALU.mult,
                   [b_p, extra[1]], [b_p])

            def stage2():
                for h in range(4):
                    MMG(S, O[:, h, 0:129], p_[:, h, :], v_ap, first and h % 2 == 0, [b_p, b_v], [b_O])
                if imp_ch is not None:
                    for h in range(4):
                        MMG(S, IMP[:, h, :], p_[:, h, :], ov[:, imp_ch * 128:(imp_ch + 1) * 128],
                            imp_ch == 0 and h == 0, [b_p, b_ov], [b_IMP])
            pending.append(stage2)
            flush(SKEW)

        def finish_branch(zoff, dst, b_dst):
            TS(S, "dve", zz[:, zoff:zoff + 4], O[:, :, 128], 1e-30, None, ALU.max, R=[b_O], W=[b_zz])
            S.op("dve", lambda e: e.reciprocal(out=zz[:, zoff + 4:zoff + 8], in_=zz[:, zoff:zoff + 4]), [b_zz], [b_zz])
            if dst is not None:
                CP(S, "dve", dst[:], O[:, :, 0:128], [b_O], [b_dst])

        for j in range(NJ):
            NBj = 8 * j + 8
            NCj = min(NCC, (64 * j + 62) // 128 + 1)
            qn_f, b_qnf = qnf[j % 2]
            qr_f, b_qrf = qrf[j % 2]
            qn_b, b_qnb = qnb[j % 2]
            qr_b, b_qrb = qrb[j % 2]
            gt, b_gt = gts[j % 2]
            cm, b_cm = cms[j % 2]
            sbi, b_sbi = sbs[j % 2]
            S.dma("sp", qn_f[:], QNT[j], None, b_qnf)
            S.dma("act", qr_f[:], QRT[j], None, b_qrf)
            S.dma("sp", gt[:], GATES[j], None, b_gt)
            S.dma("act", cm[:], CMASK[j], None, b_cm)
            S.dma("sp", sbi[:], SELB[j], None, b_sbi)
            CP(S, "pool", qn_b[:], qn_f[:], [b_qnf], [b_qnb])
            CP(S, "pool", qr_b[:], qr_f[:], [b_qrf], [b_qrb])
            for ch in range(NCj):
                attend(kcT[:, ch * 128:(ch + 1) * 128], qn_b, b_qnb, b_kcT, cm[:, ch * 128:(ch + 1) * 128],
                       [b_cm], None, vca[:, ch, :], b_vca, ch == 0, ch == NCj - 1, imp_ch=ch)
            flush()
            finish_branch(0, ocmp, b_ocmp)
            TS(S, "dve", imp[:], IMP[:, 0, :], zz[:, 4:5], None, ALU.mult, R=[b_IMP, b_zz], W=[b_imp])
            for h in range(1, 4):
                STT(S, "dve", imp[:], IMP[:, h, :], zz[:, 4 + h:5 + h], imp[:], ALU.mult, ALU.add,
                    [b_IMP, b_zz, b_imp], [b_imp])
            TT(S, "dve", imp[:], imp[:], sbi[:], ALU.add, [b_imp, b_sbi], [b_imp])
            if "top" not in skip:
                S.op("dve", lambda e: e.max(out=zz[:, 24:32], in_=imp[:]), [b_imp], [b_zz])
                S.op("dve", lambda e: e.match_replace(out=imp2[:], in_to_replace=zz[:, 24:32], in_values=imp[:],
                                                      imm_value=-3.0e38), [b_imp, b_zz], [b_imp2])
                S.op("dve", lambda e: e.max(out=zz[:, 32:40], in_=imp2[:]), [b_imp2], [b_zz])
            RED(S, "dve", zz[:, 40:41], zz[:, 32:40], ALU.min, [b_zz], [b_zz])
            TS(S, "dve", self_[:], imp[:], zz[:, 40:41], None, ALU.is_ge, R=[b_imp, b_zz], W=[b_self])
            TR(S, Mk[:, 384:512], self_[:], idf[:], [b_self, b_idf], [b_Mk])
            CP(S, "act", selT[:], Mk[:, 384:512], [b_Mk], [b_selT])
            for kb in range(NBj):
                if "slc" in skip:
                    continue
                mkt, b_mks = Mks[kb % 2]
                MM(S, mkt[:, 0:128], exE[:, kb * 128:(kb + 1) * 128], selT[:], True, True,
                   [b_exE, b_selT], [b_mks])
                extra = None
                if kb >= NBj - 8 and "extra" not in skip:
                    r = kb - (NBj - 8)
                    extra = (caus[:, r * 128:(r + 1) * 128], b_caus)
                attend(ksT[:, kb * 128:(kb + 1) * 128], qr_b, b_qrb, b_ksT, mkt[:, 0:128], [b_mks], extra,
                       vsa3[:, kb, :], b_vsa, kb == 0, kb == NBj - 1)
            flush()
            finish_branch(8, oslc, b_oslc)
            blks = [(r, 8 * j - 4 + r) for r in range(12) if 8 * j - 4 + r >= 0]
            for n_, (r, blk) in enumerate(blks):
                if "win" in skip:
                    continue
                attend(kwT[:, blk * 128:(blk + 1) * 128], qr_b, b_qrb, b_kwT, winm[:, r * 128:(r + 1) * 128],
                       [b_winm], None, vwa3[:, blk, :], b_vwa, n_ == 0, n_ == len(blks) - 1)
            flush()
            finish_branch(16, None, None)
            gv = gt[:].rearrange("p (h b) -> p h b", b=3)
            for b_i, zo in enumerate((4, 12, 20)):
                TT(S, "dve", cf[:, b_i * 4:(b_i + 1) * 4], gv[:, :, b_i], zz[:, zo:zo + 4], ALU.mult,
                   [b_gt, b_zz], [b_cf])
            o_, b_o = obuf[j % 2]
            for h in range(4):
                oh = o_[:, h * 128:(h + 1) * 128]
                TS(S, "dve", oh, ocmp[:, h, :], cf[:, h:h + 1], None, ALU.mult, R=[b_ocmp, b_cf], W=[b_o])
                STT(S, "dve", oh, oslc[:, h, :], cf[:, 4 + h:5 + h], oh, ALU.mult, ALU.add,
                    [b_oslc, b_cf, b_o], [b_o])
                STT(S, "dve", oh, O[:, h, 0:128], cf[:, 8 + h:9 + h], oh, ALU.mult, ALU.add,
                    [b_O, b_cf, b_o], [b_o])
            S.dma("pool", Y[j], o_[:], b_o, None, sem_on="src")
        S.finish([(b.dsem, b.dcnt) for _, b in obuf])
    return nc


def run_nsa(Yp, pc, cmp_pos, cmp_w1, cmp_w2, k_g0):
    Ttot = Yp.shape[0]
    NJ = Ttot // (128 * NCORES)
    NB = NJ * 8
    nc = _get(("nsa", NJ), lambda: build_nsa(NJ))
    NCMP = (Ttot - 32) // 16 + 1
    NSEL = Ttot // 64
    w1 = np.ascontiguousarray(np.transpose(cmp_w1.reshape(2, 32, 128, 128), (2, 0, 1, 3))).reshape(128, 8192)
    posT = np.ascontiguousarray(np.transpose(cmp_pos, (2, 0, 1))).reshape(128, 64)
    w2 = np.ascontiguousarray(np.transpose(cmp_w2, (1, 0, 2))).reshape(128, 256)
    n_all = np.arange(512)
    jb = np.arange(128)
    ovm = ((n_all[:, None] >= 4 * jb[None, :] - 1) & (n_all[:, None] <= 4 * jb[None, :] + 3)
           & (n_all[:, None] < NCMP) & (jb[None, :] < NSEL)).astype(np.float32)
    ov = np.ascontiguousarray(np.transpose(ovm.reshape(4, 128, 128), (1, 0, 2))).reshape(128, 512)
    s_ = np.arange(128)
    exE = np.zeros((128, NB, 128), np.float32)
    for kb in range(NB):
        exE[2 * kb + s_ // 64, kb, s_] = 1.0 if True else 0.0
    exE = exE[:128].reshape(128, NB * 128) if 2 * NB <= 128 else exE.reshape(128, NB * 128)
    tri = (s_[:, None] <= s_[None, :]).astype(np.float32)
    common = {"kcmpT": np.ascontiguousarray(pc[:, 512:640].T), "vcmpT": np.ascontiguousarray(pc[:, 640:768].T),
              "w1": w1, "posT": posT, "w2": w2, "kg0": rep128(k_g0),
              "ksT": np.ascontiguousarray(Yp[:, 1024:1152].T), "kwT": np.ascontiguousarray(Yp[:, 1152:1280].T),
              "vsa": v_aug(np.ascontiguousarray(pc[:, 896:1024])), "vwa": v_aug(np.ascontiguousarray(pc[:, 1152:1280])),
              "ov": ov, "expE": exE, "ident": IDENT}
    maps = []
    for c in range(NCORES):
        own = [8 * j + c for j in range(NJ)]
        caus = np.zeros((128, 8, 128), np.float32)
        for r in range(8):
            if r < c:
                caus[:, r, :] = 1.0
            elif r == c:
                caus[:, r, :] = tri
        winT = np.zeros((128, 12, 128), np.float32)
        for r in range(12):
            d = r - 4 - c
            if d == 0:
                winT[:, r, :] = tri
            elif d in (-1, -2, -3):
                winT[:, r, :] = 1.0
            elif d == -4:
                winT[:, r, :] = 1.0 - tri
        cmask = np.zeros((NJ, 128, 4, 128), np.float32)
        selb = np.zeros((NJ, 128, 128), np.float32)
        for j, g in enumerate(own):
            t = g * 128 + s_
            nn = np.arange(512).reshape(4, 128)
            ok = (16 * nn[:, :, None] + 31 <= t[None, None, :]) & (nn[:, :, None] < NCMP)
            cmask[j] = np.transpose(ok, (1, 0, 2)).astype(np.float32)
            cur = t // 64
            forced = (jb[None, :] == 0) | (jb[None, :] == cur[:, None]) | (jb[None, :] == cur[:, None] - 1)
            future = jb[None, :] > cur[:, None]
            selb[j] = np.where(forced, 1.0e30, np.where(future, NEGBIG, 0.0)).astype(np.float32)
        m = dict(common)
        m.update({"qnT": own_tiles_T(Yp[:, 0:512], 4, 128, c, NJ), "qrT": own_tiles_T(Yp[:, 512:1024], 4, 128, c, NJ),
                  "gates": np.ascontiguousarray(np.stack([Yp[g * 128:(g + 1) * 128, 2496:2508] for g in own])),
                  "cmask": cmask.reshape(NJ, 128, 512), "selb": selb,
                  "causT": caus.reshape(128, 1024), "winT": winT.reshape(128, 1536)})
        maps.append(m)
    res = _run(nc, maps)
    out = np.zeros((Ttot, 512), np.float32)
    for c in range(NCORES):
        for j in range(NJ):
            g = 8 * j + c
            out[g * 128:(g + 1) * 128] = res[c]["Y"][j]
    return out


def build_peer(NTL, NCH_E=128):
    nc = bass.Bass("TRN2", target_bir_lowering=False)
    T = NTL * 128
    TW = T
    K = 2048
    KC = 16
    dr = lambda name, shape: nc.dram_tensor(name, shape, F32, kind="ExternalInput").ap()
    X = dr("X", [T, K])
    G_ = dr("pg", [128, K])
    SC = dr("psc", [128, K])
    SH = dr("psh", [128, K])
    GATE = dr("gate", [128, K])
    WQ = dr("wq", [128, KC * 1024])
    SK = dr("skT", [128, 1024])
    UT = dr("UT", [NCH_E, 128, KC * 128])
    V = dr("V", [NCH_E * 128, K])
    ID = dr("ident", [128, 128])
    Y = nc.dram_tensor("Y", [T, K], F32, kind="ExternalOutput").ap()
    with contextlib.ExitStack() as es:
        S = Sched(nc, es)
        idb, b_idb = load_const(S, "id", ID, [128, 128], cast=BF16)
        A, b_A = load_const(S, "A", SC, [128, K])
        B, b_B = load_const(S, "B", SH, [128, K])
        xs = [S.sb("x%d" % j, [128, K], F32) for j in range(2)]
        tmp, b_tmp = S.sb("tmp", [128, K], F32)
        S.dma("sp", tmp[:], G_, None, b_tmp)
        STT(S, "dve", A[:], A[:], 1.0, tmp[:], ALU.add, ALU.mult, [b_A, b_tmp], [b_A])
        hb, b_hb = S.sb("hb", [128, K], BF16)
        st, b_st = S.sb("st", [128, 8], F32)
        hnT, b_hnT = S.sb("hnT", [128, KC, TW], BF16)
        accw, b_accw = S.sb("accw", [128, NTL * K], F32)
        wqb = accw[:].bitcast(BF16)
        assert 2 * NTL * K >= KC * 1024
        skb, b_skb = S.sb("skb", [128, 1024], BF16)
        PA = [S.ps("PA%d" % k, [128, 512], F32) for k in range(4)]
        PV = [S.ps("PV%d" % k, [128, 512], F32) for k in range(4)]
        stg = [(xs[0][0], xs[0][1]), (xs[1][0], xs[1][1])]
        _load_cast_cols(S, accw[:].bitcast(BF16), b_accw, WQ, 128, KC * 1024, stg)
        _load_cast_cols(S, skb, b_skb, SK, 128, 1024, stg)
        s2s = [S.sb("s2_%d" % i, [128, 8, 128], F32) for i in range(NTL)]
        THR = [S.sb("thr_%d" % i, [128, 8, 128], F32) for i in range(NTL)]
        BIA = [S.sb("bia_%d" % i, [128, 8, 128], F32) for i in range(NTL)]
        qT, b_qT = S.sb("qT", [128, 8, 128], BF16)
        sv, b_sv = S.sb("sv", [128, 8, 2, 16], F32)
        mr, b_mr = S.sb("mr", [128, 256], F32)
        cand, b_cand = S.sb("cand", [128, 8, 256], F32)
        cv, b_cv = S.sb("cv", [128, 8, 16], F32)
        sm, b_sm = S.sb("sm", [128, 6, 8], F32)
        jk16, b_jk16 = S.sb("jk16", [128, 16], F32)
        for i in range(NTL):
            x, b_x = xs[i % 2]
            S.dma("sp", x[:], X[i * 128:(i + 1) * 128, :], None, b_x)
            ACT(S, tmp[:], x[:], AF.Square, [b_x], [b_tmp, b_st], accum=st[:, 0:1])
            TS(S, "dve", st[:, 1:2], st[:, 0:1], 1.0 / K, EPS, ALU.mult, ALU.add, R=[b_st], W=[b_st])
            RSTD(S, st[:, 2:3], st[:, 3:4], st[:, 1:2], [b_st], [b_st])
            STT(S, "dve", tmp[:], x[:], st[:, 2:3], A[:], ALU.mult, ALU.mult, [b_x, b_st, b_A], [b_tmp])
            TT(S, "dve", hb[:], tmp[:], B[:], ALU.add, [b_tmp, b_B], [b_hb])
            for q4 in range(KC // 4):
                pt_, b_pt = PA[q4 % 2]
                ptb = pt_[:].bitcast(BF16)
                for t in range(4):
                    kc = q4 * 4 + t
                    TR(S, ptb[:, t * 128:(t + 1) * 128], hb[:, kc * 128:(kc + 1) * 128], idb[:], [b_hb, b_idb],
                       [b_pt], acc=(t > 0))
                CP(S, "act" if q4 % 2 == 0 else "dve", hnT[:, q4 * 4:(q4 + 1) * 4, i * 128:(i + 1) * 128],
                   ptb[:, 0:512].rearrange("p (a b) -> p a b", a=4), [b_pt], [b_hnT])
            for h in range(8):
                pq, b_pq = PA[2 + h % 2]
                for kc in range(KC):
                    MM(S, pq[:, 0:128], wqb[:, kc * 1024 + h * 128:kc * 1024 + (h + 1) * 128],
                       hnT[:, kc, i * 128:(i + 1) * 128], kc == 0, kc == KC - 1, [b_accw, b_hnT], [b_pq])
                CP(S, "act", qT[:, h, :], pq[:, 0:128], [b_pq], [b_qT])
            s2, b_s2 = s2s[i]
            s1, b_s1 = BIA[i]
            for p_ in range(2):
                dst, b_dst = (s1, b_s1) if p_ == 0 else (s2, b_s2)
                lo, hi = p_ * 64, (p_ + 1) * 64
                for hh in range(2):
                    ps_, b_ps = PA[hh]
                    for h4 in range(4):
                        h = hh * 4 + h4
                        MM(S, ps_[:, h4 * 128:(h4 + 1) * 128], qT[lo:hi, h, :], skb[lo:hi, h * 128:(h + 1) * 128],
                           True, True, [b_qT, b_skb], [b_ps])
                    CP(S, "act", dst[:, hh * 4:(hh + 1) * 4, :], ps_[:].rearrange("p (a b) -> p a b", a=4),
                       [b_ps], [b_dst])
            for h in range(8):
                for p_ in range(2):
                    src = s1[:, h, :] if p_ == 0 else s2[:, h, :]
                    b_src = b_s1 if p_ == 0 else b_s2
                    S.op("dve", lambda e, src=src, h=h, p_=p_: e.max(out=sv[:, h, p_, 0:8], in_=src), [b_src], [b_sv])
                    S.op("dve", lambda e, src=src, h=h, p_=p_: e.match_replace(
                        out=mr[:, 0:128], in_to_replace=sv[:, h, p_, 0:8], in_values=src, imm_value=-3.0e38),
                        [b_src, b_sv], [b_mr])
                    S.op("dve", lambda e, h=h, p_=p_: e.max(out=sv[:, h, p_, 8:16], in_=mr[:, 0:128]), [b_mr], [b_sv])
            for h in range(8):
                TT(S, "dve", cand[:, h, :].rearrange("p (a b) -> p a b", a=16),
                   sv[:, h, 0, :].unsqueeze(2).to_broadcast([128, 16, 16]),
                   sv[:, h, 1, :].unsqueeze(1).to_broadcast([128, 16, 16]), ALU.add, [b_sv], [b_cand])
                S.op("dve", lambda e, h=h: e.max(out=cv[:, h, 0:8], in_=cand[:, h, :]), [b_cand], [b_cv])
                S.op("dve", lambda e, h=h: e.match_replace(out=mr[:], in_to_replace=cv[:, h, 0:8],
                                                            in_values=cand[:, h, :], imm_value=-3.0e38),
                     [b_cand, b_cv], [b_mr])
                S.op("dve", lambda e, h=h: e.max(out=cv[:, h, 8:16], in_=mr[:]), [b_mr], [b_cv])
            RED(S, "dve", sm[:, 0, :], cv[:], ALU.min, [b_cv], [b_sm])
            RED(S, "dve", sm[:, 1, :], cv[:], ALU.max, [b_cv], [b_sm])
            TS(S, "dve", sm[:, 2, :], sm[:, 1, :], -1.0, None, ALU.mult, R=[b_sm], W=[b_sm])
            S.op("dve", lambda e: e.memset(sm[:, 3, :], 0.0), [b_sm], [b_sm])
            for h in range(8):
                ACT(S, jk16[:], cv[:, h, :], AF.Exp, [b_cv, b_sm], [b_jk16, b_sm], bias=sm[:, 2, h:h + 1],
                    accum=sm[:, 3, h:h + 1])
            ACT(S, sm[:, 4, :], sm[:, 3, :], AF.Ln, [b_sm], [b_sm])
            STT(S, "dve", sm[:, 5, :], sm[:, 4, :], -1.0, sm[:, 2, :], ALU.mult, ALU.add, [b_sm], [b_sm])
            TS(S, "dve", sm[:, 0, :], sm[:, 0, :], -1.0e-5, None, ALU.add, R=[b_sm], W=[b_sm])
            thr_, b_thr = THR[i]
            TT(S, "dve", thr_[:], sm[:, 0, :].unsqueeze(2).to_broadcast([128, 8, 128]), s1[:], ALU.subtract,
               [b_sm, b_s1], [b_thr])
            TT(S, "dve", s1[:], s1[:], sm[:, 5, :].unsqueeze(2).to_broadcast([128, 8, 128]), ALU.add,
               [b_s1, b_sm], [b_s1])
        NWB = 4
        NWU = 3
        utb = [S.sb("utb%d" % k, [128, KC * 128], BF16) for k in range(NWU)]
        vtb = [S.sb("vtb%d" % k, [128, K], BF16) for k in range(NWB)]
        ssb = [S.sb("ssb%d" % k, [128, TW], F32) for k in range(2)]
        g1_ = [S.sb("g1_%d" % k, [128, TW], F32) for k in range(2)]
        g2_ = [S.sb("g2_%d" % k, [128, TW], F32) for k in range(2)]
        PTs = [S.sb("PT%d" % k, [128, TW], BF16) for k in range(2)]
        NEX = 8
        exs = [S.sb("ex%d" % k, [128, 128], F32) for k in range(NEX)]
        wms = [S.sb("wm%d" % k, [128, 128], BF16) for k in range(NEX)]
        acc3 = accw[:].rearrange("p (i d) -> p i d", i=NTL)
        nw = 0
        npv = 0

        def load_chunk(c):
            ub, b_ub = utb[c % NWU]
            vb, b_vb = vtb[c % NWB]
            S.dma("pool", ub[:], UT[c], None, b_ub)
            S.dma("pool", vb[:], V[c * 128:(c + 1) * 128, :], None, b_vb)

        def finish_chunk(c):
            nonlocal npv
            at, b_at = PA[2 + c % 2]
            gb, b_gb = g2_[c % 2]
            pt_, b_pt = PTs[c % 2]
            TT(S, "dve", pt_[:], at[:, 0:TW], gb[:], ALU.mult, [b_at, b_gb], [b_pt])
            if c % 2 == 1:
                c0 = c - 1
                for i in range(NTL):
                    for dq in range(4):
                        pv, b_pv = PV[npv % 4]
                        npv += 1
                        MM(S, pv[:], PTs[0][0][:, i * 128:(i + 1) * 128], vtb[c0 % NWB][0][:, dq * 512:(dq + 1) * 512],
                           True, False, [PTs[0][1], vtb[c0 % NWB][1]], [b_pv])
                        MM(S, pv[:], PTs[1][0][:, i * 128:(i + 1) * 128], vtb[c % NWB][0][:, dq * 512:(dq + 1) * 512],
                           False, True, [PTs[1][1], vtb[c % NWB][1]], [b_pv])
                        dst = acc3[:, i, dq * 512:(dq + 1) * 512]
                        if c0 == 0:
                            CP(S, "dve", dst, pv[:], [b_pv], [b_accw])
                        else:
                            TT(S, "dve", dst, dst, pv[:], ALU.add, [b_accw, b_pv], [b_accw])

        def gelu_b(c):
            s_sb, b_ssb = ssb[c % 2]
            ga, b_ga = g1_[c % 2]
            gb, b_gb = g2_[c % 2]
            ACT(S, gb[:], ga[:], AF.Sigmoid, [b_ga], [b_gb], scale=GELU_C)
            TT(S, "pool", gb[:], gb[:], s_sb[:], ALU.mult, [b_gb, b_ssb], [b_gb])

        load_chunk(0)
        load_chunk(1)
        for c in range(NCH_E):
            ub, b_ub = utb[c % NWU]
            su, b_su = PA[c % 2]
            at, b_at = PA[2 + c % 2]
            for kc in range(KC):
                MM(S, su[:, 0:TW], ub[:, kc * 128:(kc + 1) * 128], hnT[:, kc, :], kc == 0, kc == KC - 1,
                   [b_ub, b_hnT], [b_su])
            for i in range(NTL):
                s2, b_s2 = s2s[i]
                for h in range(8):
                    ex, b_ex = exs[nw % NEX]
                    wm, b_wm = wms[nw % NEX]
                    nw += 1
                    ACT(S, ex[:], s2[:, h, :], AF.Exp, [b_s2, BIA[i][1]], [b_ex], bias=BIA[i][0][:, h, c:c + 1])
                    STT(S, "dve", wm[:], s2[:, h, :], THR[i][0][:, h, c:c + 1], ex[:], ALU.is_ge, ALU.mult,
                        [b_s2, THR[i][1], b_ex], [b_wm])
                    MMG(S, at[:, i * 128:(i + 1) * 128], wm[:], idb[:], i == 0 and h == 0, [b_wm, b_idb], [b_at])
            if c >= 1:
                gelu_b(c - 1)
            s_sb, b_ssb = ssb[c % 2]
            ga, b_ga = g1_[c % 2]
            CP(S, "act", s_sb[:], su[:, 0:TW], [b_su], [b_ssb])
            TT(S, "pool", ga[:], s_sb[:], s_sb[:], ALU.mult, [b_ssb], [b_ga])
            TS(S, "pool", ga[:], ga[:], 0.044715, 1.0, ALU.mult, ALU.add, R=[b_ga], W=[b_ga])
            TT(S, "pool", ga[:], ga[:], s_sb[:], ALU.mult, [b_ga, b_ssb], [b_ga])
            if c >= 1:
                finish_chunk(c - 1)
            if c + 2 < NCH_E:
                load_chunk(c + 2)
        gelu_b(NCH_E - 1)
        finish_chunk(NCH_E - 1)
        gate, b_gate = A, b_A
        S.dma("sp", gate[:], GATE, None, b_gate)
        for i in range(NTL):
            x, b_x = xs[i % 2]
            S.dma("sp", x[:], X[i * 128:(i + 1) * 128, :], None, b_x)
            TT(S, "dve", acc3[:, i, :], acc3[:, i, :], gate[:], ALU.mult, [b_accw, b_gate], [b_accw])
            TT(S, "pool", x[:], x[:], acc3[:, i, :], ALU.add, [b_x, b_accw], [b_x])
            S.dma("pool", Y[i * 128:(i + 1) * 128, :], x[:], b_x, None, sem_on="src")
        S.finish([(b.dsem, b.dcnt) for _, b in xs])
    return nc


def peer_weights(peer_wq, peer_subkeys, peer_u):
    wq = np.ascontiguousarray(np.transpose(peer_wq.reshape(16, 128, 1024), (1, 0, 2))).reshape(128, 16 * 1024)
    skT = np.ascontiguousarray(np.transpose(peer_subkeys, (1, 3, 0, 2))).reshape(128, 1024)
    UT = np.ascontiguousarray(np.transpose(peer_u.reshape(128, 128, 16, 128), (0, 3, 2, 1))).reshape(128, 128, 2048)
    return wq, skT, UT


def run_peer(x1, norm2_g, sc2, sh2, g2, wq, skT, UT, peer_v, NTL=4):
    Ttot = x1.shape[0]
    nc = _get(("peer", NTL), lambda: build_peer(NTL))
    T = NTL * 128
    per = T * NCORES
    outs = []
    pg, psc, psh, gate = rep128(norm2_g), rep128(sc2), rep128(sh2), rep128(g2)
    for r0 in range(0, Ttot, per):
        maps = []
        for c in range(NCORES):
            maps.append({"X": np.ascontiguousarray(x1[r0 + c * T:r0 + (c + 1) * T]), "pg": pg, "psc": psc, "psh": psh,
                         "gate": gate, "wq": wq, "skT": skT, "UT": UT, "V": peer_v, "ident": IDENT})
        res = _run(nc, maps)
        outs.append(np.concatenate([r["Y"] for r in res], axis=0))
    return np.concatenate(outs, axis=0)


def run_ada(c, ada_w, ada_b):
    L, K, N = ada_w.shape
    NC = L * N // NCORES
    nc = _get(("ada", K, NC), lambda: build_gemm(128, K, NC, False, True, silu=True))
    X = np.ascontiguousarray(np.broadcast_to(c.reshape(1, K), (128, K))).astype(np.float32)
    ones = np.ones((128, NC), np.float32)
    maps = []
    for core in range(NCORES):
        l, c0 = divmod(core * NC, N)
        maps.append({"X": X, "W": np.ascontiguousarray(ada_w[l][:, c0:c0 + NC]), "ident": IDENT,
                     "resid": rep128(ada_b[l][c0:c0 + NC]), "gate": ones})
    res = _run(nc, maps)
    flat = np.concatenate([r["Y"][0] for r in res])
    return flat.reshape(L, N)


def kernel(x, c, positions, ada_w, ada_b, norm1_g, norm2_g, w_in, w_out,
           conv_w, conv_b, conv_ln_g, conv_ln_b, sgu_ln_g, sgu_ln_b, sgu_w, sgu_b,
           nsa_q_g, nsa_k_g, nsa_cmp_pos, nsa_cmp_w1, nsa_cmp_w2, dsa_q_g, dsa_k_g,
           peer_wq, peer_subkeys, peer_u, peer_v):
    f = lambda a: np.asarray(a, dtype=np.float32)
    x = f(x)[0]
    pos = np.asarray(positions)[0]
    ada = run_ada(f(c)[0], f(ada_w), f(ada_b))
    for i in range(ada.shape[0]):
        sh1, sc1, g1, sh2, sc2, g2 = np.split(ada[i], 6)
        proj = gemm(x, f(w_in[i]), pro=(rep128(f(norm1_g[i])), rep128(sc1), rep128(sh1)))
        ya = run_conv(proj[:, 0:1024], f(conv_w[i]), f(conv_b[i]), f(conv_ln_g[i]), f(conv_ln_b[i]))
        yb = run_sgu(proj[:, 1024:2048], f(sgu_ln_g[i]), f(sgu_ln_b[i]), f(sgu_w[i]), f(sgu_b[i]))
        pc = proj[:, 2048:2048 + 1292]
        pd = proj[:, 2048 + 1292:]
        Yp = run_prep(proj[:, 2048:], pos, f(nsa_q_g[i]), f(nsa_k_g[i]), f(dsa_q_g[i]), f(dsa_k_g[i]))
        yc = run_nsa(Yp, pc, f(nsa_cmp_pos[i]), f(nsa_cmp_w1[i]), f(nsa_cmp_w2[i]), f(nsa_k_g[i][0]))
        yd = run_dsa(Yp, np.ascontiguousarray(pd[:, 640:768]))
        ycat = np.concatenate([ya, yb, yc, yd], axis=1)
        x1 = gemm(ycat, f(w_out[i]), epi=(x, rep128(g1)))
        wq, skT, UT = peer_weights(f(peer_wq[i]), f(peer_subkeys[i]), f(peer_u[i]))
        x = run_peer(x1, f(norm2_g[i]), sc2, sh2, g2, wq, skT, UT, f(peer_v[i]))
    return x[None].astype(np.float32)
```
